# Optimizing a Trainium2 kernel written in Bass

```python
import functools
import jax, jax.numpy as jnp
from jax import lax
import numpy as np

D_MODEL = 1024
BATCH = 8
SEQ = 2048
DEPTH = 1
DEC_BATCH = 128
DEC_SEQ = 4
PAST_LEN = 8192
PAGE_SIZE = 128

CONV_DIM = 1024
CONV_WIDTH = 3
HEAD_DIM = 64
N_SLOTS = 8
GROUPS = ((128, 1), (512, 4), (2048, 16))
N_GROUPS = 3
ATTN_DIM = N_GROUPS * N_SLOTS * HEAD_DIM
ATTN_OUT = N_SLOTS * HEAD_DIM
Q_BLOCK = 128
PROJ_COLS = 3 * CONV_DIM + 3 * ATTN_DIM + 2 * D_MODEL
PEER_HEADS = 8
PEER_NKEYS = 128
PEER_EXPERTS = PEER_NKEYS * PEER_NKEYS
PEER_QDIM = 256
PEER_HALF = PEER_QDIM // 2
PEER_TOPK = 16
PEER_CHUNK = 256
EPS = 1e-6

kernel_name = 'hybrid_conv_dilated_attn_peer_step'


def _rmsnorm(x, g):
    xf = x.astype(jnp.float32)
    y = xf * lax.rsqrt(jnp.mean(xf * xf, axis=-1, keepdims=True) + EPS)
    return (y * g.astype(jnp.float32)).astype(x.dtype)


def _alibi_slopes():
    return jnp.exp2(-8.0 * jnp.arange(1, N_SLOTS + 1, dtype=jnp.float32) / N_SLOTS)


def _conv_branch(b_gate, c_gate, hv, prev, conv_w, w_out_a):
    u = c_gate * hv
    ext = jnp.concatenate([prev.astype(u.dtype), u], axis=1)
    t = u.shape[1]
    y = ext[:, 0:t] * conv_w[0]
    for i in range(1, CONV_WIDTH):
        y = y + ext[:, i:i + t] * conv_w[i]
    ya = (b_gate * y) @ w_out_a
    return ya, ext[:, ext.shape[1] - (CONV_WIDTH - 1):]


def _dilated_group(q, k_ext, v_ext, q_off, base_pos, dil, n_keys, slopes):
    tq = q.shape[1]
    t = jnp.arange(tq, dtype=jnp.int32)[:, None]
    i = jnp.arange(n_keys, dtype=jnp.int32)[None, :]
    j = q_off + t - dil * i
    valid = (j >= 0) & (base_pos + j >= 0)
    jc = jnp.clip(j, 0, k_ext.shape[1] - 1)
    kg = k_ext[:, jc]
    vg = v_ext[:, jc]
    dist = (dil * jnp.arange(n_keys)).astype(jnp.float32)
    s = jnp.einsum('bthd,btkhd->bthk', q, kg, preferred_element_type=jnp.float32)
    s = s * (HEAD_DIM ** -0.5) - slopes[:, None] * dist[None, :]
    s = jnp.where(valid[None, :, None, :], s, -jnp.inf)
    m = jnp.max(s, axis=-1, keepdims=True)
    p = jnp.exp(s - m)
    l = jnp.sum(p, axis=-1, keepdims=True)
    o = jnp.einsum('bthk,btkhd->bthd', p, vg.astype(jnp.float32)) / l
    lse = (m + jnp.log(l))[..., 0]
    return o, lse


def _combine(outs, lses):
    w = jax.nn.softmax(jnp.stack(lses, axis=0), axis=0)
    return jnp.sum(w[..., None] * jnp.stack(outs, axis=0), axis=0)


def _attn_prompt(q, k, v, slopes):
    bsz, seq = q.shape[:2]
    qs, kps, vps, bufs = [], [], [], []
    for g, (win, dil) in enumerate(GROUPS):
        kg, vg = k[:, :, g], v[:, :, g]
        pad = ((0, 0), (win, 0), (0, 0), (0, 0))
        qs.append(q[:, :, g])
        kps.append(jnp.pad(kg, pad))
        vps.append(jnp.pad(vg, pad))
        keep = min(win, seq)
        bufs.append(jnp.stack([kg[:, seq - keep:], vg[:, seq - keep:]], axis=2))

    def block(bi):
        t0 = bi * Q_BLOCK
        outs, lses = [], []
        for g, (win, dil) in enumerate(GROUPS):
            qb = lax.dynamic_slice_in_dim(qs[g], t0, Q_BLOCK, axis=1)
            kb = lax.dynamic_slice_in_dim(kps[g], t0, win + Q_BLOCK, axis=1)
            vb = lax.dynamic_slice_in_dim(vps[g], t0, win + Q_BLOCK, axis=1)
            o, lse = _dilated_group(qb, kb, vb, win, t0 - win, dil, win // dil + 1, slopes)
            outs.append(o)
            lses.append(lse)
        return _combine(outs, lses)

    o = lax.map(block, jnp.arange(seq // Q_BLOCK, dtype=jnp.int32))
    o = jnp.moveaxis(o, 0, 1).reshape(bsz, seq, N_SLOTS, HEAD_DIM)
    return o, bufs


def _attn_sample(q, k, v, bufs, slopes):
    outs, lses, new_bufs = [], [], []
    for g, (win, dil) in enumerate(GROUPS):
        buf = bufs[g]
        wb = buf.shape[1]
        k_ext = jnp.concatenate([buf[:, :, 0].astype(k.dtype), k[:, :, g]], axis=1)
        v_ext = jnp.concatenate([buf[:, :, 1].astype(v.dtype), v[:, :, g]], axis=1)
        o, lse = _dilated_group(q[:, :, g], k_ext, v_ext, wb, PAST_LEN - wb, dil, win // dil + 1, slopes)
        outs.append(o)
        lses.append(lse)
        n = k_ext.shape[1]
        new_bufs.append(jnp.stack([k_ext[:, n - wb:], v_ext[:, n - wb:]], axis=2))
    return _combine(outs, lses), new_bufs


def _peer(h, wq, keys, u_tab, v_tab):
    shp = h.shape
    flat = h.reshape(-1, D_MODEL)
    n = flat.shape[0]
    flat = jnp.pad(flat, ((0, (-n) % PEER_CHUNK), (0, 0)))
    chunks = flat.reshape(-1, PEER_CHUNK, D_MODEL)
    k1 = keys[0].astype(jnp.float32)
    k2 = keys[1].astype(jnp.float32)

    def body(hc):
        qh = (hc @ wq).astype(jnp.float32).reshape(PEER_CHUNK, PEER_HEADS, 2, PEER_HALF)
        s1 = jnp.einsum('chd,hnd->chn', qh[:, :, 0], k1)
        s2 = jnp.einsum('chd,hnd->chn', qh[:, :, 1], k2)
        v1, i1 = lax.top_k(s1, PEER_TOPK)
        v2, i2 = lax.top_k(s2, PEER_TOPK)
        cand = (v1[..., :, None] + v2[..., None, :]).reshape(PEER_CHUNK, PEER_HEADS, PEER_TOPK * PEER_TOPK)
        cidx = (i1[..., :, None] * PEER_NKEYS + i2[..., None, :]).reshape(PEER_CHUNK, PEER_HEADS, PEER_TOPK * PEER_TOPK)
        sv, si = lax.top_k(cand, PEER_TOPK)
        eidx = jnp.take_along_axis(cidx, si, axis=-1)
        gate = jax.nn.softmax(sv, axis=-1)
        act = jax.nn.gelu(jnp.einsum('cd,chkd->chk', hc, u_tab[eidx]).astype(jnp.float32), approximate=False)
        out = jnp.einsum('chk,chkd->cd', gate * act, v_tab[eidx].astype(jnp.float32))
        return out.astype(hc.dtype)

    out = lax.map(body, chunks).reshape(-1, D_MODEL)[:n]
    return out.reshape(shp)


def _layer(x, conv_prev, attend, g1, w_in, conv_w, w_out_a, w_out_b, w_o,
           g2, peer_wq, peer_keys, peer_u, peer_v):
    bsz, t, _ = x.shape
    h = _rmsnorm(x, g1)
    p = h @ w_in
    splits = [CONV_DIM, 2 * CONV_DIM, 3 * CONV_DIM,
              3 * CONV_DIM + ATTN_DIM, 3 * CONV_DIM + 2 * ATTN_DIM,
              3 * CONV_DIM + 3 * ATTN_DIM, 3 * CONV_DIM + 3 * ATTN_DIM + D_MODEL]
    b_gate, c_gate, hv, q, k, v, ga, gb = jnp.split(p, splits, axis=-1)
    ya, conv_new = _conv_branch(b_gate, c_gate, hv, conv_prev, conv_w, w_out_a)
    shp5 = (bsz, t, N_GROUPS, N_SLOTS, HEAD_DIM)
    ob, kv_new = attend(q.reshape(shp5), k.reshape(shp5), v.reshape(shp5))
    yb = ob.reshape(bsz, t, ATTN_OUT).astype(x.dtype) @ w_out_b
    x = x + (jax.nn.sigmoid(ga) * ya + jax.nn.sigmoid(gb) * yb) @ w_o
    x = x + _peer(_rmsnorm(x, g2), peer_wq, peer_keys, peer_u, peer_v)
    return x, kv_new, conv_new


def setup_inputs(seed: int = 0) -> dict:
    key = jax.random.key(seed)
    ks = jax.random.split(key, 20)

    def nrm(k, shape, scale):
        return jax.random.normal(k, shape, jnp.float32) * scale

    def kvbuf(k, win):
        return nrm(k, (DEPTH, DEC_BATCH, min(win, PAST_LEN), 2, N_SLOTS, HEAD_DIM), 1.0)

    return {
        'x_prompt': nrm(ks[0], (BATCH, SEQ, D_MODEL), 1.0),
        'x_sample': nrm(ks[1], (DEC_BATCH, DEC_SEQ, D_MODEL), 1.0),
        'cache_kv_w128': kvbuf(ks[2], GROUPS[0][0]),
        'cache_kv_w512': kvbuf(ks[3], GROUPS[1][0]),
        'cache_kv_w2048': kvbuf(ks[4], GROUPS[2][0]),
        'state_conv': nrm(ks[5], (DEPTH, DEC_BATCH, CONV_WIDTH - 1, CONV_DIM), 1.0),
        'norm1_g': 1.0 + nrm(ks[6], (DEPTH, D_MODEL), 0.02),
        'w_in': nrm(ks[7], (DEPTH, D_MODEL, PROJ_COLS), D_MODEL ** -0.5),
        'conv_w': nrm(ks[8], (DEPTH, CONV_WIDTH, CONV_DIM), CONV_WIDTH ** -0.5),
        'w_out_a': nrm(ks[9], (DEPTH, CONV_DIM, D_MODEL), CONV_DIM ** -0.5),
        'w_out_b': nrm(ks[10], (DEPTH, ATTN_OUT, D_MODEL), ATTN_OUT ** -0.5),
        'w_o': nrm(ks[11], (DEPTH, D_MODEL, D_MODEL), D_MODEL ** -0.5),
        'norm2_g': 1.0 + nrm(ks[12], (DEPTH, D_MODEL), 0.02),
        'peer_wq': nrm(ks[13], (DEPTH, D_MODEL, PEER_HEADS * PEER_QDIM), D_MODEL ** -0.5),
        'peer_keys': nrm(ks[14], (DEPTH, 2, PEER_HEADS, PEER_NKEYS, PEER_HALF), PEER_HALF ** -0.5),
        'peer_u': nrm(ks[15], (DEPTH, PEER_EXPERTS, D_MODEL), D_MODEL ** -0.5),
        'peer_v': nrm(ks[16], (DEPTH, PEER_EXPERTS, D_MODEL), 0.3),
        'final_g': 1.0 + nrm(ks[17], (D_MODEL,), 0.02),
    }


def reference(x_prompt, x_sample, cache_kv_w128, cache_kv_w512, cache_kv_w2048, state_conv,
              norm1_g, w_in, conv_w, w_out_a, w_out_b, w_o, norm2_g,
              peer_wq, peer_keys, peer_u, peer_v, final_g):
    slopes = _alibi_slopes()
    attend_prompt = functools.partial(_attn_prompt, slopes=slopes)
    yp, ys = x_prompt, x_sample
    kvp = ([], [], [])
    kvs = ([], [], [])
    convp, convs = [], []
    for l in range(DEPTH):
        w = (norm1_g[l], w_in[l], conv_w[l], w_out_a[l], w_out_b[l], w_o[l],
             norm2_g[l], peer_wq[l], peer_keys[l], peer_u[l], peer_v[l])
        prev0 = jnp.zeros((yp.shape[0], CONV_WIDTH - 1, CONV_DIM), yp.dtype)
        yp, kv_p, c_p = _layer(yp, prev0, attend_prompt, *w)
        bufs = (cache_kv_w128[l], cache_kv_w512[l], cache_kv_w2048[l])
        attend_sample = functools.partial(_attn_sample, bufs=bufs, slopes=slopes)
        ys, kv_s, c_s = _layer(ys, state_conv[l], attend_sample, *w)
        for g in range(N_GROUPS):
            kvp[g].append(kv_p[g])
            kvs[g].append(kv_s[g])
        convp.append(c_p)
        convs.append(c_s)
    y_prompt = _rmsnorm(yp, final_g)
    y_sample = _rmsnorm(ys, final_g)
    return (y_prompt, y_sample,
            jnp.stack(kvp[0], 0), jnp.stack(kvp[1], 0), jnp.stack(kvp[2], 0), jnp.stack(convp, 0),
            jnp.stack(kvs[0], 0), jnp.stack(kvs[1], 0), jnp.stack(kvs[2], 0), jnp.stack(convs, 0))
```

```python
import os
import numpy as np
from contextlib import ExitStack, contextmanager
import concourse.bass as bass
import concourse.mybir as mybir
from concourse.bass_utils import run_bass_kernel_spmd
import ml_dtypes

F32 = mybir.dt.float32
BF16 = mybir.dt.bfloat16
U32 = mybir.dt.uint32
ALU = mybir.AluOpType
AF = mybir.ActivationFunctionType
AX = mybir.AxisListType

NCORES = 8
D = 1024
SEQ = 2048
NS = 64
NT = SEQ + NS
PROJ = 9728
GROUPS = ((128, 1), (512, 4), (2048, 16))
EPS = 1e-6
NEXP = 16384
ENGS = ("pe", "act", "dve", "pool", "sp")
STAGE = int(os.environ.get("MK_STAGE", "99"))
B1CUT = int(os.environ.get("MK_B1CUT", "99"))
B1SUB = int(os.environ.get("MK_B1SUB", "99"))


class Sched:
    def __init__(self, nc, n_dma_sems=120):
        self.nc = nc
        self.streams = {e: [] for e in ENGS}
        self.count = {e: 0 for e in ENGS}
        self.last_w = {}
        self.readers = {}
        self.waited = {e: {} for e in ENGS}
        self.psem = {}
        self.dsems = {}
        self.dcount = {}
        self.n_dma_sems = n_dma_sems
        self.final_events = []
        self.pending = {e: [] for e in ENGS}

    def barrier(self):
        evs = [(e, self.count[e]) for e in ("pe", "act", "dve", "pool") if self.count[e] > 0]
        evs += [(("d", k), v) for k, v in self.dcount.items() if v > 0 and not str(k).startswith("cpy")]
        for eng in ENGS:
            for s_, v in evs:
                if s_ == eng:
                    continue
                if self.waited[eng].get(s_, 0) >= v:
                    continue
                self.waited[eng][s_] = v
                self.pending[eng].append((s_, v))

    def alloc(self, stack):
        for e in ("pe", "act", "dve", "pool"):
            self.psem[e] = stack.enter_context(self.nc.semaphore("p_" + e))
        self.stack = stack

    def dsem(self, key):
        if key not in self.dsems:
            assert len(self.dsems) < self.n_dma_sems, "too many dma sems"
            self.dsems[key] = self.stack.enter_context(self.nc.semaphore("d%d" % len(self.dsems)))
            self.dcount[key] = 0
        return self.dsems[key]

    def _deps(self, eng, reads, writes):
        ev = {}

        def add(e):
            if e is None:
                return
            s, v = e
            if ev.get(s, 0) < v:
                ev[s] = v

        for t in reads:
            add(self.last_w.get(t))
        for t in writes:
            add(self.last_w.get(t))
            for r in self.readers.get(t, ()):
                add(r)
        out = []
        for s, v in ev.items():
            if eng == "pe" and s == "pe":
                continue
            if self.waited[eng].get(s, 0) >= v:
                continue
            self.waited[eng][s] = v
            out.append((s, v))
        return out

    def _commit(self, event, reads, writes):
        for t in reads:
            self.readers.setdefault(t, []).append(event)
        for t in writes:
            self.last_w[t] = event
            self.readers[t] = []

    def op(self, eng, fn, reads=(), writes=()):
        waits = self.pending[eng] + self._deps(eng, reads, writes)
        self.pending[eng] = []
        self.count[eng] += 1
        event = (eng, self.count[eng])
        self.streams[eng].append((waits, fn, ("p", eng)))
        self._commit(event, reads, writes)
        return event

    def dma(self, fn, key, reads=(), writes=(), eng="sp", final=False):
        self.dsem(key)
        waits = self.pending[eng] + self._deps(eng, reads, writes)
        self.pending[eng] = []
        self.dcount[key] += 16
        event = (("d", key), self.dcount[key])
        self.streams[eng].append((waits, fn, ("d", key)))
        self._commit(event, reads, writes)
        if final:
            self.final_events.append(event)
        return event

    def _sem(self, s):
        if isinstance(s, tuple):
            return self.dsems[s[1]]
        return self.psem[s]

    def emit(self, block):
        S = self
        fin = {}
        for s, v in self.final_events:
            if fin.get(s, 0) < v:
                fin[s] = v

        def run(engname, engobj, extra_final=False):
            for waits, fn, inc in S.streams[engname]:
                for s, v in waits:
                    engobj.wait_ge(S._sem(s), v)
                ins = fn(engobj)
                if inc[0] == "p":
                    ins.then_inc(S.psem[inc[1]], 1)
                else:
                    ins.then_inc(S.dsems[inc[1]], 16)
            if extra_final:
                for s, v in fin.items():
                    engobj.wait_ge(S._sem(s), v)

        @block.sync
        def _(e):
            run("sp", e, True)

        @block.tensor
        def _(e):
            run("pe", e)

        @block.scalar
        def _(e):
            run("act", e)

        @block.vector
        def _(e):
            run("dve", e)

        @block.gpsimd
        def _(e):
            run("pool", e)


def _consts():
    slopes = np.exp2(-8.0 * np.arange(1, 9, dtype=np.float64) / 8.0)
    kj = np.arange(128)[:, None].astype(np.float64)
    qi = np.arange(128)[None, :].astype(np.float64)
    masks = np.zeros((128, 24, 2, 128), np.float32)
    for h in range(8):
        for g, (win, dil) in enumerate(GROUPS):
            dist_d = qi - kj
            md = np.where(dist_d >= 0, np.exp(-slopes[h] * dil * dist_d), 0.0)
            dist_p = 128 + qi - kj
            mp = np.where(dist_p <= 128, np.exp(-slopes[h] * dil * dist_p), 0.0)
            masks[:, h * 3 + g, 0, :] = mp
            masks[:, h * 3 + g, 1, :] = md
    sbias = np.zeros((128, 3, 129), np.float32)
    j = np.arange(129, dtype=np.float64)
    for p in range(128):
        s = p % 8
        for g, (win, dil) in enumerate(GROUPS):
            sbias[p, g, :] = -slopes[s] * dil * (128.0 - j)
    iota = np.tile(np.arange(128, dtype=np.float32)[None, :], (128, 1))
    return dict(
        c_identf=np.eye(128, dtype=np.float32),
        c_identb=np.eye(128, dtype=np.float32).astype(ml_dtypes.bfloat16),
        c_masks=masks.astype(ml_dtypes.bfloat16),
        c_sbias=sbias,
        c_iota=iota,
    )


def build_program():
    nc = bass.Bass("TRN2", target_bir_lowering=False)

    def din(name, shape, dt=F32):
        return nc.dram_tensor(name, list(shape), dt, kind="ExternalInput").ap()

    def dout(name, shape, dt=F32):
        return nc.dram_tensor(name, list(shape), dt, kind="ExternalOutput").ap()

    def dscr(name, shape, dt=F32):
        return nc.dram_tensor(name, list(shape), dt, kind="Internal").ap()

    xp = din("xp", [SEQ, D])
    xs = din("xs", [NS, D])
    caches = [din("cache%d" % g, [16, GROUPS[g][0], 2, 8, 64]) for g in range(3)]
    stconv = din("stconv", [32, D])
    g1T = din("g1T", [128, 8])
    g2T = din("g2T", [128, 8])
    w_in = din("w_in", [D, PROJ])
    convw = din("convw", [128, 8, 3])
    w_out_a = din("w_out_a", [D, D])
    w_out_b = din("w_out_b", [512, D])
    w_o = din("w_o", [D, D])
    wq = din("wq", [D, 2048])
    keysT = din("keysT", [128, 16, 128])
    uT = din("uT", [D, NEXP])
    vtab = din("vtab", [NEXP, D])
    fgb = din("fgb", [1, D])
    c_identf = din("c_identf", [128, 128])
    c_identb = din("c_identb", [128, 128], BF16)
    c_masks = din("c_masks", [128, 24, 2, 128], BF16)
    c_sbias = din("c_sbias", [128, 3, 129])
    c_iota = din("c_iota", [128, 128])

    y_p = dout("y_p", [SEQ, D])
    y_s = dout("y_s", [NS, D])
    kvp = [dout("kvp%d" % g, [min(GROUPS[g][0], SEQ), 2, 8, 64]) for g in range(3)]
    conv_p = dout("conv_p", [2, D])
    kvs = [dout("kvs%d" % g, [16, GROUPS[g][0], 2, 8, 64]) for g in range(3)]
    conv_s = dout("conv_s", [32, D])

    dbg_ot = None
    scr_q = dscr("scr_q", [NS, 1536])
    scr_o = dscr("scr_o", [NS, 512])
    scr_x1 = dscr("scr_x1", [NT, D])
    scr_ut = dscr("scr_ut", [128, 128, 8, 128], BF16)
    scr_v = dscr("scr_v", [128, 128, D], BF16)

    with ExitStack() as st:
        S = Sched(nc)
        S.alloc(st)

        @contextmanager
        def scope():
            with ExitStack() as es:
                yield es
                S.barrier()
        cnt = [0]

        def sb(shape, dt, stack=st):
            cnt[0] += 1
            return stack.enter_context(nc.sbuf_tensor("t%d" % cnt[0], list(shape), dt))

        pb = [st.enter_context(nc.psum_tensor("pb%d" % i, [128, 512], F32)) for i in range(8)]

        identf = sb([128, 128], F32)
        identb = sb([128, 128], BF16)
        iota = sb([128, 128], F32)
        g1t = sb([128, 8], F32)
        g2t = sb([128, 8], F32)
        cwt = sb([128, 8, 3], F32)
        epst = sb([128, 1], F32)
        for (t, src, nm) in ((identf, c_identf, "identf"), (identb, c_identb, "identb"), (iota, c_iota, "iota"),
                             (g1t, g1T, "g1t"), (g2t, g2T, "g2t"), (cwt, convw, "cwt")):
            S.dma(lambda e, t=t, src=src: e.dma_start(out=t[:], in_=src), "const", writes=[nm])
        S.op("dve", lambda e: e.memset(epst[:], EPS), writes=["eps"])

        for g in range(3):
            W = GROUPS[g][0]
            nb = 16 if W == 2048 else (4 if W == 512 else 1)
            per = 16 // nb
            for i in range(nb):
                src = caches[g][i * per:(i + 1) * per, 4:W].rearrange("b r k s d -> b (r k s d)")
                dst = kvs[g][i * per:(i + 1) * per, 0:W - 4].rearrange("b r k s d -> b (r k s d)")
                S.dma(lambda e, src=src, dst=dst: e.dma_start(out=dst, in_=src), "cpy%d" % g, eng="act", final=True)

        wst = sb([128, 8, 512], F32)
        wbf = [sb([128, 8, 512], BF16) for _ in range(2)]
        wctr = [0]

        def load_w(pieces, rows=128):
            slot = wctr[0] % 2
            wctr[0] += 1
            off = 0
            toks = []
            for i, (src, kcn) in enumerate(pieces):
                n = src.shape[-1]
                r = src.shape[0] // kcn
                srcv = src.rearrange("(kc p) c -> p kc c", p=r)
                tok = "wst.%d" % i
                S.dma(lambda e, srcv=srcv, off=off, n=n, r=r, kcn=kcn: e.dma_start(out=wst[0:r, 0:kcn, off:off + n], in_=srcv),
                      "wst", writes=[tok])
                toks.append(tok)
                off += n
            S.op("pool", lambda e, slot=slot, off=off: e.tensor_copy(out=wbf[slot][:, :, 0:off], in_=wst[:, :, 0:off]),
                 reads=toks, writes=["wbf%d" % slot] + ["wstall"])
            for tok in ["wst.%d" % i for i in range(8)]:
                S.readers.setdefault(tok, []).append(S.last_w["wbf%d" % slot])
            return wbf[slot], "wbf%d" % slot

        tiles = [(i * 128, 128) for i in range(16)] + [(SEQ, NS)]
        tgroups = [(i * 512, 512) for i in range(4)] + [(SEQ, NS)]

        def x_src(t0, n):
            return xp[t0:t0 + n, :] if t0 < SEQ else xs[:, :]

        def rmsnorm_T(src_fn, gt, gname, dstT, dst_tok, xts, hb, junk, ss, stack_tag):
            for i, (t0, n) in enumerate(tiles):
                k = i % 2
                xt = xts[k]
                S.dma(lambda e, xt=xt, t0=t0, n=n: e.dma_start(out=xt[0:n, :], in_=src_fn(t0, n)), stack_tag + "x%d" % k,
                      reads=["x1dram.%d" % i] if stack_tag == "n2" else [], writes=["xt%d" % k])
                S.op("dve", lambda e: e.memset(ss[:, 0:1], 0.0), writes=["ss"])
                S.op("act", lambda e, xt=xt, n=n: e.activation(out=junk[0:n, :], in_=xt[0:n, :], func=AF.Square, accum_out=ss[0:n, 0:1]),
                     reads=["xt%d" % k, "ss"], writes=["junk", "ss"])
                S.op("act", lambda e, n=n: e.activation(out=ss[0:n, 1:2], in_=ss[0:n, 0:1], func=AF.Sqrt, bias=epst[0:n, :], scale=1.0 / D),
                     reads=["ss", "eps"], writes=["ss1"])
                S.op("dve", lambda e, n=n: e.reciprocal(out=ss[0:n, 2:3], in_=ss[0:n, 1:2]), reads=["ss1"], writes=["ss2"])
                S.op("dve", lambda e, xt=xt, n=n: e.tensor_scalar(out=hb[0:n, :], in0=xt[0:n, :], scalar1=ss[0:n, 2:3], scalar2=None, op0=ALU.mult),
                     reads=["xt%d" % k, "ss2"], writes=["hb"])
                pt = pb[k][:].bitcast(BF16)
                for kc in range(8):
                    S.op("pe", lambda e, pt=pt, kc=kc, n=n: e.transpose(out=pt[:, kc * 128:kc * 128 + n], in_=hb[0:n, kc * 128:(kc + 1) * 128], identity=identb[0:n, 0:n]),
                         reads=["hb", "identb"], writes=["pb%d" % k])
                S.op("dve", lambda e, pt=pt, t0=t0, n=n: e.tensor_tensor(
                    out=dstT[:, :, t0:t0 + n], in0=pt.rearrange("p (k t) -> p k t", k=8)[:, :, 0:n],
                    in1=gt[:, :].unsqueeze(2).broadcast_to([128, 8, n]), op=ALU.mult),
                    reads=["pb%d" % k, gname], writes=[dst_tok + ".%d" % i] + (["hT.%d" % i] if dst_tok != "hT" else []))

        hT = sb([128, 8, NT], BF16)
        hT_all = ["hT.%d" % i for i in range(17)]

        with scope() as pa:
            uf, vf, ub, vb = [], [], [], []
            prep_c = [0]

            def prep_chunk():
                c = prep_c[0]
                if c >= 128 or STAGE < 6:
                    return
                prep_c[0] += 1
                k = 0
                S.dma(lambda e, c=c, k=k: e.dma_start(out=uf[k][:], in_=uT[:, c * 128:(c + 1) * 128].rearrange("(kc p) e -> p kc e", p=128)),
                      "uf%d" % k, writes=["uf%d" % k])
                S.dma(lambda e, c=c, k=k: e.dma_start(out=vf[k][:], in_=vtab[c * 128:(c + 1) * 128, :]),
                      "vf%d" % k, writes=["vf%d" % k])
                S.op("act", lambda e, k=k: e.activation(out=ub[k][:], in_=uf[k][:], func=AF.Copy), reads=["uf%d" % k], writes=["ub%d" % k])
                S.op("pool", lambda e, k=k: e.tensor_copy(out=vb[k][:], in_=vf[k][:]), reads=["vf%d" % k], writes=["vb%d" % k])
                S.dma(lambda e, c=c, k=k: e.dma_start(out=scr_ut[c], in_=ub[k][:]), "ubs%d" % k, reads=["ub%d" % k], writes=["scr_ut%d" % c])
                S.dma(lambda e, c=c, k=k: e.dma_start(out=scr_v[c], in_=vb[k][:]), "vbs%d" % k, reads=["vb%d" % k], writes=["scr_v%d" % c])

            with scope() as pa1:
                OT = sb([64, 8, NT], BF16, pa1)
                with scope() as s1:
                    xts = [sb([128, D], F32, s1) for _ in range(2)]
                    hb = sb([128, D], BF16, s1)
                    junk = sb([128, D], BF16, s1)
                    ss = sb([128, 4], F32, s1)
                    rmsnorm_T(x_src, g1t, "g1t", hT, "hT", xts, hb, junk, ss, "n1")

                if STAGE >= 2:
                    with scope() as s2:
                        skv = sb([NS, 512], F32, s2)
                        pst = [sb([128, 512], F32, s2) for _ in range(2)]
                        pctr = 0
                        for typ in range(3):
                            for g in range(3):
                                W = GROUPS[g][0]
                                c0 = 3072 + typ * 1536 + g * 512
                                wt, wtok = load_w([(w_in[:, c0:c0 + 512], 8)])
                                for kc in range(8):
                                    S.op("pe", lambda e, wt=wt, kc=kc: e.matmul(pb[2][0:NS, :], lhsT=hT[:, kc, SEQ:NT], rhs=wt[:, kc, :], start=(kc == 0), stop=(kc == 7)),
                                         reads=[wtok, "hT.16"], writes=["pb2"])
                                S.op("act", lambda e: e.activation(out=skv[:], in_=pb[2][0:NS, :], func=AF.Copy), reads=["pb2"], writes=["skv"])
                                for b in range(16):
                                    if typ == 0:
                                        S.dma(lambda e, b=b, g=g: e.dma_start(out=scr_q[b * 4:(b + 1) * 4, g * 512:(g + 1) * 512], in_=skv[b * 4:(b + 1) * 4, :]),
                                              "skvst", reads=["skv"], writes=["scr_q.%d.%d" % (g, b)])
                                    else:
                                        S.dma(lambda e, b=b, g=g, W=W, typ=typ: e.dma_start(
                                            out=kvs[g][b, W - 4:W, typ - 1].rearrange("t s d -> t (s d)"), in_=skv[b * 4:(b + 1) * 4, :]),
                                            "skvst", reads=["skv"], writes=["kvsnew.%d.%d.%d" % (g, typ, b)], final=True)
                                if typ == 0:
                                    continue
                                keep = min(W, SEQ) // 128
                                for ti in range(16 - keep, 16):
                                    k = pctr % 2
                                    pctr += 1
                                    for kc in range(8):
                                        S.op("pe", lambda e, wt=wt, kc=kc, ti=ti, k=k: e.matmul(pb[3 + k][:, :], lhsT=hT[:, kc, ti * 128:(ti + 1) * 128], rhs=wt[:, kc, :], start=(kc == 0), stop=(kc == 7)),
                                             reads=[wtok, "hT.%d" % ti], writes=["pb%d" % (3 + k)])
                                    S.op("act", lambda e, k=k: e.activation(out=pst[k][:], in_=pb[3 + k][:], func=AF.Copy), reads=["pb%d" % (3 + k)], writes=["pst%d" % k])
                                    r0 = (ti - (16 - keep)) * 128
                                    S.dma(lambda e, g=g, r0=r0, typ=typ, k=k: e.dma_start(out=kvp[g][r0:r0 + 128, typ - 1].rearrange("t s d -> t (s d)"), in_=pst[k][:]),
                                          "pst%d" % k, reads=["pst%d" % k], final=True)

                if STAGE >= 3:
                    with scope() as s3:
                        Kt = sb([128, 132, 64], F32, s3)
                        Vt = sb([128, 132, 64], F32, s3)
                        prod = sb([128, 43, 64], F32, s3)
                        qs = sb([128, 4, 3, 64], F32, s3)
                        knew = sb([128, 3, 4, 64], F32, s3)
                        vnew = sb([128, 3, 4, 64], F32, s3)
                        sbias = sb([128, 3, 129], F32, s3)
                        sc = sb([128, 129], F32, s3)
                        ex = sb([128, 129], F32, s3)
                        lacc = sb([128, 12], F32, s3)
                        og = sb([128, 12, 64], F32, s3)
                        osum = sb([128, 4, 64], F32, s3)
                        lsum = sb([128, 4], F32, s3)
                        otok = sb([NS, 512], F32, s3)
                        S.dma(lambda e: e.dma_start(out=sbias[:], in_=c_sbias), "const", writes=["sbias"])
                        S.op("dve", lambda e: e.memset(lacc[:], 0.0), writes=["lacc.%d" % c for c in range(12)])
                        for b in range(16):
                            S.dma(lambda e, b=b: e.dma_start(out=qs[b * 8:(b + 1) * 8], in_=scr_q[b * 4:(b + 1) * 4, :].rearrange("t (g s d) -> s t g d", g=3, s=8)),
                                  "qs", reads=["scr_q.%d.%d" % (g, b) for g in range(3)], writes=["qs.%d" % b])
                            for g in range(3):
                                W = GROUPS[g][0]
                                S.dma(lambda e, b=b, g=g, W=W: e.dma_start(out=knew[b * 8:(b + 1) * 8, g], in_=kvs[g][b, W - 4:W, 0].rearrange("t s d -> s t d")),
                                      "qs", reads=["kvsnew.%d.1.%d" % (g, b)], writes=["knew.%d.%d" % (g, b)])
                                S.dma(lambda e, b=b, g=g, W=W: e.dma_start(out=vnew[b * 8:(b + 1) * 8, g], in_=kvs[g][b, W - 4:W, 1].rearrange("t s d -> s t d")),
                                      "qs", reads=["kvsnew.%d.2.%d" % (g, b)], writes=["vnew.%d.%d" % (g, b)])
                        qs_all = ["qs.%d" % b for b in range(16)]
                        kn_all = ["knew.%d.%d" % (g, b) for g in range(3) for b in range(16)]
                        vn_all = ["vnew.%d.%d" % (g, b) for g in range(3) for b in range(16)]
                        for g in range(3):
                            W, dil = GROUPS[g]
                            for t in range(4):
                                col = g * 4 + t
                                if g == 0 and t > 0:
                                    pass
                                else:
                                    for b in range(16):
                                        if g == 0:
                                            ksrc = caches[0][b, :, 0].rearrange("r s d -> s r d")
                                            vsrc = caches[0][b, :, 1].rearrange("r s d -> s r d")
                                        else:
                                            ksrc = caches[g][b, :, 0].rearrange("(j r) s d -> s r j d", r=dil)[:, t, 0:128, :]
                                            vsrc = caches[g][b, :, 1].rearrange("(j r) s d -> s r j d", r=dil)[:, t, 0:128, :]
                                        S.dma(lambda e, b=b, ksrc=ksrc: e.dma_start(out=Kt[b * 8:(b + 1) * 8, 0:128, :], in_=ksrc), "Kt", writes=["Kt.%d" % b])
                                        S.dma(lambda e, b=b, vsrc=vsrc: e.dma_start(out=Vt[b * 8:(b + 1) * 8, 0:128, :], in_=vsrc), "Vt", writes=["Vt.%d" % b])
                                    if g == 0:
                                        S.op("pool", lambda e: e.tensor_copy(out=Kt[:, 128:132, :], in_=knew[:, 0]), reads=kn_all, writes=["Ktn"])
                                        S.op("pool", lambda e: e.tensor_copy(out=Vt[:, 128:132, :], in_=vnew[:, 0]), reads=vn_all, writes=["Vtn"])
                                    else:
                                        S.op("pool", lambda e, g=g, t=t: e.tensor_copy(out=Kt[:, 128:129, :], in_=knew[:, g, t:t + 1, :]), reads=kn_all, writes=["Ktn"])
                                        S.op("pool", lambda e, g=g, t=t: e.tensor_copy(out=Vt[:, 128:129, :], in_=vnew[:, g, t:t + 1, :]), reads=vn_all, writes=["Vtn"])
                                w0 = t if g == 0 else 0
                                kt_all = ["Kt.%d" % b for b in range(16)] + ["Ktn"]
                                vt_all = ["Vt.%d" % b for b in range(16)] + ["Vtn"]
                                for ch in range(3):
                                    k0 = ch * 43
                                    S.op("dve", lambda e, w0=w0, k0=k0, t=t, g=g: e.tensor_tensor(
                                        out=prod[:], in0=Kt[:, w0 + k0:w0 + k0 + 43, :],
                                        in1=qs[:, t, g, :].unsqueeze(1).broadcast_to([128, 43, 64]), op=ALU.mult),
                                        reads=kt_all + qs_all, writes=["prod"])
                                    S.op("dve", lambda e, k0=k0: e.tensor_reduce(out=sc[:, k0:k0 + 43], in_=prod[:], axis=AX.X, op=ALU.add),
                                         reads=["prod"], writes=["sc.%d" % ch])
                                S.op("dve", lambda e, g=g: e.scalar_tensor_tensor(out=ex[:], in0=sc[:], scalar=0.125, in1=sbias[:, g, :], op0=ALU.mult, op1=ALU.add),
                                     reads=["sc.0", "sc.1", "sc.2", "sbias"], writes=["ex"])
                                S.op("act", lambda e, col=col: e.activation(out=sc[:], in_=ex[:], func=AF.Exp, accum_out=lacc[:, col:col + 1]),
                                     reads=["ex", "lacc.%d" % col], writes=["sc.0", "sc.1", "sc.2", "lacc.%d" % col])
                                for ch in range(3):
                                    k0 = ch * 43
                                    S.op("dve", lambda e, w0=w0, k0=k0: e.tensor_tensor(
                                        out=prod[:], in0=Vt[:, w0 + k0:w0 + k0 + 43, :],
                                        in1=sc[:, k0:k0 + 43].unsqueeze(2).broadcast_to([128, 43, 64]), op=ALU.mult),
                                        reads=vt_all + ["sc.0", "sc.1", "sc.2"], writes=["prod"])
                                    dst = og[:, col, :] if ch == 0 else ex[:, ch * 64 - 64:ch * 64]
                                    S.op("dve", lambda e, dst=dst: e.tensor_reduce(out=dst, in_=prod[:].rearrange("p k d -> p d k"), axis=AX.X, op=ALU.add),
                                         reads=["prod"], writes=["ogp.0"] if ch == 0 else ["ex"])
                                S.op("dve", lambda e, col=col: e.tensor_tensor(out=og[:, col, :], in0=og[:, col, :], in1=ex[:, 0:64], op=ALU.add),
                                     reads=["ogp.0", "ex"], writes=["ogp.0"])
                                S.op("dve", lambda e, col=col: e.tensor_tensor(out=og[:, col, :], in0=og[:, col, :], in1=ex[:, 64:128], op=ALU.add),
                                     reads=["ogp.0", "ex"], writes=["ogp.0", "og.%d" % col])
                        og_all = ["og.%d" % c for c in range(12)]
                        la_all = ["lacc.%d" % c for c in range(12)]
                        S.op("dve", lambda e: e.tensor_tensor(out=osum[:], in0=og[:, 0:4, :], in1=og[:, 4:8, :], op=ALU.add), reads=og_all, writes=["osum"])
                        S.op("dve", lambda e: e.tensor_tensor(out=osum[:], in0=osum[:], in1=og[:, 8:12, :], op=ALU.add), reads=og_all + ["osum"], writes=["osum"])
                        S.op("dve", lambda e: e.tensor_tensor(out=lsum[:], in0=lacc[:, 0:4], in1=lacc[:, 4:8], op=ALU.add), reads=la_all, writes=["lsum"])
                        S.op("dve", lambda e: e.tensor_tensor(out=lsum[:], in0=lsum[:], in1=lacc[:, 8:12], op=ALU.add), reads=la_all + ["lsum"], writes=["lsum"])
                        S.op("dve", lambda e: e.reciprocal(out=lsum[:], in_=lsum[:]), reads=["lsum"], writes=["lsum"])
                        S.op("dve", lambda e: e.tensor_tensor(out=osum[:], in0=osum[:], in1=lsum[:, :].unsqueeze(2).broadcast_to([128, 4, 64]), op=ALU.mult),
                             reads=["osum", "lsum"], writes=["osum"])
                        for b in range(16):
                            S.dma(lambda e, b=b: e.dma_start(out=scr_o[b * 4:(b + 1) * 4, :].rearrange("t (s d) -> s t d", s=8), in_=osum[b * 8:(b + 1) * 8]),
                                  "scro", reads=["osum"], writes=["scr_o.%d" % b])
                        S.dma(lambda e: e.dma_start(out=otok[:], in_=scr_o), "otok", reads=["scr_o.%d" % b for b in range(16)], writes=["otok"])
                        for h in range(8):
                            S.op("pe", lambda e, h=h: e.transpose(out=pb[2][0:64, h * 64:(h + 1) * 64], in_=otok[:, h * 64:(h + 1) * 64], identity=identf[0:NS, 0:NS]),
                                 reads=["otok", "identf"], writes=["pb2"])
                        S.op("act", lambda e: e.activation(out=OT[:, :, SEQ:NT], in_=pb[2][0:64, :].rearrange("p (h t) -> p h t", h=8), func=AF.Copy),
                             reads=["pb2"], writes=["OTs"])

                uf.extend([sb([128, 8, 128], F32, pa1)] * 2)
                vf.extend([sb([128, D], F32, pa1)] * 2)
                ub.extend([sb([128, 8, 128], BF16, pa1)] * 2)
                vb.extend([sb([128, D], BF16, pa1)] * 2)
                if STAGE >= 4:
                    with scope() as s4:
                        masks = sb([128, 24, 2, 128], BF16, s4)
                        QT = sb([64, 3, SEQ], BF16, s4)
                        KT = sb([64, 3, SEQ], BF16, s4)
                        Vh = sb([128, 3, 16, 128], BF16, s4)
                        acc = sb([128, SEQ], F32, s4)
                        rc = sb([64, 512], F32, s4)
                        ebuf = [sb([128, 256], F32, s4) for _ in range(2)]
                        pT = [sb([128, 256], BF16, s4) for _ in range(2)]
                        S.dma(lambda e: e.dma_start(out=masks[:], in_=c_masks), "const", writes=["masks"])
                        S.op("pool", lambda e: e.memset(Vh[:], 1.0), writes=["Vh"])

                        def permv(ap2048, dil):
                            return ap2048.rearrange("p (j r) -> p r j", r=dil)

                        uctr = 0
                        for h in range(8):
                            pieces = []
                            for typ in range(3):
                                for g in range(3):
                                    c0 = 3072 + typ * 1536 + g * 512 + h * 64
                                    pieces.append((w_in[:, c0:c0 + 64], 8))
                            wt, wtok = load_w(pieces[0:6])
                            wv, wvtok = load_w(pieces[6:9])
                            for typ, dstT, dname in ((0, QT, "QT"), (1, KT, "KT")):
                                for g in range(3):
                                    dil = GROUPS[g][1]
                                    wcol = (typ * 3 + g) * 64
                                    for tg in range(4):
                                        bk = 2 + (tg % 2)
                                        for kc in range(8):
                                            if dil == 1:
                                                rhs = hT[:, kc, tg * 512:(tg + 1) * 512]
                                            elif dil == 4:
                                                rhs = permv(hT[:, kc, 0:SEQ], 4)[:, tg, :]
                                            else:
                                                rhs = permv(hT[:, kc, 0:SEQ], 16)[:, tg * 4:(tg + 1) * 4, :]
                                            S.op("pe", lambda e, bk=bk, wt=wt, kc=kc, wcol=wcol, rhs=rhs: e.matmul(
                                                pb[bk][0:64, :], lhsT=wt[:, kc, wcol:wcol + 64], rhs=rhs, start=(kc == 0), stop=(kc == 7)),
                                                reads=[wtok] + hT_all[0:16], writes=["pb%d" % bk])
                                        S.op("act", lambda e, bk=bk, dstT=dstT, g=g, tg=tg: e.activation(out=dstT[:, g, tg * 512:(tg + 1) * 512], in_=pb[bk][0:64, :], func=AF.Copy),
                                             reads=["pb%d" % bk], writes=["%s.%d.%d" % (dname, g, tg)])
                            for g in range(3):
                                dil = GROUPS[g][1]
                                L = SEQ // dil
                                for half in range(2):
                                    bk = 4 + half
                                    for nb in range(8):
                                        n = half * 8 + nb
                                        r, lb = (n * 128) // L, ((n * 128) % L) // 128
                                        for kc in range(8):
                                            lhsT = permv(hT[:, kc, 0:SEQ], dil)[:, r, lb * 128:(lb + 1) * 128]
                                            S.op("pe", lambda e, bk=bk, nb=nb, lhsT=lhsT, kc=kc, g=g, wv=wv: e.matmul(
                                                pb[bk][:, nb * 64:(nb + 1) * 64], lhsT=lhsT, rhs=wv[:, kc, g * 64:(g + 1) * 64], start=(kc == 0), stop=(kc == 7)),
                                                reads=[wvtok] + hT_all[0:16], writes=["pb%d" % bk])
                                    S.op("act", lambda e, bk=bk, g=g, half=half: e.activation(
                                        out=Vh[:, g, half * 8:(half + 1) * 8, 0:64], in_=pb[bk][:, :].rearrange("p (n d) -> p n d", n=8), func=AF.Copy),
                                        reads=["pb%d" % bk, "Vh"], writes=["Vh.%d.%d" % (g, half)])
                            for g in range(3):
                                dil = GROUPS[g][1]
                                L = SEQ // dil
                                bpl = L // 128
                                for n in range(16):
                                    r, lb = n // bpl, n % bpl
                                    slots = ([0] if lb > 0 else []) + [1]
                                    u = uctr % 2
                                    uctr += 1
                                    if uctr % 3 == 0:
                                        prep_chunk()
                                    sbk, obk = 6, 7
                                    c_lo = slots[0] * 128
                                    for sl in slots:
                                        kb = n - 1 if sl == 0 else n
                                        S.op("pe", lambda e, sbk=sbk, sl=sl, kb=kb, n=n, g=g, u=u: e.matmul(
                                            pb[sbk][:, (2 * u + sl) * 128:(2 * u + sl + 1) * 128], lhsT=KT[:, g, kb * 128:(kb + 1) * 128], rhs=QT[:, g, n * 128:(n + 1) * 128],
                                            start=True, stop=True),
                                            reads=["KT.%d.%d" % (g, kb // 4), "QT.%d.%d" % (g, n // 4)], writes=["sps%d" % u])
                                    S.op("act", lambda e, u=u, c_lo=c_lo: e.activation(out=ebuf[u][:, c_lo:256], in_=pb[6][:, 2 * u * 128 + c_lo:(2 * u + 2) * 128], func=AF.Exp, scale=0.125),
                                         reads=["sps%d" % u], writes=["ebuf%d" % u])
                                    S.op("dve", lambda e, u=u, c_lo=c_lo, h=h, g=g: e.tensor_tensor(
                                        out=pT[u][:, c_lo:256], in0=ebuf[u][:, c_lo:256],
                                        in1=masks[:, h * 3 + g].rearrange("p s q -> p (s q)")[:, c_lo:256], op=ALU.mult),
                                        reads=["ebuf%d" % u, "masks"], writes=["pT%d" % u])
                                    for i, sl in enumerate(slots):
                                        kb = n - 1 if sl == 0 else n
                                        S.op("pe", lambda e, u=u, sl=sl, kb=kb, g=g, i=i, last=(i == len(slots) - 1): e.matmul(
                                            pb[7][:, u * 128:(u + 1) * 128], lhsT=Vh[:, g, kb, :], rhs=pT[u][:, sl * 128:(sl + 1) * 128], start=(i == 0), stop=last),
                                            reads=["pT%d" % u, "Vh.%d.%d" % (g, kb // 8), "Vh"], writes=["ops%d" % u])
                                    av = permv(acc[:, :], dil)[:, r, lb * 128:(lb + 1) * 128]
                                    if g == 0:
                                        S.op("dve", lambda e, av=av, u=u: e.tensor_copy(out=av, in_=pb[7][:, u * 128:(u + 1) * 128]), reads=["ops%d" % u], writes=["acc"])
                                    else:
                                        S.op("dve", lambda e, av=av, u=u: e.tensor_tensor(out=av, in0=av, in1=pb[7][:, u * 128:(u + 1) * 128], op=ALU.add),
                                             reads=["ops%d" % u, "acc"], writes=["acc"])
                            for tg in range(4):
                                S.op("dve", lambda e, tg=tg: e.reciprocal(out=rc[:, :], in_=acc[64:128, tg * 512:(tg + 1) * 512]), reads=["acc"], writes=["rc"])
                                S.op("dve", lambda e, tg=tg, h=h: e.tensor_tensor(out=OT[:, h, tg * 512:(tg + 1) * 512], in0=acc[0:64, tg * 512:(tg + 1) * 512], in1=rc[:, :], op=ALU.mult),
                                     reads=["acc", "rc"], writes=["OT.%d" % h])

                while prep_c[0] < 128 and STAGE >= 6:
                    prep_chunk()
                OT_all = ["OT.%d" % h for h in range(8)] + ["OTs"]
                if dbg_ot is not None:
                    S.dma(lambda e: e.dma_start(out=dbg_ot, in_=OT[:]), "dbgot", reads=OT_all, final=True)
                with scope() as pa2:
                    byT = sb([128, 8, NT], BF16, pa2)
                    zT = sb([128, 8, NT], BF16, pa2)
                    if STAGE >= 5:
                        with scope() as s5:
                            extp = sb([128, SEQ + 2], F32, s5)
                            exts = sb([128, 16, 6], F32, s5)
                            stt = sb([32, D], F32, s5)
                            hv = sb([128, 512], F32, s5)
                            yb = sb([128, 512], F32, s5)
                            cvT = sb([128, 8, 34], F32, s5)
                            cvtok = sb([34, D], F32, s5)
                            S.dma(lambda e: e.dma_start(out=stt[:], in_=stconv), "const", writes=["stt"])
                            S.op("pool", lambda e: e.memset(extp[:, 0:2], 0.0), writes=["extp"])
                            for cc in range(8):
                                wt, wtok = load_w([(w_in[:, j * 1024 + cc * 128:j * 1024 + (cc + 1) * 128], 8) for j in range(3)])
                                S.op("pe", lambda e, cc=cc: e.transpose(out=pb[5][:, 0:32], in_=stt[:, cc * 128:(cc + 1) * 128], identity=identf[0:32, 0:32]),
                                     reads=["stt", "identf"], writes=["pb5"])
                                S.op("act", lambda e: e.activation(out=exts[:, :, 0:2], in_=pb[5][:, 0:32].rearrange("p (b r) -> p b r", r=2), func=AF.Copy),
                                     reads=["pb5"], writes=["exts"])
                                for gi, (t0, n) in enumerate(tgroups):
                                    for j in range(3):
                                        for kc in range(8):
                                            S.op("pe", lambda e, j=j, kc=kc, t0=t0, n=n, wt=wt: e.matmul(
                                                pb[2 + j][:, 0:n], lhsT=wt[:, kc, j * 128:(j + 1) * 128], rhs=hT[:, kc, t0:t0 + n], start=(kc == 0), stop=(kc == 7)),
                                                reads=[wtok] + hT_all, writes=["pb%d" % (2 + j)])
                                    S.op("act", lambda e, n=n: e.activation(out=hv[:, 0:n], in_=pb[4][:, 0:n], func=AF.Copy), reads=["pb4"], writes=["hv"])
                                    if t0 < SEQ:
                                        uo = extp[:, 2 + t0:2 + t0 + n]
                                        e0, e1, e2 = extp[:, t0:t0 + n], extp[:, t0 + 1:t0 + 1 + n], extp[:, t0 + 2:t0 + 2 + n]
                                        hvv, cps, bps, yv, byv = hv[:, 0:n], pb[3][:, 0:n], pb[2][:, 0:n], yb[:, 0:n], byT[:, cc, t0:t0 + n]
                                        etok = "extp"
                                    else:
                                        uo = exts[:, :, 2:6]
                                        e0, e1, e2 = exts[:, :, 0:4], exts[:, :, 1:5], exts[:, :, 2:6]
                                        v4 = lambda ap: ap.rearrange("p (b t) -> p b t", t=4)
                                        hvv, cps, bps, yv, byv = v4(hv[:, 0:n]), v4(pb[3][:, 0:n]), v4(pb[2][:, 0:n]), v4(yb[:, 0:n]), v4(byT[:, cc, t0:t0 + n])
                                        etok = "exts"
                                    S.op("dve", lambda e, uo=uo, cps=cps, hvv=hvv: e.tensor_tensor(out=uo, in0=cps, in1=hvv, op=ALU.mult),
                                         reads=["pb3", "hv", etok], writes=[etok])
                                    S.op("dve", lambda e, yv=yv, e0=e0, cc=cc: e.tensor_scalar(out=yv, in0=e0, scalar1=cwt[:, cc, 0:1], scalar2=None, op0=ALU.mult),
                                         reads=[etok, "cwt"], writes=["yb"])
                                    S.op("dve", lambda e, yv=yv, e1=e1, cc=cc: e.scalar_tensor_tensor(out=yv, in0=e1, scalar=cwt[:, cc, 1:2], in1=yv, op0=ALU.mult, op1=ALU.add),
                                         reads=[etok, "cwt", "yb"], writes=["yb"])
                                    S.op("dve", lambda e, yv=yv, e2=e2, cc=cc: e.scalar_tensor_tensor(out=yv, in0=e2, scalar=cwt[:, cc, 2:3], in1=yv, op0=ALU.mult, op1=ALU.add),
                                         reads=[etok, "cwt", "yb"], writes=["yb"])
                                    S.op("dve", lambda e, byv=byv, bps=bps, yv=yv: e.tensor_tensor(out=byv, in0=bps, in1=yv, op=ALU.mult),
                                         reads=["pb2", "yb"], writes=["byT.%d.%d" % (cc, gi)])
                                S.op("act", lambda e, cc=cc: e.activation(out=cvT[:, cc, 0:2], in_=extp[:, SEQ:SEQ + 2], func=AF.Copy), reads=["extp"], writes=["cvT.%d" % cc])
                                S.op("act", lambda e, cc=cc: e.activation(out=cvT[:, cc, 2:34].rearrange("p (b r) -> p b r", r=2), in_=exts[:, :, 4:6], func=AF.Copy),
                                     reads=["exts", "cvT.%d" % cc], writes=["cvT.%d" % cc])
                            for cc in range(8):
                                S.op("pe", lambda e, cc=cc: e.transpose(out=pb[5][0:34, 0:128], in_=cvT[:, cc, :], identity=identf[:, :]),
                                     reads=["cvT.%d" % cc, "identf"], writes=["pb5"])
                                S.op("act", lambda e, cc=cc: e.activation(out=cvtok[:, cc * 128:(cc + 1) * 128], in_=pb[5][0:34, 0:128], func=AF.Copy),
                                     reads=["pb5"], writes=["cvtok.%d" % cc])
                            cv_all = ["cvtok.%d" % cc for cc in range(8)]
                            S.dma(lambda e: e.dma_start(out=conv_p, in_=cvtok[0:2, :]), "cvst", reads=cv_all, final=True)
                            S.dma(lambda e: e.dma_start(out=conv_s, in_=cvtok[2:34, :]), "cvst", reads=cv_all, final=True)

                    if STAGE >= 5:
                        with scope() as s6:
                            sga = sb([128, 512], F32, s6)
                            sgb = sb([128, 512], F32, s6)
                            by_all = ["byT.%d.%d" % (cc, gi) for cc in range(8) for gi in range(5)]
                            for cc in range(8):
                                wt, wtok = load_w([(w_out_a[:, cc * 128:(cc + 1) * 128], 8),
                                                   (w_in[:, 7680 + cc * 128:7680 + (cc + 1) * 128], 8),
                                                   (w_in[:, 8704 + cc * 128:8704 + (cc + 1) * 128], 8),
                                                   (w_out_b[:, cc * 128:(cc + 1) * 128], 8)])
                                for gi, (t0, n) in enumerate(tgroups):
                                    for kc in range(8):
                                        S.op("pe", lambda e, kc=kc, t0=t0, n=n, wt=wt: e.matmul(pb[2][:, 0:n], lhsT=wt[:, kc, 0:128], rhs=byT[:, kc, t0:t0 + n], start=(kc == 0), stop=(kc == 7)),
                                             reads=[wtok] + by_all, writes=["pb2"])
                                    for kc in range(8):
                                        S.op("pe", lambda e, kc=kc, t0=t0, n=n, wt=wt: e.matmul(pb[3][:, 0:n], lhsT=wt[0:64, kc, 384:512], rhs=OT[:, kc, t0:t0 + n], start=(kc == 0), stop=(kc == 7)),
                                             reads=[wtok] + OT_all, writes=["pb3"])
                                    for j in range(2):
                                        for kc in range(8):
                                            S.op("pe", lambda e, j=j, kc=kc, t0=t0, n=n, wt=wt: e.matmul(pb[4 + j][:, 0:n], lhsT=wt[:, kc, 128 + j * 128:256 + j * 128], rhs=hT[:, kc, t0:t0 + n], start=(kc == 0), stop=(kc == 7)),
                                                 reads=[wtok] + hT_all, writes=["pb%d" % (4 + j)])
                                    S.op("act", lambda e, n=n: e.activation(out=sga[:, 0:n], in_=pb[4][:, 0:n], func=AF.Sigmoid), reads=["pb4"], writes=["sga"])
                                    S.op("act", lambda e, n=n: e.activation(out=sgb[:, 0:n], in_=pb[5][:, 0:n], func=AF.Sigmoid), reads=["pb5"], writes=["sgb"])
                                    S.op("dve", lambda e, n=n: e.tensor_tensor(out=sga[:, 0:n], in0=sga[:, 0:n], in1=pb[2][:, 0:n], op=ALU.mult), reads=["sga", "pb2"], writes=["sga"])
                                    S.op("dve", lambda e, n=n: e.tensor_tensor(out=sgb[:, 0:n], in0=sgb[:, 0:n], in1=pb[3][:, 0:n], op=ALU.mult), reads=["sgb", "pb3"], writes=["sgb"])
                                    S.op("dve", lambda e, n=n, cc=cc, t0=t0: e.tensor_tensor(out=zT[:, cc, t0:t0 + n], in0=sga[:, 0:n], in1=sgb[:, 0:n], op=ALU.add),
                                         reads=["sga", "sgb"], writes=["zT.%d.%d" % (cc, gi)])

                    if STAGE >= 5:
                        with scope() as s7:
                            wo = sb([128, 8, D], BF16, s7)
                            x1t = [sb([128, D], F32, s7) for _ in range(2)]
                            z_all = ["zT.%d.%d" % (cc, gi) for cc in range(8) for gi in range(5)]
                            for half in range(2):
                                wt, wtok = load_w([(w_o[:, half * 512:(half + 1) * 512], 8)])
                                S.op("pool", lambda e, wt=wt, half=half: e.tensor_copy(out=wo[:, :, half * 512:(half + 1) * 512], in_=wt[:, :, :]), reads=[wtok], writes=["wo.%d" % half])
                            for i, (t0, n) in enumerate(tiles):
                                k = i % 2
                                S.dma(lambda e, k=k, t0=t0, n=n: e.dma_start(out=x1t[k][0:n, :], in_=x_src(t0, n)), "x1ld%d" % k, writes=["x1t%d" % k])
                                for half in range(2):
                                    for kc in range(8):
                                        S.op("pe", lambda e, half=half, kc=kc, t0=t0, n=n, k=k: e.matmul(pb[2 + 2 * k + half][0:n, :], lhsT=zT[:, kc, t0:t0 + n], rhs=wo[:, kc, half * 512:(half + 1) * 512], start=(kc == 0), stop=(kc == 7)),
                                             reads=z_all + ["wo.%d" % half], writes=["pb%d" % (2 + 2 * k + half)])
                                    S.op("dve", lambda e, half=half, n=n, k=k: e.tensor_tensor(out=x1t[k][0:n, half * 512:(half + 1) * 512], in0=x1t[k][0:n, half * 512:(half + 1) * 512], in1=pb[2 + 2 * k + half][0:n, :], op=ALU.add),
                                         reads=["x1t%d" % k, "pb%d" % (2 + 2 * k + half)], writes=["x1t%d" % k])
                                S.dma(lambda e, k=k, t0=t0, n=n: e.dma_start(out=scr_x1[t0:t0 + n, :], in_=x1t[k][0:n, :]), "x1st%d" % k, reads=["x1t%d" % k], writes=["x1dram.%d" % i])

        if STAGE >= 6:
            h2T = hT
            h2_all = ["h2T.%d" % i for i in range(17)]
            i1T = sb([128, NT], F32)
            i2T = sb([128, NT], F32)
            gTt = sb([128, NT], F32)

            def x1_src(t0, n):
                return scr_x1[t0:t0 + n, :]

            with scope() as b1:
              if not os.environ.get("MK_NOB1"):
                xts = [sb([128, D], F32, b1) for _ in range(2)]
                hb = sb([128, D], BF16, b1)
                junk = sb([128, D], BF16, b1)
                ss = sb([128, 4], F32, b1)
                rmsnorm_T(x1_src, g2t, "g2t", h2T, "h2T", xts, hb, junk, ss, "n2")
                wqb = sb([128, 8, 2048], BF16, b1)
                for pc in range(4 if B1CUT >= 1 else 0):
                    wt, wtok = load_w([(wq[:, pc * 512:(pc + 1) * 512], 8)])
                    S.op("pool", lambda e, wt=wt, pc=pc: e.tensor_copy(out=wqb[:, :, pc * 512:(pc + 1) * 512], in_=wt[:, :, :]), reads=[wtok], writes=["wqb.%d" % pc])
                wq_all = ["wqb.%d" % pc for pc in range(4)]
                kyf = sb([128, 16, 128], F32, b1)
                kyb = sb([128, 16, 128], BF16, b1)
                S.dma(lambda e: e.dma_start(out=kyf[:], in_=keysT), "const", writes=["kyf"])
                S.op("pool", lambda e: e.tensor_copy(out=kyb[:], in_=kyf[:]), reads=["kyf"], writes=["kyb"])
                qTg = sb([128, 16, 512], BF16, b1)
                scs = sb([128, 16, 128], F32, b1)
                S.op("pool", lambda e: e.memset(qTg[:], 0.0), writes=["qTg.%d" % ch for ch in range(16)])
                v12 = sb([128, 16, 16], F32, b1)
                i12 = sb([128, 16, 16], U32, b1)
                i12f = sb([128, 16, 16], F32, b1)
                wk = sb([128, 256], F32, b1)
                cand = sb([128, 8, 256], F32, b1)
                svt = sb([128, 8, 16], F32, b1)
                pos = sb([128, 8, 16], U32, b1)
                pa_ = sb([128, 8, 16], U32, b1)
                pb_ = sb([128, 8, 16], U32, b1)
                paf = sb([128, 8, 16], F32, b1)
                pbf = sb([128, 8, 16], F32, b1)
                oh = sb([128, 8, 16, 16], F32, b1)
                i1f = sb([128, 8, 16], F32, b1)
                i2f = sb([128, 8, 16], F32, b1)
                gte = sb([128, 8, 16], F32, b1)
                zs = sb([128, 8], F32, b1)
                c4 = sb([128, 2], U32, b1)
                S.op("dve", lambda e: e.memset(c4[:, 0:1], 4), writes=["c4a"])
                S.op("dve", lambda e: e.memset(c4[:, 1:2], 15), writes=["c4b"])
                tgroups_b1 = [(i * 512, 512) for i in range(4)] + [(NT - 128, 128)]
                for gi, (t0g, ng) in enumerate(tgroups_b1 if (B1CUT >= 1 and B1SUB >= 2) else []):
                    for ch in range(16):
                        bk = 2 + ch % 2
                        for kc in range(8):
                            S.op("pe", lambda e, bk=bk, ch=ch, kc=kc, t0g=t0g, ng=ng: e.matmul(pb[bk][:, 0:ng], lhsT=wqb[:, kc, ch * 128:(ch + 1) * 128], rhs=h2T[:, kc, t0g:t0g + ng], start=(kc == 0), stop=(kc == 7)),
                                 reads=wq_all + h2_all, writes=["pb%d" % bk])
                        S.op("act", lambda e, bk=bk, ch=ch, ng=ng: e.activation(out=qTg[:, ch, 0:ng], in_=pb[bk][:, 0:ng], func=AF.Copy), reads=["pb%d" % bk], writes=["qTg.%d" % ch])
                    qT_all = ["qTg.%d" % ch for ch in range(16)]
                    for tl in range((ng + 127) // 128):
                        n = min(128, ng - tl * 128)
                        t0 = t0g + tl * 128
                        if B1SUB < 3:
                            continue
                        if os.environ.get("MK_GI") and str(gi) not in os.environ["MK_GI"]:
                            continue
                        for ch in range(16):
                            S.op("pe", lambda e, ch=ch, tl=tl, n=n: e.matmul(pb[4 + ch // 4][:, (ch % 4) * 128:(ch % 4 + 1) * 128], lhsT=qTg[:, ch, tl * 128:tl * 128 + 128], rhs=kyb[:, ch, :], start=True, stop=True),
                                 reads=qT_all + ["kyb"], writes=["sc%d" % ch])
                        if B1CUT < 2:
                            continue
                        for bq in range(4):
                            S.op("act", lambda e, bq=bq: e.activation(out=scs[:, bq * 4:(bq + 1) * 4, :], in_=pb[4 + bq][:, :].rearrange("p (c k) -> p c k", c=4), func=AF.Copy),
                                 reads=["sc%d" % (bq * 4 + j) for j in range(4)], writes=["sc%d" % (bq * 4 + j) for j in range(4)] + ["scs%d" % bq])
                        for ch in range(16):
                            sv_ = scs[0:n, ch, :]
                            S.op("dve", lambda e, sv_=sv_, ch=ch, n=n: e.max(out=v12[0:n, ch, 0:8], in_=sv_), reads=["scs%d" % (ch // 4)], writes=["v12a"])
                            S.op("dve", lambda e, sv_=sv_, ch=ch, n=n: e.max_index(out=i12[0:n, ch, 0:8], in_max=v12[0:n, ch, 0:8], in_values=sv_), reads=["scs%d" % (ch // 4), "v12a"], writes=["i12a"])
                            S.op("dve", lambda e, sv_=sv_, ch=ch, n=n: e.match_replace(out=wk[0:n, 0:128], in_to_replace=v12[0:n, ch, 0:8], in_values=sv_, imm_value=-1e30), reads=["scs%d" % (ch // 4), "v12a"], writes=["wk"])
                            S.op("dve", lambda e, ch=ch, n=n: e.max(out=v12[0:n, ch, 8:16], in_=wk[0:n, 0:128]), reads=["wk"], writes=["v12b"])
                            S.op("dve", lambda e, ch=ch, n=n: e.max_index(out=i12[0:n, ch, 8:16], in_max=v12[0:n, ch, 8:16], in_values=wk[0:n, 0:128]), reads=["wk", "v12b"], writes=["i12.%d" % ch, "v12.%d" % ch])
                        if B1CUT < 3:
                            continue
                        v_all = ["v12.%d" % ch for ch in range(16)]
                        i_all = ["i12.%d" % ch for ch in range(16)]
                        v12v = v12[:, :, :].rearrange("p (h f) k -> p h f k", f=2)
                        S.op("dve", lambda e, n=n, v12v=v12v: e.tensor_tensor(
                            out=cand[0:n].rearrange("p h (a b) -> p h a b", a=16), in0=v12v[0:n, :, 0, :].unsqueeze(3).broadcast_to([n, 8, 16, 16]),
                            in1=v12v[0:n, :, 1, :].unsqueeze(2).broadcast_to([n, 8, 16, 16]), op=ALU.add), reads=v_all, writes=["cand"])
                        S.op("dve", lambda e, n=n: e.tensor_copy(out=i12f[0:n], in_=i12[0:n]), reads=i_all, writes=["i12f"])
                        for h in range(8):
                            S.op("dve", lambda e, h=h, n=n: e.max(out=svt[0:n, h, 0:8], in_=cand[0:n, h, :]), reads=["cand"], writes=["sva"])
                            S.op("dve", lambda e, h=h, n=n: e.max_index(out=pos[0:n, h, 0:8], in_max=svt[0:n, h, 0:8], in_values=cand[0:n, h, :]), reads=["cand", "sva"], writes=["posa"])
                            S.op("dve", lambda e, h=h, n=n: e.match_replace(out=wk[0:n, :], in_to_replace=svt[0:n, h, 0:8], in_values=cand[0:n, h, :], imm_value=-1e30), reads=["cand", "sva"], writes=["wk"])
                            S.op("dve", lambda e, h=h, n=n: e.max(out=svt[0:n, h, 8:16], in_=wk[0:n, :]), reads=["wk"], writes=["svb"])
                            S.op("dve", lambda e, h=h, n=n: e.max_index(out=pos[0:n, h, 8:16], in_max=svt[0:n, h, 8:16], in_values=wk[0:n, :]), reads=["wk", "svb"], writes=["pos.%d" % h, "sv.%d" % h])
                        if B1CUT < 4:
                            continue
                        p_all = ["pos.%d" % h for h in range(8)]
                        s_all = ["sv.%d" % h for h in range(8)]
                        S.op("dve", lambda e, n=n: e.tensor_scalar(out=pa_[0:n], in0=pos[0:n], scalar1=c4[0:n, 0:1], scalar2=None, op0=ALU.logical_shift_right), reads=p_all + ["c4a"], writes=["pa_"])
                        S.op("dve", lambda e, n=n: e.tensor_scalar(out=pb_[0:n], in0=pos[0:n], scalar1=c4[0:n, 1:2], scalar2=None, op0=ALU.bitwise_and), reads=p_all + ["c4b"], writes=["pb_"])
                        S.op("dve", lambda e, n=n: e.tensor_copy(out=paf[0:n], in_=pa_[0:n]), reads=["pa_"], writes=["paf"])
                        S.op("dve", lambda e, n=n: e.tensor_copy(out=pbf[0:n], in_=pb_[0:n]), reads=["pb_"], writes=["pbf"])
                        i12v = i12f[:, :, :].rearrange("p (h f) k -> p h f k", f=2)
                        for (pf, pfn, f, dst, dn) in ((paf, "paf", 0, i1f, "i1f"), (pbf, "pbf", 1, i2f, "i2f")):
                            S.op("dve", lambda e, n=n, pf=pf: e.tensor_tensor(
                                out=oh[0:n], in0=pf[0:n].unsqueeze(3).broadcast_to([n, 8, 16, 16]),
                                in1=iota[0:n, 0:16].unsqueeze(1).unsqueeze(1).broadcast_to([n, 8, 16, 16]), op=ALU.is_equal), reads=[pfn, "iota"], writes=["oh"])
                            S.op("dve", lambda e, n=n, f=f, i12v=i12v: e.tensor_tensor(
                                out=oh[0:n], in0=oh[0:n], in1=i12v[0:n, :, f, :].unsqueeze(2).broadcast_to([n, 8, 16, 16]), op=ALU.mult), reads=["oh", "i12f"], writes=["oh"])
                            S.op("dve", lambda e, n=n, dst=dst: e.tensor_reduce(out=dst[0:n], in_=oh[0:n], axis=AX.X, op=ALU.add), reads=["oh"], writes=[dn])
                        if B1CUT < 5:
                            continue
                        S.op("dve", lambda e, n=n: e.tensor_tensor(out=gte[0:n], in0=svt[0:n], in1=svt[0:n, :, 0:1].broadcast_to([n, 8, 16]), op=ALU.subtract), reads=s_all, writes=["gte"])
                        S.op("act", lambda e, n=n: e.activation(out=gte[0:n], in_=gte[0:n], func=AF.Exp), reads=["gte"], writes=["gte"])
                        S.op("dve", lambda e, n=n: e.tensor_reduce(out=zs[0:n], in_=gte[0:n], axis=AX.X, op=ALU.add), reads=["gte"], writes=["zs"])
                        S.op("dve", lambda e, n=n: e.reciprocal(out=zs[0:n], in_=zs[0:n]), reads=["zs"], writes=["zs"])
                        S.op("dve", lambda e, n=n: e.tensor_tensor(out=gte[0:n], in0=gte[0:n], in1=zs[0:n, :].unsqueeze(2).broadcast_to([n, 8, 16]), op=ALU.mult), reads=["gte", "zs"], writes=["gte"])
                        for (srcx, sn, dstx, dn) in ((i1f, "i1f", i1T, "i1T"), (i2f, "i2f", i2T, "i2T"), (gte, "gte", gTt, "gTt")):
                            S.op("pe", lambda e, srcx=srcx, n=n: e.transpose(out=pb[2][:, 0:n], in_=srcx[0:n].rearrange("p h k -> p (h k)"), identity=identf[0:n, 0:n]),
                                 reads=[sn, "identf"], writes=["pb2"])
                            S.op("act", lambda e, dstx=dstx, t0=t0, n=n: e.activation(out=dstx[:, t0:t0 + n], in_=pb[2][:, 0:n], func=AF.Copy), reads=["pb2"], writes=[dn])

            with scope() as b2:
              if STAGE >= 7:
                  TS = 384
                  Wsb = sb([128, TS, 128], BF16, b2)
                  wb0 = wbf[0][:].rearrange("p a b -> p (a b)")
                  wb1 = wbf[1][:].rearrange("p a b -> p (a b)")
                  E1 = wb0[:, 0:2048].rearrange("p (t i) -> p t i", t=16)
                  E2 = wb0[:, 2048:4096].rearrange("p (t i) -> p t i", t=16)
                  G2 = wb1[:, 0:2048].rearrange("p (t i) -> p t i", t=16)
                  NSLOT = 4
                  wsb16 = wst[:].bitcast(BF16).rearrange("p a b -> p (a b)")
                  utb = [wsb16[:, sl * 2048:sl * 2048 + 1024].rearrange("p (k e) -> p k e", k=8) for sl in range(NSLOT)]
                  vtb = [wsb16[:, sl * 2048 + 1024:(sl + 1) * 2048] for sl in range(NSLOT)]
                  ge = [sb([128, TS], F32, b2) for _ in range(2)]
                  WA = [sb([128, TS], BF16, b2) for _ in range(2)]
                  ysb = [sb([128, D], F32, b2) for _ in range(2)]
                  x1r = [sb([128, D], F32, b2)] * 2
                  fg = sb([128, D], F32, b2)
                  junk2 = wb1[:, 2048:3072]
                  ss2 = sb([128, 4], F32, b2)
                  S.dma(lambda e: e.dma_start(out=fg[:], in_=fgb.broadcast_to([128, D])), "const", writes=["fg"])
                  stiles = [(i * TS, TS) for i in range(5)] + [(5 * TS, NT - 5 * TS)]
                  lctr = 0
                  fctr = 0
                  for (s0, T) in stiles:
                      for sbk in range(T // 16):
                          tt0 = sbk * 16
                          S.op("dve", lambda e, s0=s0, tt0=tt0: e.tensor_tensor(
                              out=E1, in0=iota[:, :].unsqueeze(1).broadcast_to([128, 16, 128]),
                              in1=i1T[:, s0 + tt0:s0 + tt0 + 16].unsqueeze(2).broadcast_to([128, 16, 128]), op=ALU.is_equal), reads=["iota", "i1T"], writes=["E1"])
                          S.op("dve", lambda e, s0=s0, tt0=tt0: e.tensor_tensor(
                              out=E2, in0=iota[:, :].unsqueeze(1).broadcast_to([128, 16, 128]),
                              in1=i2T[:, s0 + tt0:s0 + tt0 + 16].unsqueeze(2).broadcast_to([128, 16, 128]), op=ALU.is_equal), reads=["iota", "i2T"], writes=["E2"])
                          S.op("dve", lambda e, s0=s0, tt0=tt0: e.tensor_tensor(
                              out=G2, in0=E2, in1=gTt[:, s0 + tt0:s0 + tt0 + 16].unsqueeze(2).broadcast_to([128, 16, 128]), op=ALU.mult), reads=["E2", "gTt"], writes=["G2"])
                          for q4 in range(4):
                              bk = 6 + q4 % 2
                              for j in range(4):
                                  tl = q4 * 4 + j
                                  S.op("pe", lambda e, bk=bk, j=j, tl=tl: e.matmul(pb[bk][:, j * 128:(j + 1) * 128], lhsT=G2[:, tl, :], rhs=E1[:, tl, :], start=True, stop=True),
                                       reads=["G2", "E1"], writes=["pb%d" % bk])
                              S.op("act", lambda e, bk=bk, tt0=tt0, q4=q4: e.activation(out=Wsb[:, tt0 + q4 * 4:tt0 + q4 * 4 + 4, :], in_=pb[bk][:, :].rearrange("p (t i) -> p t i", t=4), func=AF.Copy),
                                   reads=["pb%d" % bk], writes=["Wsb"])
                      ntile = (T + 127) // 128
                      for c in range(128):
                          sl = lctr % NSLOT
                          lctr += 1
                          S.dma(lambda e, c=c, sl=sl: e.dma_start(out=utb[sl], in_=scr_ut[c]), "utb%d" % sl, reads=["scr_ut%d" % c], writes=["utb%d" % sl])
                          S.dma(lambda e, c=c, sl=sl: e.dma_start(out=vtb[sl], in_=scr_v[c]), "vtb%d" % sl, reads=["scr_v%d" % c], writes=["vtb%d" % sl])
                          k = c % 2
                          for kc in range(8):
                              S.op("pe", lambda e, k=k, kc=kc, sl=sl, s0=s0, T=T: e.matmul(pb[6 + k][:, 0:T], lhsT=utb[sl][:, kc, :], rhs=h2T[:, kc, s0:s0 + T], start=(kc == 0), stop=(kc == 7)),
                                   reads=["utb%d" % sl] + h2_all, writes=["pb%d" % (6 + k)])
                          S.op("act", lambda e, k=k, T=T: e.activation(out=ge[k][:, 0:T], in_=pb[6 + k][:, 0:T], func=AF.Gelu), reads=["pb%d" % (6 + k)], writes=["ge%d" % k])
                          S.op("dve", lambda e, k=k, T=T, c=c: e.tensor_tensor(out=WA[k][:, 0:T], in0=ge[k][:, 0:T], in1=Wsb[:, 0:T, c], op=ALU.mult), reads=["ge%d" % k, "Wsb"], writes=["WA%d" % k])
                          for ti in range(ntile):
                              n = min(128, T - ti * 128)
                              for half in range(2):
                                  S.op("pe", lambda e, ti=ti, half=half, n=n, k=k, sl=sl, c=c: e.matmul(pb[2 * ti + half][0:n, :], lhsT=WA[k][:, ti * 128:ti * 128 + n], rhs=vtb[sl][:, half * 512:(half + 1) * 512], start=(c == 0), stop=(c == 127)),
                                       reads=["WA%d" % k, "vtb%d" % sl], writes=["pb%d" % (2 * ti + half)])
                      for ti in range(ntile):
                          n = min(128, T - ti * 128)
                          t0 = s0 + ti * 128
                          k = fctr % 2
                          fctr += 1
                          tix = t0 // 128
                          S.dma(lambda e, k=k, t0=t0, n=n: e.dma_start(out=x1r[k][0:n, :], in_=scr_x1[t0:t0 + n, :]), "x1r", reads=["x1dram.%d" % tix], writes=["x1r"])
                          for half in range(2):
                              S.op("dve", lambda e, k=k, n=n, ti=ti, half=half: e.tensor_tensor(out=ysb[k][0:n, half * 512:(half + 1) * 512], in0=x1r[k][0:n, half * 512:(half + 1) * 512], in1=pb[2 * ti + half][0:n, :], op=ALU.add),
                                   reads=["x1r", "pb%d" % (2 * ti + half)], writes=["ysb%d" % k])
                          S.op("dve", lambda e: e.memset(ss2[:, 0:1], 0.0), writes=["ss2"])
                          S.op("act", lambda e, k=k, n=n: e.activation(out=junk2[0:n, :], in_=ysb[k][0:n, :], func=AF.Square, accum_out=ss2[0:n, 0:1]), reads=["ysb%d" % k, "ss2"], writes=["junk2", "ss2"])
                          S.op("act", lambda e, n=n: e.activation(out=ss2[0:n, 1:2], in_=ss2[0:n, 0:1], func=AF.Sqrt, bias=epst[0:n, :], scale=1.0 / D), reads=["ss2", "eps"], writes=["ss21"])
                          S.op("dve", lambda e, n=n: e.reciprocal(out=ss2[0:n, 2:3], in_=ss2[0:n, 1:2]), reads=["ss21"], writes=["ss22"])
                          S.op("dve", lambda e, k=k, n=n: e.scalar_tensor_tensor(out=ysb[k][0:n, :], in0=ysb[k][0:n, :], scalar=ss2[0:n, 2:3], in1=fg[0:n, :], op0=ALU.mult, op1=ALU.mult),
                               reads=["ysb%d" % k, "ss22", "fg"], writes=["ysb%d" % k])
                          dst = y_p[t0:t0 + n, :] if t0 < SEQ else y_s[:, :]
                          S.dma(lambda e, k=k, n=n, dst=dst: e.dma_start(out=dst, in_=ysb[k][0:n, :]), "yst%d" % k, reads=["ysb%d" % k], final=True)

        with nc.Block() as block:
            S.emit(block)
    return nc


_PROG = None


def kernel(x_prompt, x_sample, cache_kv_w128, cache_kv_w512, cache_kv_w2048, state_conv,
           norm1_g, w_in, conv_w, w_out_a, w_out_b, w_o, norm2_g,
           peer_wq, peer_keys, peer_u, peer_v, final_g):
    global _PROG
    if _PROG is None:
        _PROG = build_program()
    nc = _PROG
    f = lambda a: np.ascontiguousarray(np.asarray(a, dtype=np.float32))
    consts = _consts()
    shared = dict(
        g1T=f(np.asarray(norm1_g)[0].reshape(8, 128).T),
        g2T=f(np.asarray(norm2_g)[0].reshape(8, 128).T),
        w_in=f(np.asarray(w_in)[0]),
        convw=f(np.asarray(conv_w)[0].reshape(3, 8, 128).transpose(2, 1, 0)),
        w_out_a=f(np.asarray(w_out_a)[0]),
        w_out_b=f(np.asarray(w_out_b)[0]),
        w_o=f(np.asarray(w_o)[0]),
        wq=f(np.asarray(peer_wq)[0]),
        keysT=f(np.asarray(peer_keys)[0].transpose(1, 0, 2, 3).reshape(16, 128, 128).transpose(2, 0, 1)),
        uT=f(np.asarray(peer_u)[0].T),
        vtab=f(np.asarray(peer_v)[0]),
        fgb=f(np.asarray(final_g).reshape(1, D)),
    )
    shared.update(consts)
    caches = [np.asarray(cache_kv_w128)[0], np.asarray(cache_kv_w512)[0], np.asarray(cache_kv_w2048)[0]]
    xpr = np.asarray(x_prompt)
    xsa = np.asarray(x_sample)
    stc = np.asarray(state_conv)[0]
    in_maps = []
    for c in range(NCORES):
        m = dict(shared)
        m["xp"] = f(xpr[c])
        m["xs"] = f(xsa[c * 16:(c + 1) * 16].reshape(NS, D))
        for g in range(3):
            m["cache%d" % g] = f(caches[g][c * 16:(c + 1) * 16])
        m["stconv"] = f(stc[c * 16:(c + 1) * 16].reshape(32, D))
        in_maps.append(m)
    ncr = int(os.environ.get("MK_CORES", str(NCORES)))
    res = run_bass_kernel_spmd(nc, in_maps[0:ncr], core_ids=list(range(ncr)))
    R = list(res.results)
    while len(R) < NCORES:
        R.append(R[0])
    y_prompt = np.stack([R[c]["y_p"] for c in range(NCORES)], 0)
    y_sample = np.concatenate([R[c]["y_s"].reshape(16, 4, D) for c in range(NCORES)], 0)
    kvp = [np.stack([R[c]["kvp%d" % g] for c in range(NCORES)], 0)[None] for g in range(3)]
    convp = np.stack([R[c]["conv_p"] for c in range(NCORES)], 0)[None]
    kvs = [np.concatenate([R[c]["kvs%d" % g] for c in range(NCORES)], 0)[None] for g in range(3)]
    convs = np.concatenate([R[c]["conv_s"].reshape(16, 2, D) for c in range(NCORES)], 0)[None]
    return (y_prompt.astype(np.float32), y_sample.astype(np.float32), kvp[0], kvp[1], kvp[2], convp,
            kvs[0], kvs[1], kvs[2], convs)
```

```python
import os
import numpy as np
from contextlib import ExitStack, contextmanager
import concourse.bass as bass
import concourse.mybir as mybir
from concourse.bass_utils import run_bass_kernel_spmd
import ml_dtypes

F32 = mybir.dt.float32
BF16 = mybir.dt.bfloat16
U32 = mybir.dt.uint32
ALU = mybir.AluOpType
AF = mybir.ActivationFunctionType
AX = mybir.AxisListType

NCORES = 8
D = 1024
SEQ = 2048
NS = 64
NT = SEQ + NS
PROJ = 9728
GROUPS = ((128, 1), (512, 4), (2048, 16))
EPS = 1e-6
NEXP = 16384
ENGS = ("pe", "act", "dve", "pool", "sp")
STAGE = int(os.environ.get("MK_STAGE", "99"))
B1CUT = int(os.environ.get("MK_B1CUT", "99"))
CPY_ENG = os.environ.get("MK_CPYENG", "pe")
B1SUB = int(os.environ.get("MK_B1SUB", "99"))


class Sched:
    def __init__(self, nc, n_dma_sems=120):
        self.nc = nc
        self.streams = {e: [] for e in ENGS}
        self.count = {e: 0 for e in ENGS}
        self.last_w = {}
        self.readers = {}
        self.waited = {e: {} for e in ENGS}
        self.psem = {}
        self.dsems = {}
        self.dcount = {}
        self.n_dma_sems = n_dma_sems
        self.final_events = []
        self.pending = {e: [] for e in ENGS}

    def barrier(self):
        evs = [(e, self.count[e]) for e in ("pe", "act", "dve", "pool") if self.count[e] > 0]
        evs += [(("d", k), v) for k, v in self.dcount.items() if v > 0 and not str(k).startswith("cpy")]
        for eng in ENGS:
            for s_, v in evs:
                if s_ == eng:
                    continue
                if self.waited[eng].get(s_, 0) >= v:
                    continue
                self.waited[eng][s_] = v
                self.pending[eng].append((s_, v))

    def alloc(self, stack):
        for e in ("pe", "act", "dve", "pool"):
            self.psem[e] = stack.enter_context(self.nc.semaphore("p_" + e))
        self.stack = stack

    def dsem(self, key):
        if key not in self.dsems:
            assert len(self.dsems) < self.n_dma_sems, "too many dma sems"
            self.dsems[key] = self.stack.enter_context(self.nc.semaphore("d%d" % len(self.dsems)))
            self.dcount[key] = 0
        return self.dsems[key]

    def _deps(self, eng, reads, writes):
        ev = {}

        def add(e):
            if e is None:
                return
            s, v = e
            if ev.get(s, 0) < v:
                ev[s] = v

        for t in reads:
            add(self.last_w.get(t))
        for t in writes:
            add(self.last_w.get(t))
            for r in self.readers.get(t, ()):
                add(r)
        out = []
        for s, v in ev.items():
            if eng == "pe" and s == "pe":
                continue
            if self.waited[eng].get(s, 0) >= v:
                continue
            self.waited[eng][s] = v
            out.append((s, v))
        return out

    def _commit(self, event, reads, writes):
        for t in reads:
            self.readers.setdefault(t, []).append(event)
        for t in writes:
            self.last_w[t] = event
            self.readers[t] = []

    def op(self, eng, fn, reads=(), writes=()):
        waits = self.pending[eng] + self._deps(eng, reads, writes)
        self.pending[eng] = []
        self.count[eng] += 1
        event = (eng, self.count[eng])
        self.streams[eng].append((waits, fn, ("p", eng)))
        self._commit(event, reads, writes)
        return event

    def dma(self, fn, key, reads=(), writes=(), eng="sp", final=False):
        self.dsem(key)
        waits = self.pending[eng] + self._deps(eng, reads, writes)
        self.pending[eng] = []
        self.dcount[key] += 16
        event = (("d", key), self.dcount[key])
        self.streams[eng].append((waits, fn, ("d", key)))
        self._commit(event, reads, writes)
        if final:
            self.final_events.append(event)
        return event

    def _sem(self, s):
        if isinstance(s, tuple):
            return self.dsems[s[1]]
        return self.psem[s]

    def emit(self, block):
        S = self
        fin = {}
        for s, v in self.final_events:
            if fin.get(s, 0) < v:
                fin[s] = v

        def run(engname, engobj, extra_final=False):
            for waits, fn, inc in S.streams[engname]:
                for s, v in waits:
                    engobj.wait_ge(S._sem(s), v)
                ins = fn(engobj)
                if inc[0] == "p":
                    ins.then_inc(S.psem[inc[1]], 1)
                else:
                    ins.then_inc(S.dsems[inc[1]], 16)
            if extra_final:
                for s, v in fin.items():
                    engobj.wait_ge(S._sem(s), v)

        @block.sync
        def _(e):
            run("sp", e, True)

        @block.tensor
        def _(e):
            run("pe", e)

        @block.scalar
        def _(e):
            run("act", e)

        @block.vector
        def _(e):
            run("dve", e)

        @block.gpsimd
        def _(e):
            run("pool", e)


def _consts():
    slopes = np.exp2(-8.0 * np.arange(1, 9, dtype=np.float64) / 8.0)
    kj = np.arange(128)[:, None].astype(np.float64)
    qi = np.arange(128)[None, :].astype(np.float64)
    masks = np.zeros((128, 24, 2, 128), np.float32)
    for h in range(8):
        for g, (win, dil) in enumerate(GROUPS):
            dist_d = qi - kj
            md = np.where(dist_d >= 0, np.exp(-slopes[h] * dil * dist_d), 0.0)
            dist_p = 128 + qi - kj
            mp = np.where(dist_p <= 128, np.exp(-slopes[h] * dil * dist_p), 0.0)
            masks[:, h * 3 + g, 0, :] = mp
            masks[:, h * 3 + g, 1, :] = md
    sbias = np.zeros((128, 3, 129), np.float32)
    j = np.arange(129, dtype=np.float64)
    for p in range(128):
        s = p % 8
        for g, (win, dil) in enumerate(GROUPS):
            sbias[p, g, :] = -slopes[s] * dil * (128.0 - j)
    iota = np.tile(np.arange(128, dtype=np.float32)[None, :], (128, 1))
    return dict(
        c_identf=np.eye(128, dtype=np.float32),
        c_identb=np.eye(128, dtype=np.float32).astype(ml_dtypes.bfloat16),
        c_masks=masks.astype(ml_dtypes.bfloat16),
        c_sbias=sbias,
        c_iota=iota,
    )


def build_program():
    nc = bass.Bass("TRN2", target_bir_lowering=False)

    def din(name, shape, dt=F32):
        return nc.dram_tensor(name, list(shape), dt, kind="ExternalInput").ap()

    def dout(name, shape, dt=F32):
        return nc.dram_tensor(name, list(shape), dt, kind="ExternalOutput").ap()

    def dscr(name, shape, dt=F32):
        return nc.dram_tensor(name, list(shape), dt, kind="Internal").ap()

    xp = din("xp", [SEQ, D])
    xs = din("xs", [NS, D])
    caches = [din("cache%d" % g, [16, GROUPS[g][0], 2, 8, 64]) for g in range(3)]
    stconv = din("stconv", [32, D])
    g1T = din("g1T", [128, 8])
    g2T = din("g2T", [128, 8])
    w_in = din("w_in", [D, PROJ])
    convw = din("convw", [128, 8, 3])
    w_out_a = din("w_out_a", [D, D])
    w_out_b = din("w_out_b", [512, D])
    w_o = din("w_o", [D, D])
    wq = din("wq", [D, 2048])
    keysT = din("keysT", [128, 16, 128])
    uT = din("uT", [D, NEXP])
    vtab = din("vtab", [NEXP, D])
    fgb = din("fgb", [1, D])
    c_identf = din("c_identf", [128, 128])
    c_identb = din("c_identb", [128, 128], BF16)
    c_masks = din("c_masks", [128, 24, 2, 128], BF16)
    c_sbias = din("c_sbias", [128, 3, 129])
    c_iota = din("c_iota", [128, 128])

    y_p = dout("y_p", [SEQ, D])
    y_s = dout("y_s", [NS, D])
    kvp = [dout("kvp%d" % g, [min(GROUPS[g][0], SEQ), 2, 8, 64]) for g in range(3)]
    conv_p = dout("conv_p", [2, D])
    kvs = [dout("kvs%d" % g, [16, GROUPS[g][0], 2, 8, 64]) for g in range(3)]
    conv_s = dout("conv_s", [32, D])

    dbg_ot = None
    scr_q = dscr("scr_q", [NS, 1536])
    scr_o = dscr("scr_o", [NS, 512])
    scr_x1 = dscr("scr_x1", [NT, D])
    scr_ut = dscr("scr_ut", [128, 128, 8, 128], BF16)
    scr_v = dscr("scr_v", [128, 128, D], BF16)

    with ExitStack() as st:
        S = Sched(nc)
        S.alloc(st)

        @contextmanager
        def scope():
            with ExitStack() as es:
                yield es
                S.barrier()
        cnt = [0]

        def sb(shape, dt, stack=st):
            cnt[0] += 1
            return stack.enter_context(nc.sbuf_tensor("t%d" % cnt[0], list(shape), dt))

        pb = [st.enter_context(nc.psum_tensor("pb%d" % i, [128, 512], F32)) for i in range(8)]

        identf = sb([128, 128], F32)
        identb = sb([128, 128], BF16)
        iota = sb([128, 128], F32)
        g1t = sb([128, 8], F32)
        g2t = sb([128, 8], F32)
        cwt = sb([128, 8, 3], F32)
        epst = sb([128, 1], F32)
        for (t, src, nm) in ((identf, c_identf, "identf"), (identb, c_identb, "identb"), (iota, c_iota, "iota"),
                             (g1t, g1T, "g1t"), (g2t, g2T, "g2t"), (cwt, convw, "cwt")):
            S.dma(lambda e, t=t, src=src: e.dma_start(out=t[:], in_=src), "const", writes=[nm])
        S.op("dve", lambda e: e.memset(epst[:], EPS), writes=["eps"])

        wst = sb([128, 8, 512], F32)
        wbf = [sb([128, 8, 512], BF16) for _ in range(2)]
        wctr = [0]

        def load_w(pieces, rows=128):
            slot = wctr[0] % 2
            wctr[0] += 1
            off = 0
            toks = []
            for i, (src, kcn) in enumerate(pieces):
                n = src.shape[-1]
                r = src.shape[0] // kcn
                srcv = src.rearrange("(kc p) c -> p kc c", p=r)
                tok = "wst.%d" % i
                S.dma(lambda e, srcv=srcv, off=off, n=n, r=r, kcn=kcn: e.dma_start(out=wst[0:r, 0:kcn, off:off + n], in_=srcv),
                      "wst", writes=[tok])
                toks.append(tok)
                off += n
            S.op("pool", lambda e, slot=slot, off=off: e.tensor_copy(out=wbf[slot][:, :, 0:off], in_=wst[:, :, 0:off]),
                 reads=toks, writes=["wbf%d" % slot] + ["wstall"])
            for tok in ["wst.%d" % i for i in range(8)]:
                S.readers.setdefault(tok, []).append(S.last_w["wbf%d" % slot])
            return wbf[slot], "wbf%d" % slot

        tiles = [(i * 128, 128) for i in range(16)] + [(SEQ, NS)]
        tgroups = [(i * 512, 512) for i in range(4)] + [(SEQ, NS)]

        def x_src(t0, n):
            return xp[t0:t0 + n, :] if t0 < SEQ else xs[:, :]

        def rmsnorm_T(src_fn, gt, gname, dstT, dst_tok, xts, hb, junk, ss, stack_tag):
            for i, (t0, n) in enumerate(tiles):
                k = i % 2
                xt = xts[k]
                S.dma(lambda e, xt=xt, t0=t0, n=n: e.dma_start(out=xt[0:n, :], in_=src_fn(t0, n)), stack_tag + "x%d" % k,
                      reads=["x1dram.%d" % i] if stack_tag == "n2" else [], writes=["xt%d" % k])
                S.op("dve", lambda e: e.memset(ss[:, 0:1], 0.0), writes=["ss"])
                S.op("act", lambda e, xt=xt, n=n: e.activation(out=junk[0:n, :], in_=xt[0:n, :], func=AF.Square, accum_out=ss[0:n, 0:1]),
                     reads=["xt%d" % k, "ss"], writes=["junk", "ss"])
                S.op("act", lambda e, n=n: e.activation(out=ss[0:n, 1:2], in_=ss[0:n, 0:1], func=AF.Sqrt, bias=epst[0:n, :], scale=1.0 / D),
                     reads=["ss", "eps"], writes=["ss1"])
                S.op("dve", lambda e, n=n: e.reciprocal(out=ss[0:n, 2:3], in_=ss[0:n, 1:2]), reads=["ss1"], writes=["ss2"])
                S.op("dve", lambda e, xt=xt, n=n: e.tensor_scalar(out=hb[0:n, :], in0=xt[0:n, :], scalar1=ss[0:n, 2:3], scalar2=None, op0=ALU.mult),
                     reads=["xt%d" % k, "ss2"], writes=["hb"])
                pt = pb[k][:].bitcast(BF16)
                for kc in range(8):
                    S.op("pe", lambda e, pt=pt, kc=kc, n=n: e.transpose(out=pt[:, kc * 128:kc * 128 + n], in_=hb[0:n, kc * 128:(kc + 1) * 128], identity=identb[0:n, 0:n]),
                         reads=["hb", "identb"], writes=["pb%d" % k])
                S.op("dve", lambda e, pt=pt, t0=t0, n=n: e.tensor_tensor(
                    out=dstT[:, :, t0:t0 + n], in0=pt.rearrange("p (k t) -> p k t", k=8)[:, :, 0:n],
                    in1=gt[:, :].unsqueeze(2).broadcast_to([128, 8, n]), op=ALU.mult),
                    reads=["pb%d" % k, gname], writes=[dst_tok + ".%d" % i] + (["hT.%d" % i] if dst_tok != "hT" else []))

        hT = sb([128, 8, NT], BF16)
        hT_all = ["hT.%d" % i for i in range(17)]

        with scope() as pa:
            uf, vf, ub, vb = [], [], [], []
            prep_c = [0]

            def prep_chunk():
                c = prep_c[0]
                if c >= 128 or STAGE < 6:
                    return
                prep_c[0] += 1
                k = 0
                S.dma(lambda e, c=c, k=k: e.dma_start(out=uf[k][:], in_=uT[:, c * 128:(c + 1) * 128].rearrange("(kc p) e -> p kc e", p=128)),
                      "uf%d" % k, writes=["uf%d" % k])
                S.dma(lambda e, c=c, k=k: e.dma_start(out=vf[k][:], in_=vtab[c * 128:(c + 1) * 128, :]),
                      "vf%d" % k, writes=["vf%d" % k])
                S.op("act", lambda e, k=k: e.activation(out=ub[k][:], in_=uf[k][:], func=AF.Copy), reads=["uf%d" % k], writes=["ub%d" % k])
                S.op("pool", lambda e, k=k: e.tensor_copy(out=vb[k][:], in_=vf[k][:]), reads=["vf%d" % k], writes=["vb%d" % k])
                S.dma(lambda e, c=c, k=k: e.dma_start(out=scr_ut[c], in_=ub[k][:]), "ubs%d" % k, reads=["ub%d" % k], writes=["scr_ut%d" % c])
                S.dma(lambda e, c=c, k=k: e.dma_start(out=scr_v[c], in_=vb[k][:]), "vbs%d" % k, reads=["vb%d" % k], writes=["scr_v%d" % c])

            with scope() as pa1:
                OT = sb([64, 8, NT], BF16, pa1)
                with scope() as s1:
                    xts = [sb([128, D], F32, s1) for _ in range(2)]
                    hb = sb([128, D], BF16, s1)
                    junk = sb([128, D], BF16, s1)
                    ss = sb([128, 4], F32, s1)
                    rmsnorm_T(x_src, g1t, "g1t", hT, "hT", xts, hb, junk, ss, "n1")

                if STAGE >= 2:
                    with scope() as s2:
                        skv = sb([NS, 512], F32, s2)
                        pst = [sb([128, 512], F32, s2) for _ in range(2)]
                        pctr = 0
                        for typ in range(3):
                            for g in range(3):
                                W = GROUPS[g][0]
                                c0 = 3072 + typ * 1536 + g * 512
                                wt, wtok = load_w([(w_in[:, c0:c0 + 512], 8)])
                                for kc in range(8):
                                    S.op("pe", lambda e, wt=wt, kc=kc: e.matmul(pb[2][0:NS, :], lhsT=hT[:, kc, SEQ:NT], rhs=wt[:, kc, :], start=(kc == 0), stop=(kc == 7)),
                                         reads=[wtok, "hT.16"], writes=["pb2"])
                                S.op("act", lambda e: e.activation(out=skv[:], in_=pb[2][0:NS, :], func=AF.Copy), reads=["pb2"], writes=["skv"])
                                for b in range(16):
                                    if typ == 0:
                                        S.dma(lambda e, b=b, g=g: e.dma_start(out=scr_q[b * 4:(b + 1) * 4, g * 512:(g + 1) * 512], in_=skv[b * 4:(b + 1) * 4, :]),
                                              "skvst", reads=["skv"], writes=["scr_q.%d.%d" % (g, b)])
                                    else:
                                        S.dma(lambda e, b=b, g=g, W=W, typ=typ: e.dma_start(
                                            out=kvs[g][b, W - 4:W, typ - 1].rearrange("t s d -> t (s d)"), in_=skv[b * 4:(b + 1) * 4, :]),
                                            "skvst", reads=["skv"], writes=["kvsnew.%d.%d.%d" % (g, typ, b)], final=True)
                                if typ == 0:
                                    continue
                                keep = min(W, SEQ) // 128
                                for ti in range(16 - keep, 16):
                                    k = pctr % 2
                                    pctr += 1
                                    for kc in range(8):
                                        S.op("pe", lambda e, wt=wt, kc=kc, ti=ti, k=k: e.matmul(pb[3 + k][:, :], lhsT=hT[:, kc, ti * 128:(ti + 1) * 128], rhs=wt[:, kc, :], start=(kc == 0), stop=(kc == 7)),
                                             reads=[wtok, "hT.%d" % ti], writes=["pb%d" % (3 + k)])
                                    S.op("act", lambda e, k=k: e.activation(out=pst[k][:], in_=pb[3 + k][:], func=AF.Copy), reads=["pb%d" % (3 + k)], writes=["pst%d" % k])
                                    r0 = (ti - (16 - keep)) * 128
                                    S.dma(lambda e, g=g, r0=r0, typ=typ, k=k: e.dma_start(out=kvp[g][r0:r0 + 128, typ - 1].rearrange("t s d -> t (s d)"), in_=pst[k][:]),
                                          "pst%d" % k, reads=["pst%d" % k], final=True)

                if STAGE >= 3:
                    with scope() as s3:
                        Kt = sb([128, 132, 64], F32, s3)
                        Vt = sb([128, 132, 64], F32, s3)
                        prod = sb([128, 43, 64], F32, s3)
                        qs = sb([128, 4, 3, 64], F32, s3)
                        knew = sb([128, 3, 4, 64], F32, s3)
                        vnew = sb([128, 3, 4, 64], F32, s3)
                        sbias = sb([128, 3, 129], F32, s3)
                        sc = sb([128, 129], F32, s3)
                        ex = sb([128, 129], F32, s3)
                        lacc = sb([128, 12], F32, s3)
                        og = sb([128, 12, 64], F32, s3)
                        osum = sb([128, 4, 64], F32, s3)
                        lsum = sb([128, 4], F32, s3)
                        otok = sb([NS, 512], F32, s3)
                        S.dma(lambda e: e.dma_start(out=sbias[:], in_=c_sbias), "const", writes=["sbias"])
                        S.op("dve", lambda e: e.memset(lacc[:], 0.0), writes=["lacc.%d" % c for c in range(12)])
                        for b in range(16):
                            S.dma(lambda e, b=b: e.dma_start(out=qs[b * 8:(b + 1) * 8], in_=scr_q[b * 4:(b + 1) * 4, :].rearrange("t (g s d) -> s t g d", g=3, s=8)),
                                  "qs", reads=["scr_q.%d.%d" % (g, b) for g in range(3)], writes=["qs.%d" % b])
                            for g in range(3):
                                W = GROUPS[g][0]
                                S.dma(lambda e, b=b, g=g, W=W: e.dma_start(out=knew[b * 8:(b + 1) * 8, g], in_=kvs[g][b, W - 4:W, 0].rearrange("t s d -> s t d")),
                                      "qs", reads=["kvsnew.%d.1.%d" % (g, b)], writes=["knew.%d.%d" % (g, b)])
                                S.dma(lambda e, b=b, g=g, W=W: e.dma_start(out=vnew[b * 8:(b + 1) * 8, g], in_=kvs[g][b, W - 4:W, 1].rearrange("t s d -> s t d")),
                                      "qs", reads=["kvsnew.%d.2.%d" % (g, b)], writes=["vnew.%d.%d" % (g, b)])
                        qs_all = ["qs.%d" % b for b in range(16)]
                        kn_all = ["knew.%d.%d" % (g, b) for g in range(3) for b in range(16)]
                        vn_all = ["vnew.%d.%d" % (g, b) for g in range(3) for b in range(16)]
                        for g in range(3):
                            W, dil = GROUPS[g]
                            for t in range(4):
                                col = g * 4 + t
                                if g == 0 and t > 0:
                                    pass
                                else:
                                    for b in range(16):
                                        if g == 0:
                                            ksrc = caches[0][b, :, 0].rearrange("r s d -> s r d")
                                            vsrc = caches[0][b, :, 1].rearrange("r s d -> s r d")
                                        else:
                                            ksrc = caches[g][b, :, 0].rearrange("(j r) s d -> s r j d", r=dil)[:, t, 0:128, :]
                                            vsrc = caches[g][b, :, 1].rearrange("(j r) s d -> s r j d", r=dil)[:, t, 0:128, :]
                                        S.dma(lambda e, b=b, ksrc=ksrc: e.dma_start(out=Kt[b * 8:(b + 1) * 8, 0:128, :], in_=ksrc), "Kt", writes=["Kt.%d" % b])
                                        S.dma(lambda e, b=b, vsrc=vsrc: e.dma_start(out=Vt[b * 8:(b + 1) * 8, 0:128, :], in_=vsrc), "Vt", writes=["Vt.%d" % b], eng=os.environ.get("MK_VENG", "act"))
                                    if g == 0:
                                        S.op("pool", lambda e: e.tensor_copy(out=Kt[:, 128:132, :], in_=knew[:, 0]), reads=kn_all, writes=["Ktn"])
                                        S.op("pool", lambda e: e.tensor_copy(out=Vt[:, 128:132, :], in_=vnew[:, 0]), reads=vn_all, writes=["Vtn"])
                                    else:
                                        S.op("pool", lambda e, g=g, t=t: e.tensor_copy(out=Kt[:, 128:129, :], in_=knew[:, g, t:t + 1, :]), reads=kn_all, writes=["Ktn"])
                                        S.op("pool", lambda e, g=g, t=t: e.tensor_copy(out=Vt[:, 128:129, :], in_=vnew[:, g, t:t + 1, :]), reads=vn_all, writes=["Vtn"])
                                w0 = t if g == 0 else 0
                                kt_all = ["Kt.%d" % b for b in range(16)] + ["Ktn"]
                                vt_all = ["Vt.%d" % b for b in range(16)] + ["Vtn"]
                                for ch in range(3):
                                    k0 = ch * 43
                                    S.op("dve", lambda e, w0=w0, k0=k0, t=t, g=g: e.tensor_tensor(
                                        out=prod[:], in0=Kt[:, w0 + k0:w0 + k0 + 43, :],
                                        in1=qs[:, t, g, :].unsqueeze(1).broadcast_to([128, 43, 64]), op=ALU.mult),
                                        reads=kt_all + qs_all, writes=["prod"])
                                    S.op("dve", lambda e, k0=k0: e.tensor_reduce(out=sc[:, k0:k0 + 43], in_=prod[:], axis=AX.X, op=ALU.add),
                                         reads=["prod"], writes=["sc.%d" % ch])
                                S.op("dve", lambda e, g=g: e.scalar_tensor_tensor(out=ex[:], in0=sc[:], scalar=0.125, in1=sbias[:, g, :], op0=ALU.mult, op1=ALU.add),
                                     reads=["sc.0", "sc.1", "sc.2", "sbias"], writes=["ex"])
                                S.op("act", lambda e, col=col: e.activation(out=sc[:], in_=ex[:], func=AF.Exp, accum_out=lacc[:, col:col + 1]),
                                     reads=["ex", "lacc.%d" % col], writes=["sc.0", "sc.1", "sc.2", "lacc.%d" % col])
                                for ch in range(3):
                                    k0 = ch * 43
                                    S.op("dve", lambda e, w0=w0, k0=k0: e.tensor_tensor(
                                        out=prod[:], in0=Vt[:, w0 + k0:w0 + k0 + 43, :],
                                        in1=sc[:, k0:k0 + 43].unsqueeze(2).broadcast_to([128, 43, 64]), op=ALU.mult),
                                        reads=vt_all + ["sc.0", "sc.1", "sc.2"], writes=["prod"])
                                    dst = og[:, col, :] if ch == 0 else ex[:, ch * 64 - 64:ch * 64]
                                    S.op("dve", lambda e, dst=dst: e.tensor_reduce(out=dst, in_=prod[:].rearrange("p k d -> p d k"), axis=AX.X, op=ALU.add),
                                         reads=["prod"], writes=["ogp.0"] if ch == 0 else ["ex"])
                                S.op("dve", lambda e, col=col: e.tensor_tensor(out=og[:, col, :], in0=og[:, col, :], in1=ex[:, 0:64], op=ALU.add),
                                     reads=["ogp.0", "ex"], writes=["ogp.0"])
                                S.op("dve", lambda e, col=col: e.tensor_tensor(out=og[:, col, :], in0=og[:, col, :], in1=ex[:, 64:128], op=ALU.add),
                                     reads=["ogp.0", "ex"], writes=["ogp.0", "og.%d" % col])
                        og_all = ["og.%d" % c for c in range(12)]
                        la_all = ["lacc.%d" % c for c in range(12)]
                        S.op("dve", lambda e: e.tensor_tensor(out=osum[:], in0=og[:, 0:4, :], in1=og[:, 4:8, :], op=ALU.add), reads=og_all, writes=["osum"])
                        S.op("dve", lambda e: e.tensor_tensor(out=osum[:], in0=osum[:], in1=og[:, 8:12, :], op=ALU.add), reads=og_all + ["osum"], writes=["osum"])
                        S.op("dve", lambda e: e.tensor_tensor(out=lsum[:], in0=lacc[:, 0:4], in1=lacc[:, 4:8], op=ALU.add), reads=la_all, writes=["lsum"])
                        S.op("dve", lambda e: e.tensor_tensor(out=lsum[:], in0=lsum[:], in1=lacc[:, 8:12], op=ALU.add), reads=la_all + ["lsum"], writes=["lsum"])
                        S.op("dve", lambda e: e.reciprocal(out=lsum[:], in_=lsum[:]), reads=["lsum"], writes=["lsum"])
                        S.op("dve", lambda e: e.tensor_tensor(out=osum[:], in0=osum[:], in1=lsum[:, :].unsqueeze(2).broadcast_to([128, 4, 64]), op=ALU.mult),
                             reads=["osum", "lsum"], writes=["osum"])
                        for b in range(16):
                            S.dma(lambda e, b=b: e.dma_start(out=scr_o[b * 4:(b + 1) * 4, :].rearrange("t (s d) -> s t d", s=8), in_=osum[b * 8:(b + 1) * 8]),
                                  "scro", reads=["osum"], writes=["scr_o.%d" % b])
                        S.dma(lambda e: e.dma_start(out=otok[:], in_=scr_o), "otok", reads=["scr_o.%d" % b for b in range(16)], writes=["otok"])
                        for h in range(8):
                            S.op("pe", lambda e, h=h: e.transpose(out=pb[2][0:64, h * 64:(h + 1) * 64], in_=otok[:, h * 64:(h + 1) * 64], identity=identf[0:NS, 0:NS]),
                                 reads=["otok", "identf"], writes=["pb2"])
                        S.op("act", lambda e: e.activation(out=OT[:, :, SEQ:NT], in_=pb[2][0:64, :].rearrange("p (h t) -> p h t", h=8), func=AF.Copy),
                             reads=["pb2"], writes=["OTs"])

                uf.extend([sb([128, 8, 128], F32, pa1)] * 2)
                vf.extend([sb([128, D], F32, pa1)] * 2)
                ub.extend([sb([128, 8, 128], BF16, pa1)] * 2)
                vb.extend([sb([128, D], BF16, pa1)] * 2)
                if STAGE >= 4:
                    with scope() as s4:
                        masks = sb([128, 24, 2, 128], BF16, s4)
                        QT = sb([64, 3, SEQ], BF16, s4)
                        KT = sb([64, 3, SEQ], BF16, s4)
                        Vh = sb([128, 3, 16, 128], BF16, s4)
                        acc = sb([128, SEQ], F32, s4)
                        rc = sb([64, 512], F32, s4)
                        ebuf = [sb([128, 256], F32, s4) for _ in range(2)]
                        pT = [sb([128, 256], BF16, s4) for _ in range(2)]
                        S.dma(lambda e: e.dma_start(out=masks[:], in_=c_masks), "const", writes=["masks"])
                        S.op("pool", lambda e: e.memset(Vh[:], 1.0), writes=["Vh"])

                        def permv(ap2048, dil):
                            return ap2048.rearrange("p (j r) -> p r j", r=dil)

                        uctr = 0
                        for h in range(8):
                            pieces = []
                            for typ in range(3):
                                for g in range(3):
                                    c0 = 3072 + typ * 1536 + g * 512 + h * 64
                                    pieces.append((w_in[:, c0:c0 + 64], 8))
                            wt, wtok = load_w(pieces[0:6])
                            wv, wvtok = load_w(pieces[6:9])
                            for typ, dstT, dname in ((0, QT, "QT"), (1, KT, "KT")):
                                for g in range(3):
                                    dil = GROUPS[g][1]
                                    wcol = (typ * 3 + g) * 64
                                    for tg in range(4):
                                        bk = 2 + (tg % 2)
                                        for kc in range(8):
                                            if dil == 1:
                                                rhs = hT[:, kc, tg * 512:(tg + 1) * 512]
                                            elif dil == 4:
                                                rhs = permv(hT[:, kc, 0:SEQ], 4)[:, tg, :]
                                            else:
                                                rhs = permv(hT[:, kc, 0:SEQ], 16)[:, tg * 4:(tg + 1) * 4, :]
                                            S.op("pe", lambda e, bk=bk, wt=wt, kc=kc, wcol=wcol, rhs=rhs: e.matmul(
                                                pb[bk][0:64, :], lhsT=wt[:, kc, wcol:wcol + 64], rhs=rhs, start=(kc == 0), stop=(kc == 7)),
                                                reads=[wtok] + hT_all[0:16], writes=["pb%d" % bk])
                                        S.op("act", lambda e, bk=bk, dstT=dstT, g=g, tg=tg: e.activation(out=dstT[:, g, tg * 512:(tg + 1) * 512], in_=pb[bk][0:64, :], func=AF.Copy),
                                             reads=["pb%d" % bk], writes=["%s.%d.%d" % (dname, g, tg)])
                            for g in range(3):
                                dil = GROUPS[g][1]
                                L = SEQ // dil
                                for half in range(2):
                                    bk = 4 + half
                                    for nb in range(8):
                                        n = half * 8 + nb
                                        r, lb = (n * 128) // L, ((n * 128) % L) // 128
                                        for kc in range(8):
                                            lhsT = permv(hT[:, kc, 0:SEQ], dil)[:, r, lb * 128:(lb + 1) * 128]
                                            S.op("pe", lambda e, bk=bk, nb=nb, lhsT=lhsT, kc=kc, g=g, wv=wv: e.matmul(
                                                pb[bk][:, nb * 64:(nb + 1) * 64], lhsT=lhsT, rhs=wv[:, kc, g * 64:(g + 1) * 64], start=(kc == 0), stop=(kc == 7)),
                                                reads=[wvtok] + hT_all[0:16], writes=["pb%d" % bk])
                                    S.op("act", lambda e, bk=bk, g=g, half=half: e.activation(
                                        out=Vh[:, g, half * 8:(half + 1) * 8, 0:64], in_=pb[bk][:, :].rearrange("p (n d) -> p n d", n=8), func=AF.Copy),
                                        reads=["pb%d" % bk, "Vh"], writes=["Vh.%d.%d" % (g, half)])
                            def front(g, n, u):
                                dil = GROUPS[g][1]
                                bpl = (SEQ // dil) // 128
                                lb = n % bpl
                                slots = ([0] if lb > 0 else []) + [1]
                                c_lo = slots[0] * 128
                                for sl in slots:
                                    kb = n - 1 if sl == 0 else n
                                    S.op("pe", lambda e, sl=sl, kb=kb, n=n, g=g, u=u: e.matmul(
                                        pb[4 + u][:, sl * 128:(sl + 1) * 128], lhsT=KT[:, g, kb * 128:(kb + 1) * 128], rhs=QT[:, g, n * 128:(n + 1) * 128],
                                        start=True, stop=True),
                                        reads=["KT.%d.%d" % (g, kb // 4), "QT.%d.%d" % (g, n // 4)], writes=["pb%d" % (4 + u)])
                                S.op("act", lambda e, u=u, c_lo=c_lo: e.activation(out=ebuf[u][:, c_lo:256], in_=pb[4 + u][:, c_lo:256], func=AF.Exp, scale=0.125),
                                     reads=["pb%d" % (4 + u)], writes=["ebuf%d" % u])
                                S.op("dve", lambda e, u=u, c_lo=c_lo, h=h, g=g: e.tensor_tensor(
                                    out=pT[u][:, c_lo:256], in0=ebuf[u][:, c_lo:256],
                                    in1=masks[:, h * 3 + g].rearrange("p s q -> p (s q)")[:, c_lo:256], op=ALU.mult),
                                    reads=["ebuf%d" % u, "masks"], writes=["pT%d" % u])

                            def back(g, n, u):
                                dil = GROUPS[g][1]
                                bpl = (SEQ // dil) // 128
                                r, lb = n // bpl, n % bpl
                                slots = ([0] if lb > 0 else []) + [1]
                                for i, sl in enumerate(slots):
                                    kb = n - 1 if sl == 0 else n
                                    S.op("pe", lambda e, u=u, sl=sl, kb=kb, g=g, i=i, last=(i == len(slots) - 1): e.matmul(
                                        pb[6 + u][:, 0:128], lhsT=Vh[:, g, kb, :], rhs=pT[u][:, sl * 128:(sl + 1) * 128], start=(i == 0), stop=last),
                                        reads=["pT%d" % u, "Vh.%d.%d" % (g, kb // 8), "Vh"], writes=["pb%d" % (6 + u)])
                                av = permv(acc[:, :], dil)[:, r, lb * 128:(lb + 1) * 128]
                                if g == 0:
                                    S.op("dve", lambda e, av=av, u=u: e.tensor_copy(out=av, in_=pb[6 + u][:, 0:128]), reads=["pb%d" % (6 + u)], writes=["acc"])
                                else:
                                    S.op("dve", lambda e, av=av, u=u: e.tensor_tensor(out=av, in0=av, in1=pb[6 + u][:, 0:128], op=ALU.add),
                                         reads=["pb%d" % (6 + u), "acc"], writes=["acc"])

                            units = [(g, n) for g in range(3) for n in range(16)]
                            front(units[0][0], units[0][1], 0)
                            for ui, (g, n) in enumerate(units):
                                if ui + 1 < len(units):
                                    front(units[ui + 1][0], units[ui + 1][1], (ui + 1) % 2)
                                back(g, n, ui % 2)
                                uctr += 1
                                if uctr % 3 == 0:
                                    prep_chunk()
                            for tg in range(4):
                                S.op("dve", lambda e, tg=tg: e.reciprocal(out=rc[:, :], in_=acc[64:128, tg * 512:(tg + 1) * 512]), reads=["acc"], writes=["rc"])
                                S.op("dve", lambda e, tg=tg, h=h: e.tensor_tensor(out=OT[:, h, tg * 512:(tg + 1) * 512], in0=acc[0:64, tg * 512:(tg + 1) * 512], in1=rc[:, :], op=ALU.mult),
                                     reads=["acc", "rc"], writes=["OT.%d" % h])

                while prep_c[0] < 128 and STAGE >= 6:
                    prep_chunk()
                OT_all = ["OT.%d" % h for h in range(8)] + ["OTs"]
                if dbg_ot is not None:
                    S.dma(lambda e: e.dma_start(out=dbg_ot, in_=OT[:]), "dbgot", reads=OT_all, final=True)
                with scope() as pa2:
                    byT = sb([128, 8, NT], BF16, pa2)
                    zT = sb([128, 8, NT], BF16, pa2)
                    if STAGE >= 5:
                        with scope() as s5:
                            extp = sb([128, SEQ + 2], F32, s5)
                            exts = sb([128, 16, 6], F32, s5)
                            stt = sb([32, D], F32, s5)
                            hv = sb([128, 512], F32, s5)
                            yb = sb([128, 512], F32, s5)
                            cvT = sb([128, 8, 34], F32, s5)
                            cvtok = sb([34, D], F32, s5)
                            S.dma(lambda e: e.dma_start(out=stt[:], in_=stconv), "const", writes=["stt"])
                            S.op("pool", lambda e: e.memset(extp[:, 0:2], 0.0), writes=["extp"])
                            for cc in range(8):
                                wt, wtok = load_w([(w_in[:, j * 1024 + cc * 128:j * 1024 + (cc + 1) * 128], 8) for j in range(3)])
                                S.op("pe", lambda e, cc=cc: e.transpose(out=pb[5][:, 0:32], in_=stt[:, cc * 128:(cc + 1) * 128], identity=identf[0:32, 0:32]),
                                     reads=["stt", "identf"], writes=["pb5"])
                                S.op("act", lambda e: e.activation(out=exts[:, :, 0:2], in_=pb[5][:, 0:32].rearrange("p (b r) -> p b r", r=2), func=AF.Copy),
                                     reads=["pb5"], writes=["exts"])
                                for gi, (t0, n) in enumerate(tgroups):
                                    for j in range(3):
                                        for kc in range(8):
                                            S.op("pe", lambda e, j=j, kc=kc, t0=t0, n=n, wt=wt: e.matmul(
                                                pb[2 + j][:, 0:n], lhsT=wt[:, kc, j * 128:(j + 1) * 128], rhs=hT[:, kc, t0:t0 + n], start=(kc == 0), stop=(kc == 7)),
                                                reads=[wtok] + hT_all, writes=["pb%d" % (2 + j)])
                                    S.op("act", lambda e, n=n: e.activation(out=hv[:, 0:n], in_=pb[4][:, 0:n], func=AF.Copy), reads=["pb4"], writes=["hv"])
                                    if t0 < SEQ:
                                        uo = extp[:, 2 + t0:2 + t0 + n]
                                        e0, e1, e2 = extp[:, t0:t0 + n], extp[:, t0 + 1:t0 + 1 + n], extp[:, t0 + 2:t0 + 2 + n]
                                        hvv, cps, bps, yv, byv = hv[:, 0:n], pb[3][:, 0:n], pb[2][:, 0:n], yb[:, 0:n], byT[:, cc, t0:t0 + n]
                                        etok = "extp"
                                    else:
                                        uo = exts[:, :, 2:6]
                                        e0, e1, e2 = exts[:, :, 0:4], exts[:, :, 1:5], exts[:, :, 2:6]
                                        v4 = lambda ap: ap.rearrange("p (b t) -> p b t", t=4)
                                        hvv, cps, bps, yv, byv = v4(hv[:, 0:n]), v4(pb[3][:, 0:n]), v4(pb[2][:, 0:n]), v4(yb[:, 0:n]), v4(byT[:, cc, t0:t0 + n])
                                        etok = "exts"
                                    S.op("dve", lambda e, uo=uo, cps=cps, hvv=hvv: e.tensor_tensor(out=uo, in0=cps, in1=hvv, op=ALU.mult),
                                         reads=["pb3", "hv", etok], writes=[etok])
                                    S.op("dve", lambda e, yv=yv, e0=e0, cc=cc: e.tensor_scalar(out=yv, in0=e0, scalar1=cwt[:, cc, 0:1], scalar2=None, op0=ALU.mult),
                                         reads=[etok, "cwt"], writes=["yb"])
                                    S.op("dve", lambda e, yv=yv, e1=e1, cc=cc: e.scalar_tensor_tensor(out=yv, in0=e1, scalar=cwt[:, cc, 1:2], in1=yv, op0=ALU.mult, op1=ALU.add),
                                         reads=[etok, "cwt", "yb"], writes=["yb"])
                                    S.op("dve", lambda e, yv=yv, e2=e2, cc=cc: e.scalar_tensor_tensor(out=yv, in0=e2, scalar=cwt[:, cc, 2:3], in1=yv, op0=ALU.mult, op1=ALU.add),
                                         reads=[etok, "cwt", "yb"], writes=["yb"])
                                    S.op("dve", lambda e, byv=byv, bps=bps, yv=yv: e.tensor_tensor(out=byv, in0=bps, in1=yv, op=ALU.mult),
                                         reads=["pb2", "yb"], writes=["byT.%d.%d" % (cc, gi)])
                                S.op("act", lambda e, cc=cc: e.activation(out=cvT[:, cc, 0:2], in_=extp[:, SEQ:SEQ + 2], func=AF.Copy), reads=["extp"], writes=["cvT.%d" % cc])
                                S.op("act", lambda e, cc=cc: e.activation(out=cvT[:, cc, 2:34].rearrange("p (b r) -> p b r", r=2), in_=exts[:, :, 4:6], func=AF.Copy),
                                     reads=["exts", "cvT.%d" % cc], writes=["cvT.%d" % cc])
                            for cc in range(8):
                                S.op("pe", lambda e, cc=cc: e.transpose(out=pb[5][0:34, 0:128], in_=cvT[:, cc, :], identity=identf[:, :]),
                                     reads=["cvT.%d" % cc, "identf"], writes=["pb5"])
                                S.op("act", lambda e, cc=cc: e.activation(out=cvtok[:, cc * 128:(cc + 1) * 128], in_=pb[5][0:34, 0:128], func=AF.Copy),
                                     reads=["pb5"], writes=["cvtok.%d" % cc])
                            cv_all = ["cvtok.%d" % cc for cc in range(8)]
                            S.dma(lambda e: e.dma_start(out=conv_p, in_=cvtok[0:2, :]), "cvst", reads=cv_all, final=True)
                            S.dma(lambda e: e.dma_start(out=conv_s, in_=cvtok[2:34, :]), "cvst", reads=cv_all, final=True)

                    if STAGE >= 5:
                        with scope() as s6:
                            sga = sb([128, 512], F32, s6)
                            sgb = sb([128, 512], F32, s6)
                            by_all = ["byT.%d.%d" % (cc, gi) for cc in range(8) for gi in range(5)]
                            for cc in range(8):
                                wt, wtok = load_w([(w_out_a[:, cc * 128:(cc + 1) * 128], 8),
                                                   (w_in[:, 7680 + cc * 128:7680 + (cc + 1) * 128], 8),
                                                   (w_in[:, 8704 + cc * 128:8704 + (cc + 1) * 128], 8),
                                                   (w_out_b[:, cc * 128:(cc + 1) * 128], 8)])
                                for gi, (t0, n) in enumerate(tgroups):
                                    for kc in range(8):
                                        S.op("pe", lambda e, kc=kc, t0=t0, n=n, wt=wt: e.matmul(pb[2][:, 0:n], lhsT=wt[:, kc, 0:128], rhs=byT[:, kc, t0:t0 + n], start=(kc == 0), stop=(kc == 7)),
                                             reads=[wtok] + by_all, writes=["pb2"])
                                    for kc in range(8):
                                        S.op("pe", lambda e, kc=kc, t0=t0, n=n, wt=wt: e.matmul(pb[3][:, 0:n], lhsT=wt[0:64, kc, 384:512], rhs=OT[:, kc, t0:t0 + n], start=(kc == 0), stop=(kc == 7)),
                                             reads=[wtok] + OT_all, writes=["pb3"])
                                    for j in range(2):
                                        for kc in range(8):
                                            S.op("pe", lambda e, j=j, kc=kc, t0=t0, n=n, wt=wt: e.matmul(pb[4 + j][:, 0:n], lhsT=wt[:, kc, 128 + j * 128:256 + j * 128], rhs=hT[:, kc, t0:t0 + n], start=(kc == 0), stop=(kc == 7)),
                                                 reads=[wtok] + hT_all, writes=["pb%d" % (4 + j)])
                                    S.op("act", lambda e, n=n: e.activation(out=sga[:, 0:n], in_=pb[4][:, 0:n], func=AF.Sigmoid), reads=["pb4"], writes=["sga"])
                                    S.op("act", lambda e, n=n: e.activation(out=sgb[:, 0:n], in_=pb[5][:, 0:n], func=AF.Sigmoid), reads=["pb5"], writes=["sgb"])
                                    S.op("dve", lambda e, n=n: e.tensor_tensor(out=sga[:, 0:n], in0=sga[:, 0:n], in1=pb[2][:, 0:n], op=ALU.mult), reads=["sga", "pb2"], writes=["sga"])
                                    S.op("dve", lambda e, n=n: e.tensor_tensor(out=sgb[:, 0:n], in0=sgb[:, 0:n], in1=pb[3][:, 0:n], op=ALU.mult), reads=["sgb", "pb3"], writes=["sgb"])
                                    S.op("dve", lambda e, n=n, cc=cc, t0=t0: e.tensor_tensor(out=zT[:, cc, t0:t0 + n], in0=sga[:, 0:n], in1=sgb[:, 0:n], op=ALU.add),
                                         reads=["sga", "sgb"], writes=["zT.%d.%d" % (cc, gi)])

                    if STAGE >= 5:
                        with scope() as s7:
                            wo = sb([128, 8, D], BF16, s7)
                            x1t = [sb([128, D], F32, s7) for _ in range(2)]
                            z_all = ["zT.%d.%d" % (cc, gi) for cc in range(8) for gi in range(5)]
                            for half in range(2):
                                wt, wtok = load_w([(w_o[:, half * 512:(half + 1) * 512], 8)])
                                S.op("pool", lambda e, wt=wt, half=half: e.tensor_copy(out=wo[:, :, half * 512:(half + 1) * 512], in_=wt[:, :, :]), reads=[wtok], writes=["wo.%d" % half])
                            for i, (t0, n) in enumerate(tiles):
                                k = i % 2
                                S.dma(lambda e, k=k, t0=t0, n=n: e.dma_start(out=x1t[k][0:n, :], in_=x_src(t0, n)), "x1ld%d" % k, writes=["x1t%d" % k])
                                for half in range(2):
                                    for kc in range(8):
                                        S.op("pe", lambda e, half=half, kc=kc, t0=t0, n=n, k=k: e.matmul(pb[2 + 2 * k + half][0:n, :], lhsT=zT[:, kc, t0:t0 + n], rhs=wo[:, kc, half * 512:(half + 1) * 512], start=(kc == 0), stop=(kc == 7)),
                                             reads=z_all + ["wo.%d" % half], writes=["pb%d" % (2 + 2 * k + half)])
                                    S.op("dve", lambda e, half=half, n=n, k=k: e.tensor_tensor(out=x1t[k][0:n, half * 512:(half + 1) * 512], in0=x1t[k][0:n, half * 512:(half + 1) * 512], in1=pb[2 + 2 * k + half][0:n, :], op=ALU.add),
                                         reads=["x1t%d" % k, "pb%d" % (2 + 2 * k + half)], writes=["x1t%d" % k])
                                S.dma(lambda e, k=k, t0=t0, n=n: e.dma_start(out=scr_x1[t0:t0 + n, :], in_=x1t[k][0:n, :]), "x1st%d" % k, reads=["x1t%d" % k], writes=["x1dram.%d" % i])

        if STAGE >= 6:
            h2T = hT
            h2_all = ["h2T.%d" % i for i in range(17)]
            i1T = sb([128, NT], F32)
            i2T = sb([128, NT], F32)
            gTt = sb([128, NT], F32)

            def x1_src(t0, n):
                return scr_x1[t0:t0 + n, :]

            with scope() as b1:
              if not os.environ.get("MK_NOB1"):
                xts = [sb([128, D], F32, b1) for _ in range(2)]
                hb = sb([128, D], BF16, b1)
                junk = sb([128, D], BF16, b1)
                ss = sb([128, 4], F32, b1)
                rmsnorm_T(x1_src, g2t, "g2t", h2T, "h2T", xts, hb, junk, ss, "n2")
                wqb = sb([128, 8, 2048], BF16, b1)
                for pc in range(4 if B1CUT >= 1 else 0):
                    wt, wtok = load_w([(wq[:, pc * 512:(pc + 1) * 512], 8)])
                    S.op("pool", lambda e, wt=wt, pc=pc: e.tensor_copy(out=wqb[:, :, pc * 512:(pc + 1) * 512], in_=wt[:, :, :]), reads=[wtok], writes=["wqb.%d" % pc])
                wq_all = ["wqb.%d" % pc for pc in range(4)]
                kyf = sb([128, 16, 128], F32, b1)
                kyb = sb([128, 16, 128], BF16, b1)
                S.dma(lambda e: e.dma_start(out=kyf[:], in_=keysT), "const", writes=["kyf"])
                S.op("pool", lambda e: e.tensor_copy(out=kyb[:], in_=kyf[:]), reads=["kyf"], writes=["kyb"])
                for g in range(3):
                    W = GROUPS[g][0]
                    nb = 16 if W == 2048 else (4 if W == 512 else 1)
                    per = 16 // nb
                    for i in range(nb):
                        src = caches[g][i * per:(i + 1) * per, 4:W].rearrange("b r k s d -> b (r k s d)")
                        dst = kvs[g][i * per:(i + 1) * per, 0:W - 4].rearrange("b r k s d -> b (r k s d)")
                        S.dma(lambda e, src=src, dst=dst: e.dma_start(out=dst, in_=src), "cpy%d" % g, eng="sp", final=True)

                qTg = sb([128, 16, 512], BF16, b1)
                scs = sb([128, 16, 128], F32, b1)
                S.op("pool", lambda e: e.memset(qTg[:], 0.0), writes=["qTg.%d" % ch for ch in range(16)])
                v12 = sb([128, 16, 16], F32, b1)
                i12 = sb([128, 16, 16], U32, b1)
                i12f = sb([128, 16, 16], F32, b1)
                wk = sb([128, 256], F32, b1)
                cand = sb([128, 8, 256], F32, b1)
                svt = sb([128, 8, 16], F32, b1)
                pos = sb([128, 8, 16], U32, b1)
                pa_ = sb([128, 8, 16], U32, b1)
                pb_ = sb([128, 8, 16], U32, b1)
                paf = sb([128, 8, 16], F32, b1)
                pbf = sb([128, 8, 16], F32, b1)
                oh = sb([128, 8, 16, 16], F32, b1)
                i1f = sb([128, 8, 16], F32, b1)
                i2f = sb([128, 8, 16], F32, b1)
                gte = sb([128, 8, 16], F32, b1)
                zs = sb([128, 8], F32, b1)
                c4 = sb([128, 2], U32, b1)
                S.op("dve", lambda e: e.memset(c4[:, 0:1], 4), writes=["c4a"])
                S.op("dve", lambda e: e.memset(c4[:, 1:2], 15), writes=["c4b"])
                tgroups_b1 = [(i * 512, 512) for i in range(4)] + [(NT - 128, 128)]
                for gi, (t0g, ng) in enumerate(tgroups_b1 if (B1CUT >= 1 and B1SUB >= 2) else []):
                    for ch in range(16):
                        bk = 2 + ch % 2
                        for kc in range(8):
                            S.op("pe", lambda e, bk=bk, ch=ch, kc=kc, t0g=t0g, ng=ng: e.matmul(pb[bk][:, 0:ng], lhsT=wqb[:, kc, ch * 128:(ch + 1) * 128], rhs=h2T[:, kc, t0g:t0g + ng], start=(kc == 0), stop=(kc == 7)),
                                 reads=wq_all + h2_all, writes=["pb%d" % bk])
                        S.op("act", lambda e, bk=bk, ch=ch, ng=ng: e.activation(out=qTg[:, ch, 0:ng], in_=pb[bk][:, 0:ng], func=AF.Copy), reads=["pb%d" % bk], writes=["qTg.%d" % ch])
                    qT_all = ["qTg.%d" % ch for ch in range(16)]
                    for tl in range((ng + 127) // 128):
                        n = min(128, ng - tl * 128)
                        t0 = t0g + tl * 128
                        if B1SUB < 3:
                            continue
                        if os.environ.get("MK_GI") and str(gi) not in os.environ["MK_GI"]:
                            continue
                        for ch in range(16):
                            S.op("pe", lambda e, ch=ch, tl=tl, n=n: e.matmul(pb[4 + ch // 4][:, (ch % 4) * 128:(ch % 4 + 1) * 128], lhsT=qTg[:, ch, tl * 128:tl * 128 + 128], rhs=kyb[:, ch, :], start=True, stop=True),
                                 reads=qT_all + ["kyb"], writes=["sc%d" % ch])
                        if B1CUT < 2:
                            continue
                        for bq in range(4):
                            S.op("act", lambda e, bq=bq: e.activation(out=scs[:, bq * 4:(bq + 1) * 4, :], in_=pb[4 + bq][:, :].rearrange("p (c k) -> p c k", c=4), func=AF.Copy),
                                 reads=["sc%d" % (bq * 4 + j) for j in range(4)], writes=["sc%d" % (bq * 4 + j) for j in range(4)] + ["scs%d" % bq])
                        for ch in range(16):
                            sv_ = scs[0:n, ch, :]
                            S.op("dve", lambda e, sv_=sv_, ch=ch, n=n: e.max(out=v12[0:n, ch, 0:8], in_=sv_), reads=["scs%d" % (ch // 4)], writes=["v12a"])
                            S.op("dve", lambda e, sv_=sv_, ch=ch, n=n: e.max_index(out=i12[0:n, ch, 0:8], in_max=v12[0:n, ch, 0:8], in_values=sv_), reads=["scs%d" % (ch // 4), "v12a"], writes=["i12a"])
                            S.op("dve", lambda e, sv_=sv_, ch=ch, n=n: e.match_replace(out=wk[0:n, 0:128], in_to_replace=v12[0:n, ch, 0:8], in_values=sv_, imm_value=-1e30), reads=["scs%d" % (ch // 4), "v12a"], writes=["wk"])
                            S.op("dve", lambda e, ch=ch, n=n: e.max(out=v12[0:n, ch, 8:16], in_=wk[0:n, 0:128]), reads=["wk"], writes=["v12b"])
                            S.op("dve", lambda e, ch=ch, n=n: e.max_index(out=i12[0:n, ch, 8:16], in_max=v12[0:n, ch, 8:16], in_values=wk[0:n, 0:128]), reads=["wk", "v12b"], writes=["i12.%d" % ch, "v12.%d" % ch])
                        if B1CUT < 3:
                            continue
                        v_all = ["v12.%d" % ch for ch in range(16)]
                        i_all = ["i12.%d" % ch for ch in range(16)]
                        v12v = v12[:, :, :].rearrange("p (h f) k -> p h f k", f=2)
                        S.op("dve", lambda e, n=n, v12v=v12v: e.tensor_tensor(
                            out=cand[0:n].rearrange("p h (a b) -> p h a b", a=16), in0=v12v[0:n, :, 0, :].unsqueeze(3).broadcast_to([n, 8, 16, 16]),
                            in1=v12v[0:n, :, 1, :].unsqueeze(2).broadcast_to([n, 8, 16, 16]), op=ALU.add), reads=v_all, writes=["cand"])
                        S.op("dve", lambda e, n=n: e.tensor_copy(out=i12f[0:n], in_=i12[0:n]), reads=i_all, writes=["i12f"])
                        for h in range(8):
                            S.op("dve", lambda e, h=h, n=n: e.max(out=svt[0:n, h, 0:8], in_=cand[0:n, h, :]), reads=["cand"], writes=["sva"])
                            S.op("dve", lambda e, h=h, n=n: e.max_index(out=pos[0:n, h, 0:8], in_max=svt[0:n, h, 0:8], in_values=cand[0:n, h, :]), reads=["cand", "sva"], writes=["posa"])
                            S.op("dve", lambda e, h=h, n=n: e.match_replace(out=wk[0:n, :], in_to_replace=svt[0:n, h, 0:8], in_values=cand[0:n, h, :], imm_value=-1e30), reads=["cand", "sva"], writes=["wk"])
                            S.op("dve", lambda e, h=h, n=n: e.max(out=svt[0:n, h, 8:16], in_=wk[0:n, :]), reads=["wk"], writes=["svb"])
                            S.op("dve", lambda e, h=h, n=n: e.max_index(out=pos[0:n, h, 8:16], in_max=svt[0:n, h, 8:16], in_values=wk[0:n, :]), reads=["wk", "svb"], writes=["pos.%d" % h, "sv.%d" % h])
                        if B1CUT < 4:
                            continue
                        p_all = ["pos.%d" % h for h in range(8)]
                        s_all = ["sv.%d" % h for h in range(8)]
                        S.op("dve", lambda e, n=n: e.tensor_scalar(out=pa_[0:n], in0=pos[0:n], scalar1=c4[0:n, 0:1], scalar2=None, op0=ALU.logical_shift_right), reads=p_all + ["c4a"], writes=["pa_"])
                        S.op("dve", lambda e, n=n: e.tensor_scalar(out=pb_[0:n], in0=pos[0:n], scalar1=c4[0:n, 1:2], scalar2=None, op0=ALU.bitwise_and), reads=p_all + ["c4b"], writes=["pb_"])
                        S.op("dve", lambda e, n=n: e.tensor_copy(out=paf[0:n], in_=pa_[0:n]), reads=["pa_"], writes=["paf"])
                        S.op("dve", lambda e, n=n: e.tensor_copy(out=pbf[0:n], in_=pb_[0:n]), reads=["pb_"], writes=["pbf"])
                        i12v = i12f[:, :, :].rearrange("p (h f) k -> p h f k", f=2)
                        for (pf, pfn, f, dst, dn) in ((paf, "paf", 0, i1f, "i1f"), (pbf, "pbf", 1, i2f, "i2f")):
                            S.op("dve", lambda e, n=n, pf=pf: e.tensor_tensor(
                                out=oh[0:n], in0=pf[0:n].unsqueeze(3).broadcast_to([n, 8, 16, 16]),
                                in1=iota[0:n, 0:16].unsqueeze(1).unsqueeze(1).broadcast_to([n, 8, 16, 16]), op=ALU.is_equal), reads=[pfn, "iota"], writes=["oh"])
                            S.op("dve", lambda e, n=n, f=f, i12v=i12v: e.tensor_tensor(
                                out=oh[0:n], in0=oh[0:n], in1=i12v[0:n, :, f, :].unsqueeze(2).broadcast_to([n, 8, 16, 16]), op=ALU.mult), reads=["oh", "i12f"], writes=["oh"])
                            S.op("dve", lambda e, n=n, dst=dst: e.tensor_reduce(out=dst[0:n], in_=oh[0:n], axis=AX.X, op=ALU.add), reads=["oh"], writes=[dn])
                        if B1CUT < 5:
                            continue
                        S.op("dve", lambda e, n=n: e.tensor_tensor(out=gte[0:n], in0=svt[0:n], in1=svt[0:n, :, 0:1].broadcast_to([n, 8, 16]), op=ALU.subtract), reads=s_all, writes=["gte"])
                        S.op("act", lambda e, n=n: e.activation(out=gte[0:n], in_=gte[0:n], func=AF.Exp), reads=["gte"], writes=["gte"])
                        S.op("dve", lambda e, n=n: e.tensor_reduce(out=zs[0:n], in_=gte[0:n], axis=AX.X, op=ALU.add), reads=["gte"], writes=["zs"])
                        S.op("dve", lambda e, n=n: e.reciprocal(out=zs[0:n], in_=zs[0:n]), reads=["zs"], writes=["zs"])
                        S.op("dve", lambda e, n=n: e.tensor_tensor(out=gte[0:n], in0=gte[0:n], in1=zs[0:n, :].unsqueeze(2).broadcast_to([n, 8, 16]), op=ALU.mult), reads=["gte", "zs"], writes=["gte"])
                        for (srcx, sn, dstx, dn) in ((i1f, "i1f", i1T, "i1T"), (i2f, "i2f", i2T, "i2T"), (gte, "gte", gTt, "gTt")):
                            S.op("pe", lambda e, srcx=srcx, n=n: e.transpose(out=pb[2][:, 0:n], in_=srcx[0:n].rearrange("p h k -> p (h k)"), identity=identf[0:n, 0:n]),
                                 reads=[sn, "identf"], writes=["pb2"])
                            S.op("act", lambda e, dstx=dstx, t0=t0, n=n: e.activation(out=dstx[:, t0:t0 + n], in_=pb[2][:, 0:n], func=AF.Copy), reads=["pb2"], writes=[dn])

            with scope() as b2:
              if STAGE >= 7:
                  TS = 384
                  Wsb = sb([128, TS, 128], BF16, b2)
                  wb0 = wbf[0][:].rearrange("p a b -> p (a b)")
                  wb1 = wbf[1][:].rearrange("p a b -> p (a b)")
                  E1 = wb0[:, 0:2048].rearrange("p (t i) -> p t i", t=16)
                  E2 = wb0[:, 2048:4096].rearrange("p (t i) -> p t i", t=16)
                  G2 = wb1[:, 0:2048].rearrange("p (t i) -> p t i", t=16)
                  NSLOT = 4
                  wsb16 = wst[:].bitcast(BF16).rearrange("p a b -> p (a b)")
                  utb = [wsb16[:, sl * 2048:sl * 2048 + 1024].rearrange("p (k e) -> p k e", k=8) for sl in range(NSLOT)]
                  vtb = [wsb16[:, sl * 2048 + 1024:(sl + 1) * 2048] for sl in range(NSLOT)]
                  ge = [sb([128, TS], F32, b2) for _ in range(2)]
                  WA = [sb([128, TS], BF16, b2) for _ in range(2)]
                  ysb = [sb([128, D], F32, b2) for _ in range(2)]
                  x1r = [sb([128, D], F32, b2)] * 2
                  fg = sb([128, D], F32, b2)
                  junk2 = wb1[:, 2048:3072]
                  ss2 = sb([128, 4], F32, b2)
                  S.dma(lambda e: e.dma_start(out=fg[:], in_=fgb.broadcast_to([128, D])), "const", writes=["fg"])
                  stiles = [(i * TS, TS) for i in range(5)] + [(5 * TS, NT - 5 * TS)]
                  lctr = 0
                  fctr = 0
                  for (s0, T) in stiles:
                      for sbk in range(T // 16):
                          tt0 = sbk * 16
                          S.op("dve", lambda e, s0=s0, tt0=tt0: e.tensor_tensor(
                              out=E1, in0=iota[:, :].unsqueeze(1).broadcast_to([128, 16, 128]),
                              in1=i1T[:, s0 + tt0:s0 + tt0 + 16].unsqueeze(2).broadcast_to([128, 16, 128]), op=ALU.is_equal), reads=["iota", "i1T"], writes=["E1"])
                          S.op("dve", lambda e, s0=s0, tt0=tt0: e.tensor_tensor(
                              out=E2, in0=iota[:, :].unsqueeze(1).broadcast_to([128, 16, 128]),
                              in1=i2T[:, s0 + tt0:s0 + tt0 + 16].unsqueeze(2).broadcast_to([128, 16, 128]), op=ALU.is_equal), reads=["iota", "i2T"], writes=["E2"])
                          S.op("dve", lambda e, s0=s0, tt0=tt0: e.tensor_tensor(
                              out=G2, in0=E2, in1=gTt[:, s0 + tt0:s0 + tt0 + 16].unsqueeze(2).broadcast_to([128, 16, 128]), op=ALU.mult), reads=["E2", "gTt"], writes=["G2"])
                          for q4 in range(4):
                              bk = 6 + q4 % 2
                              for j in range(4):
                                  tl = q4 * 4 + j
                                  S.op("pe", lambda e, bk=bk, j=j, tl=tl: e.matmul(pb[bk][:, j * 128:(j + 1) * 128], lhsT=G2[:, tl, :], rhs=E1[:, tl, :], start=True, stop=True),
                                       reads=["G2", "E1"], writes=["pb%d" % bk])
                              S.op("act", lambda e, bk=bk, tt0=tt0, q4=q4: e.activation(out=Wsb[:, tt0 + q4 * 4:tt0 + q4 * 4 + 4, :], in_=pb[bk][:, :].rearrange("p (t i) -> p t i", t=4), func=AF.Copy),
                                   reads=["pb%d" % bk], writes=["Wsb"])
                      ntile = (T + 127) // 128
                      def ld(c):
                          sl = c % NSLOT
                          S.dma(lambda e, c=c, sl=sl: e.dma_start(out=utb[sl], in_=scr_ut[c]), "utb%d" % sl, reads=["scr_ut%d" % c], writes=["utb%d" % sl])
                          S.dma(lambda e, c=c, sl=sl: e.dma_start(out=vtb[sl], in_=scr_v[c]), "vtb%d" % sl, reads=["scr_v%d" % c], writes=["vtb%d" % sl])

                      def actp(c):
                          sl = c % NSLOT
                          k = c % 2
                          for kc in range(8):
                              S.op("pe", lambda e, k=k, kc=kc, sl=sl, s0=s0, T=T: e.matmul(pb[6 + k][:, 0:T], lhsT=utb[sl][:, kc, :], rhs=h2T[:, kc, s0:s0 + T], start=(kc == 0), stop=(kc == 7)),
                                   reads=["utb%d" % sl] + h2_all, writes=["pb%d" % (6 + k)])
                          S.op("act", lambda e, k=k, T=T: e.activation(out=ge[k][:, 0:T], in_=pb[6 + k][:, 0:T], func=AF.Gelu), reads=["pb%d" % (6 + k)], writes=["ge%d" % k])
                          S.op("dve", lambda e, k=k, T=T, c=c: e.tensor_tensor(out=WA[k][:, 0:T], in0=ge[k][:, 0:T], in1=Wsb[:, 0:T, c], op=ALU.mult), reads=["ge%d" % k, "Wsb"], writes=["WA%d" % k])

                      def vp(c):
                          sl = c % NSLOT
                          k = c % 2
                          for ti in range(ntile):
                              n = min(128, T - ti * 128)
                              for half in range(2):
                                  S.op("pe", lambda e, ti=ti, half=half, n=n, k=k, sl=sl, c=c: e.matmul(pb[2 * ti + half][0:n, :], lhsT=WA[k][:, ti * 128:ti * 128 + n], rhs=vtb[sl][:, half * 512:(half + 1) * 512], start=(c == 0), stop=(c == 127)),
                                       reads=["WA%d" % k, "vtb%d" % sl], writes=["pb%d" % (2 * ti + half)])

                      for c in range(NSLOT - 1):
                          ld(c)
                      actp(0)
                      for c in range(128):
                          if c + NSLOT - 1 < 128:
                              ld(c + NSLOT - 1)
                          if c + 1 < 128:
                              actp(c + 1)
                          vp(c)
                      for ti in range(ntile):
                          n = min(128, T - ti * 128)
                          t0 = s0 + ti * 128
                          k = fctr % 2
                          fctr += 1
                          tix = t0 // 128
                          S.dma(lambda e, k=k, t0=t0, n=n: e.dma_start(out=x1r[k][0:n, :], in_=scr_x1[t0:t0 + n, :]), "x1r", reads=["x1dram.%d" % tix], writes=["x1r"])
                          for half in range(2):
                              S.op("dve", lambda e, k=k, n=n, ti=ti, half=half: e.tensor_tensor(out=ysb[k][0:n, half * 512:(half + 1) * 512], in0=x1r[k][0:n, half * 512:(half + 1) * 512], in1=pb[2 * ti + half][0:n, :], op=ALU.add),
                                   reads=["x1r", "pb%d" % (2 * ti + half)], writes=["ysb%d" % k])
                          S.op("dve", lambda e: e.memset(ss2[:, 0:1], 0.0), writes=["ss2"])
                          S.op("act", lambda e, k=k, n=n: e.activation(out=junk2[0:n, :], in_=ysb[k][0:n, :], func=AF.Square, accum_out=ss2[0:n, 0:1]), reads=["ysb%d" % k, "ss2"], writes=["junk2", "ss2"])
                          S.op("act", lambda e, n=n: e.activation(out=ss2[0:n, 1:2], in_=ss2[0:n, 0:1], func=AF.Sqrt, bias=epst[0:n, :], scale=1.0 / D), reads=["ss2", "eps"], writes=["ss21"])
                          S.op("dve", lambda e, n=n: e.reciprocal(out=ss2[0:n, 2:3], in_=ss2[0:n, 1:2]), reads=["ss21"], writes=["ss22"])
                          S.op("dve", lambda e, k=k, n=n: e.scalar_tensor_tensor(out=ysb[k][0:n, :], in0=ysb[k][0:n, :], scalar=ss2[0:n, 2:3], in1=fg[0:n, :], op0=ALU.mult, op1=ALU.mult),
                               reads=["ysb%d" % k, "ss22", "fg"], writes=["ysb%d" % k])
                          dst = y_p[t0:t0 + n, :] if t0 < SEQ else y_s[:, :]
                          S.dma(lambda e, k=k, n=n, dst=dst: e.dma_start(out=dst, in_=ysb[k][0:n, :]), "yst%d" % k, reads=["ysb%d" % k], final=True)

        with nc.Block() as block:
            S.emit(block)
    return nc


_PROG = None


def kernel(x_prompt, x_sample, cache_kv_w128, cache_kv_w512, cache_kv_w2048, state_conv,
           norm1_g, w_in, conv_w, w_out_a, w_out_b, w_o, norm2_g,
           peer_wq, peer_keys, peer_u, peer_v, final_g):
    global _PROG
    if _PROG is None:
        _PROG = build_program()
    nc = _PROG
    f = lambda a: np.ascontiguousarray(np.asarray(a, dtype=np.float32))
    consts = _consts()
    shared = dict(
        g1T=f(np.asarray(norm1_g)[0].reshape(8, 128).T),
        g2T=f(np.asarray(norm2_g)[0].reshape(8, 128).T),
        w_in=f(np.asarray(w_in)[0]),
        convw=f(np.asarray(conv_w)[0].reshape(3, 8, 128).transpose(2, 1, 0)),
        w_out_a=f(np.asarray(w_out_a)[0]),
        w_out_b=f(np.asarray(w_out_b)[0]),
        w_o=f(np.asarray(w_o)[0]),
        wq=f(np.asarray(peer_wq)[0]),
        keysT=f(np.asarray(peer_keys)[0].transpose(1, 0, 2, 3).reshape(16, 128, 128).transpose(2, 0, 1)),
        uT=f(np.asarray(peer_u)[0].T),
        vtab=f(np.asarray(peer_v)[0]),
        fgb=f(np.asarray(final_g).reshape(1, D)),
    )
    shared.update(consts)
    caches = [np.asarray(cache_kv_w128)[0], np.asarray(cache_kv_w512)[0], np.asarray(cache_kv_w2048)[0]]
    xpr = np.asarray(x_prompt)
    xsa = np.asarray(x_sample)
    stc = np.asarray(state_conv)[0]
    in_maps = []
    for c in range(NCORES):
        m = dict(shared)
        m["xp"] = f(xpr[c])
        m["xs"] = f(xsa[c * 16:(c + 1) * 16].reshape(NS, D))
        for g in range(3):
            m["cache%d" % g] = f(caches[g][c * 16:(c + 1) * 16])
        m["stconv"] = f(stc[c * 16:(c + 1) * 16].reshape(32, D))
        in_maps.append(m)
    ncr = int(os.environ.get("MK_CORES", str(NCORES)))
    res = run_bass_kernel_spmd(nc, in_maps[0:ncr], core_ids=list(range(ncr)))
    R = list(res.results)
    while len(R) < NCORES:
        R.append(R[0])
    y_prompt = np.stack([R[c]["y_p"] for c in range(NCORES)], 0)
    y_sample = np.concatenate([R[c]["y_s"].reshape(16, 4, D) for c in range(NCORES)], 0)
    kvp = [np.stack([R[c]["kvp%d" % g] for c in range(NCORES)], 0)[None] for g in range(3)]
    convp = np.stack([R[c]["conv_p"] for c in range(NCORES)], 0)[None]
    kvs = [np.concatenate([R[c]["kvs%d" % g] for c in range(NCORES)], 0)[None] for g in range(3)]
    convs = np.concatenate([R[c]["conv_s"].reshape(16, 2, D) for c in range(NCORES)], 0)[None]
    return (y_prompt.astype(np.float32), y_sample.astype(np.float32), kvp[0], kvp[1], kvp[2], convp,
            kvs[0], kvs[1], kvs[2], convs)
```

```python
import os
import numpy as np
from contextlib import ExitStack, contextmanager
import concourse.bass as bass
import concourse.mybir as mybir
from concourse.bass_utils import run_bass_kernel_spmd
import ml_dtypes

F32 = mybir.dt.float32
BF16 = mybir.dt.bfloat16
U32 = mybir.dt.uint32
ALU = mybir.AluOpType
AF = mybir.ActivationFunctionType
AX = mybir.AxisListType

NCORES = 8
D = 1024
SEQ = 2048
NS = 64
NT = SEQ + NS
PROJ = 9728
GROUPS = ((128, 1), (512, 4), (2048, 16))
EPS = 1e-6
NEXP = 16384
ENGS = ("pe", "act", "dve", "pool", "sp")
STAGE = int(os.environ.get("MK_STAGE", "99"))
B1CUT = int(os.environ.get("MK_B1CUT", "99"))
CPY_ENG = os.environ.get("MK_CPYENG", "pe")
B1SUB = int(os.environ.get("MK_B1SUB", "99"))


class Sched:
    def __init__(self, nc, n_dma_sems=120):
        self.nc = nc
        self.streams = {e: [] for e in ENGS}
        self.count = {e: 0 for e in ENGS}
        self.last_w = {}
        self.readers = {}
        self.waited = {e: {} for e in ENGS}
        self.psem = {}
        self.dsems = {}
        self.dcount = {}
        self.n_dma_sems = n_dma_sems
        self.final_events = []
        self.pending = {e: [] for e in ENGS}

    def barrier(self):
        evs = [(e, self.count[e]) for e in ("pe", "act", "dve", "pool") if self.count[e] > 0]
        evs += [(("d", k), v) for k, v in self.dcount.items() if v > 0 and not str(k).startswith("cpy")]
        for eng in ENGS:
            for s_, v in evs:
                if s_ == eng:
                    continue
                if self.waited[eng].get(s_, 0) >= v:
                    continue
                self.waited[eng][s_] = v
                self.pending[eng].append((s_, v))

    def alloc(self, stack):
        for e in ("pe", "act", "dve", "pool"):
            self.psem[e] = stack.enter_context(self.nc.semaphore("p_" + e))
        self.stack = stack

    def dsem(self, key):
        if key not in self.dsems:
            assert len(self.dsems) < self.n_dma_sems, "too many dma sems"
            self.dsems[key] = self.stack.enter_context(self.nc.semaphore("d%d" % len(self.dsems)))
            self.dcount[key] = 0
        return self.dsems[key]

    def _deps(self, eng, reads, writes):
        ev = {}

        def add(e):
            if e is None:
                return
            s, v = e
            if ev.get(s, 0) < v:
                ev[s] = v

        for t in reads:
            add(self.last_w.get(t))
        for t in writes:
            add(self.last_w.get(t))
            for r in self.readers.get(t, ()):
                add(r)
        out = []
        for s, v in ev.items():
            if eng == "pe" and s == "pe":
                continue
            if self.waited[eng].get(s, 0) >= v:
                continue
            self.waited[eng][s] = v
            out.append((s, v))
        return out

    def _commit(self, event, reads, writes):
        for t in reads:
            self.readers.setdefault(t, []).append(event)
        for t in writes:
            self.last_w[t] = event
            self.readers[t] = []

    def op(self, eng, fn, reads=(), writes=()):
        waits = self.pending[eng] + self._deps(eng, reads, writes)
        self.pending[eng] = []
        self.count[eng] += 1
        event = (eng, self.count[eng])
        self.streams[eng].append((waits, fn, ("p", eng)))
        self._commit(event, reads, writes)
        return event

    def dma(self, fn, key, reads=(), writes=(), eng="sp", final=False):
        self.dsem(key)
        waits = self.pending[eng] + self._deps(eng, reads, writes)
        self.pending[eng] = []
        self.dcount[key] += 16
        event = (("d", key), self.dcount[key])
        self.streams[eng].append((waits, fn, ("d", key)))
        self._commit(event, reads, writes)
        if final:
            self.final_events.append(event)
        return event

    def _sem(self, s):
        if isinstance(s, tuple):
            return self.dsems[s[1]]
        return self.psem[s]

    def emit(self, block):
        S = self
        fin = {}
        for s, v in self.final_events:
            if fin.get(s, 0) < v:
                fin[s] = v

        def run(engname, engobj, extra_final=False):
            for waits, fn, inc in S.streams[engname]:
                for s, v in waits:
                    engobj.wait_ge(S._sem(s), v)
                ins = fn(engobj)
                if inc[0] == "p":
                    ins.then_inc(S.psem[inc[1]], 1)
                else:
                    ins.then_inc(S.dsems[inc[1]], 16)
            if extra_final:
                for s, v in fin.items():
                    engobj.wait_ge(S._sem(s), v)

        @block.sync
        def _(e):
            run("sp", e, True)

        @block.tensor
        def _(e):
            run("pe", e)

        @block.scalar
        def _(e):
            run("act", e)

        @block.vector
        def _(e):
            run("dve", e)

        @block.gpsimd
        def _(e):
            run("pool", e)


def _consts():
    slopes = np.exp2(-8.0 * np.arange(1, 9, dtype=np.float64) / 8.0)
    kj = np.arange(128)[:, None].astype(np.float64)
    qi = np.arange(128)[None, :].astype(np.float64)
    masks = np.zeros((128, 24, 2, 128), np.float32)
    for h in range(8):
        for g, (win, dil) in enumerate(GROUPS):
            dist_d = qi - kj
            md = np.where(dist_d >= 0, np.exp(-slopes[h] * dil * dist_d), 0.0)
            dist_p = 128 + qi - kj
            mp = np.where(dist_p <= 128, np.exp(-slopes[h] * dil * dist_p), 0.0)
            masks[:, h * 3 + g, 0, :] = mp
            masks[:, h * 3 + g, 1, :] = md
    sbias = np.zeros((128, 3, 129), np.float32)
    j = np.arange(129, dtype=np.float64)
    for p in range(128):
        s = p % 8
        for g, (win, dil) in enumerate(GROUPS):
            sbias[p, g, :] = -slopes[s] * dil * (128.0 - j)
    iota = np.tile(np.arange(128, dtype=np.float32)[None, :], (128, 1))
    return dict(
        c_identf=np.eye(128, dtype=np.float32),
        c_identb=np.eye(128, dtype=np.float32).astype(ml_dtypes.bfloat16),
        c_masks=masks.astype(ml_dtypes.bfloat16),
        c_sbias=sbias,
        c_iota=iota,
    )


def build_program():
    nc = bass.Bass("TRN2", target_bir_lowering=False)

    def din(name, shape, dt=F32):
        return nc.dram_tensor(name, list(shape), dt, kind="ExternalInput").ap()

    def dout(name, shape, dt=F32):
        return nc.dram_tensor(name, list(shape), dt, kind="ExternalOutput").ap()

    def dscr(name, shape, dt=F32):
        return nc.dram_tensor(name, list(shape), dt, kind="Internal").ap()

    xp = din("xp", [SEQ, D])
    xs = din("xs", [NS, D])
    caches = [din("cache%d" % g, [16, GROUPS[g][0], 2, 8, 64]) for g in range(3)]
    stconv = din("stconv", [32, D])
    g1T = din("g1T", [128, 8])
    g2T = din("g2T", [128, 8])
    w_in = din("w_in", [D, PROJ])
    convw = din("convw", [128, 8, 3])
    w_out_a = din("w_out_a", [D, D])
    w_out_b = din("w_out_b", [512, D])
    w_o = din("w_o", [D, D])
    wq = din("wq", [D, 2048])
    keysT = din("keysT", [128, 16, 128])
    uT = din("uT", [D, NEXP])
    vtab = din("vtab", [NEXP, D])
    fgb = din("fgb", [1, D])
    c_identf = din("c_identf", [128, 128])
    c_identb = din("c_identb", [128, 128], BF16)
    c_masks = din("c_masks", [128, 24, 2, 128], BF16)
    c_sbias = din("c_sbias", [128, 3, 129])
    c_iota = din("c_iota", [128, 128])

    y_p = dout("y_p", [SEQ, D])
    y_s = dout("y_s", [NS, D])
    kvp = [dout("kvp%d" % g, [min(GROUPS[g][0], SEQ), 2, 8, 64]) for g in range(3)]
    conv_p = dout("conv_p", [2, D])
    kvs = [dout("kvs%d" % g, [16, GROUPS[g][0], 2, 8, 64]) for g in range(3)]
    conv_s = dout("conv_s", [32, D])

    dbg_ot = None
    scr_q = dscr("scr_q", [NS, 1536])
    scr_o = dscr("scr_o", [NS, 512])
    scr_x1 = dscr("scr_x1", [NT, D])
    scr_ut = dscr("scr_ut", [128, 128, 8, 128], BF16)
    scr_v = dscr("scr_v", [128, 128, D], BF16)

    with ExitStack() as st:
        S = Sched(nc)
        S.alloc(st)

        @contextmanager
        def scope():
            with ExitStack() as es:
                yield es
                S.barrier()
        cnt = [0]

        def sb(shape, dt, stack=st):
            cnt[0] += 1
            return stack.enter_context(nc.sbuf_tensor("t%d" % cnt[0], list(shape), dt))

        pb = [st.enter_context(nc.psum_tensor("pb%d" % i, [128, 512], F32)) for i in range(8)]

        identf = sb([128, 128], F32)
        identb = sb([128, 128], BF16)
        iota = sb([128, 128], F32)
        g1t = sb([128, 8], F32)
        g2t = sb([128, 8], F32)
        cwt = sb([128, 8, 3], F32)
        epst = sb([128, 1], F32)
        for (t, src, nm) in ((identf, c_identf, "identf"), (identb, c_identb, "identb"), (iota, c_iota, "iota"),
                             (g1t, g1T, "g1t"), (g2t, g2T, "g2t"), (cwt, convw, "cwt")):
            S.dma(lambda e, t=t, src=src: e.dma_start(out=t[:], in_=src), "const", writes=[nm])
        S.op("dve", lambda e: e.memset(epst[:], EPS), writes=["eps"])

        wst = sb([128, 8, 512], F32)
        wbf = [sb([128, 8, 512], BF16) for _ in range(2)]
        wctr = [0]

        def load_w(pieces, rows=128):
            slot = wctr[0] % 2
            wctr[0] += 1
            off = 0
            toks = []
            for i, (src, kcn) in enumerate(pieces):
                n = src.shape[-1]
                r = src.shape[0] // kcn
                srcv = src.rearrange("(kc p) c -> p kc c", p=r)
                tok = "wst.%d" % i
                S.dma(lambda e, srcv=srcv, off=off, n=n, r=r, kcn=kcn: e.dma_start(out=wst[0:r, 0:kcn, off:off + n], in_=srcv),
                      "wst", writes=[tok])
                toks.append(tok)
                off += n
            S.op("pool", lambda e, slot=slot, off=off: e.tensor_copy(out=wbf[slot][:, :, 0:off], in_=wst[:, :, 0:off]),
                 reads=toks, writes=["wbf%d" % slot] + ["wstall"])
            for tok in ["wst.%d" % i for i in range(8)]:
                S.readers.setdefault(tok, []).append(S.last_w["wbf%d" % slot])
            return wbf[slot], "wbf%d" % slot

        tiles = [(i * 128, 128) for i in range(16)] + [(SEQ, NS)]
        tgroups = [(i * 512, 512) for i in range(4)] + [(SEQ, NS)]

        def x_src(t0, n):
            return xp[t0:t0 + n, :] if t0 < SEQ else xs[:, :]

        def rmsnorm_T(src_fn, gt, gname, dstT, dst_tok, xts, hb, junk, ss, stack_tag):
            for i, (t0, n) in enumerate(tiles):
                k = i % 2
                xt = xts[k]
                S.dma(lambda e, xt=xt, t0=t0, n=n: e.dma_start(out=xt[0:n, :], in_=src_fn(t0, n)), stack_tag + "x%d" % k,
                      reads=["x1dram.%d" % i] if stack_tag == "n2" else [], writes=["xt%d" % k])
                S.op("dve", lambda e: e.memset(ss[:, 0:1], 0.0), writes=["ss"])
                S.op("act", lambda e, xt=xt, n=n: e.activation(out=junk[0:n, :], in_=xt[0:n, :], func=AF.Square, accum_out=ss[0:n, 0:1]),
                     reads=["xt%d" % k, "ss"], writes=["junk", "ss"])
                S.op("act", lambda e, n=n: e.activation(out=ss[0:n, 1:2], in_=ss[0:n, 0:1], func=AF.Sqrt, bias=epst[0:n, :], scale=1.0 / D),
                     reads=["ss", "eps"], writes=["ss1"])
                S.op("dve", lambda e, n=n: e.reciprocal(out=ss[0:n, 2:3], in_=ss[0:n, 1:2]), reads=["ss1"], writes=["ss2"])
                S.op("dve", lambda e, xt=xt, n=n: e.tensor_scalar(out=hb[0:n, :], in0=xt[0:n, :], scalar1=ss[0:n, 2:3], scalar2=None, op0=ALU.mult),
                     reads=["xt%d" % k, "ss2"], writes=["hb"])
                pt = pb[k][:].bitcast(BF16)
                for kc in range(8):
                    S.op("pe", lambda e, pt=pt, kc=kc, n=n: e.transpose(out=pt[:, kc * 128:kc * 128 + n], in_=hb[0:n, kc * 128:(kc + 1) * 128], identity=identb[0:n, 0:n]),
                         reads=["hb", "identb"], writes=["pb%d" % k])
                S.op("dve", lambda e, pt=pt, t0=t0, n=n: e.tensor_tensor(
                    out=dstT[:, :, t0:t0 + n], in0=pt.rearrange("p (k t) -> p k t", k=8)[:, :, 0:n],
                    in1=gt[:, :].unsqueeze(2).broadcast_to([128, 8, n]), op=ALU.mult),
                    reads=["pb%d" % k, gname], writes=[dst_tok + ".%d" % i] + (["hT.%d" % i] if dst_tok != "hT" else []))

        hT = sb([128, 8, NT], BF16)
        hT_all = ["hT.%d" % i for i in range(17)]

        with scope() as pa:
            uf, vf, ub, vb = [], [], [], []
            prep_c = [0]

            def prep_chunk():
                c = prep_c[0]
                if c >= 128 or STAGE < 6:
                    return
                prep_c[0] += 1
                k = 0
                S.dma(lambda e, c=c, k=k: e.dma_start(out=uf[k][:], in_=uT[:, c * 128:(c + 1) * 128].rearrange("(kc p) e -> p kc e", p=128)),
                      "uf%d" % k, writes=["uf%d" % k])
                S.dma(lambda e, c=c, k=k: e.dma_start(out=vf[k][:], in_=vtab[c * 128:(c + 1) * 128, :]),
                      "vf%d" % k, writes=["vf%d" % k])
                S.op("act", lambda e, k=k: e.activation(out=ub[k][:], in_=uf[k][:], func=AF.Copy), reads=["uf%d" % k], writes=["ub%d" % k])
                S.op("pool", lambda e, k=k: e.tensor_copy(out=vb[k][:], in_=vf[k][:]), reads=["vf%d" % k], writes=["vb%d" % k])
                S.dma(lambda e, c=c, k=k: e.dma_start(out=scr_ut[c], in_=ub[k][:]), "ubs%d" % k, reads=["ub%d" % k], writes=["scr_ut%d" % c])
                S.dma(lambda e, c=c, k=k: e.dma_start(out=scr_v[c], in_=vb[k][:]), "vbs%d" % k, reads=["vb%d" % k], writes=["scr_v%d" % c])

            with scope() as pa1:
                OT = sb([64, 8, NT], BF16, pa1)
                with scope() as s1:
                    xts = [sb([128, D], F32, s1) for _ in range(2)]
                    hb = sb([128, D], BF16, s1)
                    junk = sb([128, D], BF16, s1)
                    ss = sb([128, 4], F32, s1)
                    rmsnorm_T(x_src, g1t, "g1t", hT, "hT", xts, hb, junk, ss, "n1")

                if STAGE >= 2:
                    with scope() as s2:
                        skv = sb([NS, 512], F32, s2)
                        pst = [sb([128, 512], F32, s2) for _ in range(2)]
                        pctr = 0
                        for typ in range(3):
                            for g in range(3):
                                W = GROUPS[g][0]
                                c0 = 3072 + typ * 1536 + g * 512
                                wt, wtok = load_w([(w_in[:, c0:c0 + 512], 8)])
                                for kc in range(8):
                                    S.op("pe", lambda e, wt=wt, kc=kc: e.matmul(pb[2][0:NS, :], lhsT=hT[:, kc, SEQ:NT], rhs=wt[:, kc, :], start=(kc == 0), stop=(kc == 7)),
                                         reads=[wtok, "hT.16"], writes=["pb2"])
                                S.op("act", lambda e: e.activation(out=skv[:], in_=pb[2][0:NS, :], func=AF.Copy), reads=["pb2"], writes=["skv"])
                                for b in range(16):
                                    if typ == 0:
                                        S.dma(lambda e, b=b, g=g: e.dma_start(out=scr_q[b * 4:(b + 1) * 4, g * 512:(g + 1) * 512], in_=skv[b * 4:(b + 1) * 4, :]),
                                              "skvst", reads=["skv"], writes=["scr_q.%d.%d" % (g, b)])
                                    else:
                                        S.dma(lambda e, b=b, g=g, W=W, typ=typ: e.dma_start(
                                            out=kvs[g][b, W - 4:W, typ - 1].rearrange("t s d -> t (s d)"), in_=skv[b * 4:(b + 1) * 4, :]),
                                            "skvst", reads=["skv"], writes=["kvsnew.%d.%d.%d" % (g, typ, b)], final=True)
                                if typ == 0:
                                    continue
                                keep = min(W, SEQ) // 128
                                for ti in range(16 - keep, 16):
                                    k = pctr % 2
                                    pctr += 1
                                    for kc in range(8):
                                        S.op("pe", lambda e, wt=wt, kc=kc, ti=ti, k=k: e.matmul(pb[3 + k][:, :], lhsT=hT[:, kc, ti * 128:(ti + 1) * 128], rhs=wt[:, kc, :], start=(kc == 0), stop=(kc == 7)),
                                             reads=[wtok, "hT.%d" % ti], writes=["pb%d" % (3 + k)])
                                    S.op("act", lambda e, k=k: e.activation(out=pst[k][:], in_=pb[3 + k][:], func=AF.Copy), reads=["pb%d" % (3 + k)], writes=["pst%d" % k])
                                    r0 = (ti - (16 - keep)) * 128
                                    S.dma(lambda e, g=g, r0=r0, typ=typ, k=k: e.dma_start(out=kvp[g][r0:r0 + 128, typ - 1].rearrange("t s d -> t (s d)"), in_=pst[k][:]),
                                          "pst%d" % k, reads=["pst%d" % k], final=True)

                if STAGE >= 3:
                    with scope() as s3:
                        Kt = sb([128, 132, 64], F32, s3)
                        Vt = sb([128, 132, 64], F32, s3)
                        prod = sb([128, 43, 64], F32, s3)
                        qs = sb([128, 4, 3, 64], F32, s3)
                        knew = sb([128, 3, 4, 64], F32, s3)
                        vnew = sb([128, 3, 4, 64], F32, s3)
                        sbias = sb([128, 3, 129], F32, s3)
                        sc = sb([128, 129], F32, s3)
                        ex = sb([128, 129], F32, s3)
                        lacc = sb([128, 12], F32, s3)
                        og = sb([128, 12, 64], F32, s3)
                        osum = sb([128, 4, 64], F32, s3)
                        lsum = sb([128, 4], F32, s3)
                        otok = sb([NS, 512], F32, s3)
                        S.dma(lambda e: e.dma_start(out=sbias[:], in_=c_sbias), "const", writes=["sbias"])
                        S.op("dve", lambda e: e.memset(lacc[:], 0.0), writes=["lacc.%d" % c for c in range(12)])
                        for b in range(16):
                            S.dma(lambda e, b=b: e.dma_start(out=qs[b * 8:(b + 1) * 8], in_=scr_q[b * 4:(b + 1) * 4, :].rearrange("t (g s d) -> s t g d", g=3, s=8)),
                                  "qs", reads=["scr_q.%d.%d" % (g, b) for g in range(3)], writes=["qs.%d" % b])
                            for g in range(3):
                                W = GROUPS[g][0]
                                S.dma(lambda e, b=b, g=g, W=W: e.dma_start(out=knew[b * 8:(b + 1) * 8, g], in_=kvs[g][b, W - 4:W, 0].rearrange("t s d -> s t d")),
                                      "qs", reads=["kvsnew.%d.1.%d" % (g, b)], writes=["knew.%d.%d" % (g, b)])
                                S.dma(lambda e, b=b, g=g, W=W: e.dma_start(out=vnew[b * 8:(b + 1) * 8, g], in_=kvs[g][b, W - 4:W, 1].rearrange("t s d -> s t d")),
                                      "qs", reads=["kvsnew.%d.2.%d" % (g, b)], writes=["vnew.%d.%d" % (g, b)])
                        qs_all = ["qs.%d" % b for b in range(16)]
                        kn_all = ["knew.%d.%d" % (g, b) for g in range(3) for b in range(16)]
                        vn_all = ["vnew.%d.%d" % (g, b) for g in range(3) for b in range(16)]
                        for g in range(3):
                            W, dil = GROUPS[g]
                            for t in range(4):
                                col = g * 4 + t
                                if g == 0 and t > 0:
                                    pass
                                else:
                                    for b in range(16):
                                        if g == 0:
                                            ksrc = caches[0][b, :, 0].rearrange("r s d -> s r d")
                                            vsrc = caches[0][b, :, 1].rearrange("r s d -> s r d")
                                        else:
                                            ksrc = caches[g][b, :, 0].rearrange("(j r) s d -> s r j d", r=dil)[:, t, 0:128, :]
                                            vsrc = caches[g][b, :, 1].rearrange("(j r) s d -> s r j d", r=dil)[:, t, 0:128, :]
                                        S.dma(lambda e, b=b, ksrc=ksrc: e.dma_start(out=Kt[b * 8:(b + 1) * 8, 0:128, :], in_=ksrc), "Kt", writes=["Kt.%d" % b])
                                        S.dma(lambda e, b=b, vsrc=vsrc: e.dma_start(out=Vt[b * 8:(b + 1) * 8, 0:128, :], in_=vsrc), "Vt", writes=["Vt.%d" % b], eng=os.environ.get("MK_VENG", "act"))
                                    if g == 0:
                                        S.op("pool", lambda e: e.tensor_copy(out=Kt[:, 128:132, :], in_=knew[:, 0]), reads=kn_all, writes=["Ktn"])
                                        S.op("pool", lambda e: e.tensor_copy(out=Vt[:, 128:132, :], in_=vnew[:, 0]), reads=vn_all, writes=["Vtn"])
                                    else:
                                        S.op("pool", lambda e, g=g, t=t: e.tensor_copy(out=Kt[:, 128:129, :], in_=knew[:, g, t:t + 1, :]), reads=kn_all, writes=["Ktn"])
                                        S.op("pool", lambda e, g=g, t=t: e.tensor_copy(out=Vt[:, 128:129, :], in_=vnew[:, g, t:t + 1, :]), reads=vn_all, writes=["Vtn"])
                                w0 = t if g == 0 else 0
                                kt_all = ["Kt.%d" % b for b in range(16)] + ["Ktn"]
                                vt_all = ["Vt.%d" % b for b in range(16)] + ["Vtn"]
                                for ch in range(3):
                                    k0 = ch * 43
                                    S.op("dve", lambda e, w0=w0, k0=k0, t=t, g=g: e.tensor_tensor(
                                        out=prod[:], in0=Kt[:, w0 + k0:w0 + k0 + 43, :],
                                        in1=qs[:, t, g, :].unsqueeze(1).broadcast_to([128, 43, 64]), op=ALU.mult),
                                        reads=kt_all + qs_all, writes=["prod"])
                                    S.op("dve", lambda e, k0=k0: e.tensor_reduce(out=sc[:, k0:k0 + 43], in_=prod[:], axis=AX.X, op=ALU.add),
                                         reads=["prod"], writes=["sc.%d" % ch])
                                S.op("dve", lambda e, g=g: e.scalar_tensor_tensor(out=ex[:], in0=sc[:], scalar=0.125, in1=sbias[:, g, :], op0=ALU.mult, op1=ALU.add),
                                     reads=["sc.0", "sc.1", "sc.2", "sbias"], writes=["ex"])
                                S.op("act", lambda e, col=col: e.activation(out=sc[:], in_=ex[:], func=AF.Exp, accum_out=lacc[:, col:col + 1]),
                                     reads=["ex", "lacc.%d" % col], writes=["sc.0", "sc.1", "sc.2", "lacc.%d" % col])
                                for ch in range(3):
                                    k0 = ch * 43
                                    S.op("dve", lambda e, w0=w0, k0=k0: e.tensor_tensor(
                                        out=prod[:], in0=Vt[:, w0 + k0:w0 + k0 + 43, :],
                                        in1=sc[:, k0:k0 + 43].unsqueeze(2).broadcast_to([128, 43, 64]), op=ALU.mult),
                                        reads=vt_all + ["sc.0", "sc.1", "sc.2"], writes=["prod"])
                                    dst = og[:, col, :] if ch == 0 else ex[:, ch * 64 - 64:ch * 64]
                                    S.op("dve", lambda e, dst=dst: e.tensor_reduce(out=dst, in_=prod[:].rearrange("p k d -> p d k"), axis=AX.X, op=ALU.add),
                                         reads=["prod"], writes=["ogp.0"] if ch == 0 else ["ex"])
                                S.op("dve", lambda e, col=col: e.tensor_tensor(out=og[:, col, :], in0=og[:, col, :], in1=ex[:, 0:64], op=ALU.add),
                                     reads=["ogp.0", "ex"], writes=["ogp.0"])
                                S.op("dve", lambda e, col=col: e.tensor_tensor(out=og[:, col, :], in0=og[:, col, :], in1=ex[:, 64:128], op=ALU.add),
                                     reads=["ogp.0", "ex"], writes=["ogp.0", "og.%d" % col])
                        og_all = ["og.%d" % c for c in range(12)]
                        la_all = ["lacc.%d" % c for c in range(12)]
                        S.op("dve", lambda e: e.tensor_tensor(out=osum[:], in0=og[:, 0:4, :], in1=og[:, 4:8, :], op=ALU.add), reads=og_all, writes=["osum"])
                        S.op("dve", lambda e: e.tensor_tensor(out=osum[:], in0=osum[:], in1=og[:, 8:12, :], op=ALU.add), reads=og_all + ["osum"], writes=["osum"])
                        S.op("dve", lambda e: e.tensor_tensor(out=lsum[:], in0=lacc[:, 0:4], in1=lacc[:, 4:8], op=ALU.add), reads=la_all, writes=["lsum"])
                        S.op("dve", lambda e: e.tensor_tensor(out=lsum[:], in0=lsum[:], in1=lacc[:, 8:12], op=ALU.add), reads=la_all + ["lsum"], writes=["lsum"])
                        S.op("dve", lambda e: e.reciprocal(out=lsum[:], in_=lsum[:]), reads=["lsum"], writes=["lsum"])
                        S.op("dve", lambda e: e.tensor_tensor(out=osum[:], in0=osum[:], in1=lsum[:, :].unsqueeze(2).broadcast_to([128, 4, 64]), op=ALU.mult),
                             reads=["osum", "lsum"], writes=["osum"])
                        for b in range(16):
                            S.dma(lambda e, b=b: e.dma_start(out=scr_o[b * 4:(b + 1) * 4, :].rearrange("t (s d) -> s t d", s=8), in_=osum[b * 8:(b + 1) * 8]),
                                  "scro", reads=["osum"], writes=["scr_o.%d" % b])
                        S.dma(lambda e: e.dma_start(out=otok[:], in_=scr_o), "otok", reads=["scr_o.%d" % b for b in range(16)], writes=["otok"])
                        for h in range(8):
                            S.op("pe", lambda e, h=h: e.transpose(out=pb[2][0:64, h * 64:(h + 1) * 64], in_=otok[:, h * 64:(h + 1) * 64], identity=identf[0:NS, 0:NS]),
                                 reads=["otok", "identf"], writes=["pb2"])
                        S.op("act", lambda e: e.activation(out=OT[:, :, SEQ:NT], in_=pb[2][0:64, :].rearrange("p (h t) -> p h t", h=8), func=AF.Copy),
                             reads=["pb2"], writes=["OTs"])

                uf.extend([sb([128, 8, 128], F32, pa1)] * 2)
                vf.extend([sb([128, D], F32, pa1)] * 2)
                ub.extend([sb([128, 8, 128], BF16, pa1)] * 2)
                vb.extend([sb([128, D], BF16, pa1)] * 2)
                if STAGE >= 4:
                    with scope() as s4:
                        masks = sb([128, 24, 2, 128], BF16, s4)
                        QT = sb([64, 3, SEQ], BF16, s4)
                        KT = sb([64, 3, SEQ], BF16, s4)
                        Vh = sb([128, 3, 16, 128], BF16, s4)
                        acc = sb([128, SEQ], F32, s4)
                        rc = sb([64, 512], F32, s4)
                        ebuf = [sb([128, 256], F32, s4) for _ in range(3)]
                        pT = [sb([128, 256], BF16, s4) for _ in range(3)]
                        S.dma(lambda e: e.dma_start(out=masks[:], in_=c_masks), "const", writes=["masks"])
                        S.op("pool", lambda e: e.memset(Vh[:], 1.0), writes=["Vh"])

                        def permv(ap2048, dil):
                            return ap2048.rearrange("p (j r) -> p r j", r=dil)

                        uctr = 0
                        for h in range(8):
                            pieces = []
                            for typ in range(3):
                                for g in range(3):
                                    c0 = 3072 + typ * 1536 + g * 512 + h * 64
                                    pieces.append((w_in[:, c0:c0 + 64], 8))
                            wt, wtok = load_w(pieces[0:6])
                            wv, wvtok = load_w(pieces[6:9])
                            for typ, dstT, dname in ((0, QT, "QT"), (1, KT, "KT")):
                                for g in range(3):
                                    dil = GROUPS[g][1]
                                    wcol = (typ * 3 + g) * 64
                                    for tg in range(4):
                                        bk = 2 + (tg % 2)
                                        for kc in range(8):
                                            if dil == 1:
                                                rhs = hT[:, kc, tg * 512:(tg + 1) * 512]
                                            elif dil == 4:
                                                rhs = permv(hT[:, kc, 0:SEQ], 4)[:, tg, :]
                                            else:
                                                rhs = permv(hT[:, kc, 0:SEQ], 16)[:, tg * 4:(tg + 1) * 4, :]
                                            S.op("pe", lambda e, bk=bk, wt=wt, kc=kc, wcol=wcol, rhs=rhs: e.matmul(
                                                pb[bk][0:64, :], lhsT=wt[:, kc, wcol:wcol + 64], rhs=rhs, start=(kc == 0), stop=(kc == 7)),
                                                reads=[wtok] + hT_all[0:16], writes=["pb%d" % bk])
                                        S.op("act", lambda e, bk=bk, dstT=dstT, g=g, tg=tg: e.activation(out=dstT[:, g, tg * 512:(tg + 1) * 512], in_=pb[bk][0:64, :], func=AF.Copy),
                                             reads=["pb%d" % bk], writes=["%s.%d.%d" % (dname, g, tg)])
                            for g in range(3):
                                dil = GROUPS[g][1]
                                L = SEQ // dil
                                for half in range(2):
                                    bk = 4 + half
                                    for nb in range(8):
                                        n = half * 8 + nb
                                        r, lb = (n * 128) // L, ((n * 128) % L) // 128
                                        for kc in range(8):
                                            lhsT = permv(hT[:, kc, 0:SEQ], dil)[:, r, lb * 128:(lb + 1) * 128]
                                            S.op("pe", lambda e, bk=bk, nb=nb, lhsT=lhsT, kc=kc, g=g, wv=wv: e.matmul(
                                                pb[bk][:, nb * 64:(nb + 1) * 64], lhsT=lhsT, rhs=wv[:, kc, g * 64:(g + 1) * 64], start=(kc == 0), stop=(kc == 7)),
                                                reads=[wvtok] + hT_all[0:16], writes=["pb%d" % bk])
                                    S.op("act", lambda e, bk=bk, g=g, half=half: e.activation(
                                        out=Vh[:, g, half * 8:(half + 1) * 8, 0:64], in_=pb[bk][:, :].rearrange("p (n d) -> p n d", n=8), func=AF.Copy),
                                        reads=["pb%d" % bk, "Vh"], writes=["Vh.%d.%d" % (g, half)])
                            def front(g, n, u):
                                dil = GROUPS[g][1]
                                bpl = (SEQ // dil) // 128
                                lb = n % bpl
                                slots = ([0] if lb > 0 else []) + [1]
                                c_lo = slots[0] * 128
                                for sl in slots:
                                    kb = n - 1 if sl == 0 else n
                                    S.op("pe", lambda e, sl=sl, kb=kb, n=n, g=g, u=u: e.matmul(
                                        pb[2 + u][:, sl * 128:(sl + 1) * 128], lhsT=KT[:, g, kb * 128:(kb + 1) * 128], rhs=QT[:, g, n * 128:(n + 1) * 128],
                                        start=True, stop=True),
                                        reads=["KT.%d.%d" % (g, kb // 4), "QT.%d.%d" % (g, n // 4)], writes=["pb%d" % (2 + u)])
                                S.op("act", lambda e, u=u, c_lo=c_lo: e.activation(out=ebuf[u][:, c_lo:256], in_=pb[2 + u][:, c_lo:256], func=AF.Exp, scale=0.125),
                                     reads=["pb%d" % (2 + u)], writes=["ebuf%d" % u])
                                S.op("dve", lambda e, u=u, c_lo=c_lo, h=h, g=g: e.tensor_tensor(
                                    out=pT[u][:, c_lo:256], in0=ebuf[u][:, c_lo:256],
                                    in1=masks[:, h * 3 + g].rearrange("p s q -> p (s q)")[:, c_lo:256], op=ALU.mult),
                                    reads=["ebuf%d" % u, "masks"], writes=["pT%d" % u])

                            def back(g, n, u):
                                dil = GROUPS[g][1]
                                bpl = (SEQ // dil) // 128
                                r, lb = n // bpl, n % bpl
                                slots = ([0] if lb > 0 else []) + [1]
                                for i, sl in enumerate(slots):
                                    kb = n - 1 if sl == 0 else n
                                    S.op("pe", lambda e, u=u, sl=sl, kb=kb, g=g, i=i, last=(i == len(slots) - 1): e.matmul(
                                        pb[5 + u][:, 0:128], lhsT=Vh[:, g, kb, :], rhs=pT[u][:, sl * 128:(sl + 1) * 128], start=(i == 0), stop=last),
                                        reads=["pT%d" % u, "Vh.%d.%d" % (g, kb // 8), "Vh"], writes=["pb%d" % (5 + u)])
                                av = permv(acc[:, :], dil)[:, r, lb * 128:(lb + 1) * 128]
                                if g == 0:
                                    S.op("dve", lambda e, av=av, u=u: e.tensor_copy(out=av, in_=pb[5 + u][:, 0:128]), reads=["pb%d" % (5 + u)], writes=["acc"])
                                else:
                                    S.op("dve", lambda e, av=av, u=u: e.tensor_tensor(out=av, in0=av, in1=pb[5 + u][:, 0:128], op=ALU.add),
                                         reads=["pb%d" % (5 + u), "acc"], writes=["acc"])

                            units = [(g, n) for g in range(3) for n in range(16)]
                            front(units[0][0], units[0][1], 0)
                            front(units[1][0], units[1][1], 1)
                            for ui, (g, n) in enumerate(units):
                                if ui + 2 < len(units):
                                    front(units[ui + 2][0], units[ui + 2][1], (ui + 2) % 3)
                                back(g, n, ui % 3)
                                uctr += 1
                                if uctr % 3 == 0:
                                    prep_chunk()
                            for tg in range(4):
                                S.op("dve", lambda e, tg=tg: e.reciprocal(out=rc[:, :], in_=acc[64:128, tg * 512:(tg + 1) * 512]), reads=["acc"], writes=["rc"])
                                S.op("dve", lambda e, tg=tg, h=h: e.tensor_tensor(out=OT[:, h, tg * 512:(tg + 1) * 512], in0=acc[0:64, tg * 512:(tg + 1) * 512], in1=rc[:, :], op=ALU.mult),
                                     reads=["acc", "rc"], writes=["OT.%d" % h])

                while prep_c[0] < 128 and STAGE >= 6:
                    prep_chunk()
                OT_all = ["OT.%d" % h for h in range(8)] + ["OTs"]
                if dbg_ot is not None:
                    S.dma(lambda e: e.dma_start(out=dbg_ot, in_=OT[:]), "dbgot", reads=OT_all, final=True)
                with scope() as pa2:
                    byT = sb([128, 8, NT], BF16, pa2)
                    zT = sb([128, 8, NT], BF16, pa2)
                    if STAGE >= 5:
                        with scope() as s5:
                            extp = sb([128, SEQ + 2], F32, s5)
                            exts = sb([128, 16, 6], F32, s5)
                            stt = sb([32, D], F32, s5)
                            hv = sb([128, 512], F32, s5)
                            yb = sb([128, 512], F32, s5)
                            cvT = sb([128, 8, 34], F32, s5)
                            cvtok = sb([34, D], F32, s5)
                            S.dma(lambda e: e.dma_start(out=stt[:], in_=stconv), "const", writes=["stt"])
                            S.op("pool", lambda e: e.memset(extp[:, 0:2], 0.0), writes=["extp"])
                            for cc in range(8):
                                wt, wtok = load_w([(w_in[:, j * 1024 + cc * 128:j * 1024 + (cc + 1) * 128], 8) for j in range(3)])
                                S.op("pe", lambda e, cc=cc: e.transpose(out=pb[5][:, 0:32], in_=stt[:, cc * 128:(cc + 1) * 128], identity=identf[0:32, 0:32]),
                                     reads=["stt", "identf"], writes=["pb5"])
                                S.op("act", lambda e: e.activation(out=exts[:, :, 0:2], in_=pb[5][:, 0:32].rearrange("p (b r) -> p b r", r=2), func=AF.Copy),
                                     reads=["pb5"], writes=["exts"])
                                for gi, (t0, n) in enumerate(tgroups):
                                    for j in range(3):
                                        for kc in range(8):
                                            S.op("pe", lambda e, j=j, kc=kc, t0=t0, n=n, wt=wt: e.matmul(
                                                pb[2 + j][:, 0:n], lhsT=wt[:, kc, j * 128:(j + 1) * 128], rhs=hT[:, kc, t0:t0 + n], start=(kc == 0), stop=(kc == 7)),
                                                reads=[wtok] + hT_all, writes=["pb%d" % (2 + j)])
                                    S.op("act", lambda e, n=n: e.activation(out=hv[:, 0:n], in_=pb[4][:, 0:n], func=AF.Copy), reads=["pb4"], writes=["hv"])
                                    if t0 < SEQ:
                                        uo = extp[:, 2 + t0:2 + t0 + n]
                                        e0, e1, e2 = extp[:, t0:t0 + n], extp[:, t0 + 1:t0 + 1 + n], extp[:, t0 + 2:t0 + 2 + n]
                                        hvv, cps, bps, yv, byv = hv[:, 0:n], pb[3][:, 0:n], pb[2][:, 0:n], yb[:, 0:n], byT[:, cc, t0:t0 + n]
                                        etok = "extp"
                                    else:
                                        uo = exts[:, :, 2:6]
                                        e0, e1, e2 = exts[:, :, 0:4], exts[:, :, 1:5], exts[:, :, 2:6]
                                        v4 = lambda ap: ap.rearrange("p (b t) -> p b t", t=4)
                                        hvv, cps, bps, yv, byv = v4(hv[:, 0:n]), v4(pb[3][:, 0:n]), v4(pb[2][:, 0:n]), v4(yb[:, 0:n]), v4(byT[:, cc, t0:t0 + n])
                                        etok = "exts"
                                    S.op("dve", lambda e, uo=uo, cps=cps, hvv=hvv: e.tensor_tensor(out=uo, in0=cps, in1=hvv, op=ALU.mult),
                                         reads=["pb3", "hv", etok], writes=[etok])
                                    S.op("dve", lambda e, yv=yv, e0=e0, cc=cc: e.tensor_scalar(out=yv, in0=e0, scalar1=cwt[:, cc, 0:1], scalar2=None, op0=ALU.mult),
                                         reads=[etok, "cwt"], writes=["yb"])
                                    S.op("dve", lambda e, yv=yv, e1=e1, cc=cc: e.scalar_tensor_tensor(out=yv, in0=e1, scalar=cwt[:, cc, 1:2], in1=yv, op0=ALU.mult, op1=ALU.add),
                                         reads=[etok, "cwt", "yb"], writes=["yb"])
                                    S.op("dve", lambda e, yv=yv, e2=e2, cc=cc: e.scalar_tensor_tensor(out=yv, in0=e2, scalar=cwt[:, cc, 2:3], in1=yv, op0=ALU.mult, op1=ALU.add),
                                         reads=[etok, "cwt", "yb"], writes=["yb"])
                                    S.op("dve", lambda e, byv=byv, bps=bps, yv=yv: e.tensor_tensor(out=byv, in0=bps, in1=yv, op=ALU.mult),
                                         reads=["pb2", "yb"], writes=["byT.%d.%d" % (cc, gi)])
                                S.op("act", lambda e, cc=cc: e.activation(out=cvT[:, cc, 0:2], in_=extp[:, SEQ:SEQ + 2], func=AF.Copy), reads=["extp"], writes=["cvT.%d" % cc])
                                S.op("act", lambda e, cc=cc: e.activation(out=cvT[:, cc, 2:34].rearrange("p (b r) -> p b r", r=2), in_=exts[:, :, 4:6], func=AF.Copy),
                                     reads=["exts", "cvT.%d" % cc], writes=["cvT.%d" % cc])
                            for cc in range(8):
                                S.op("pe", lambda e, cc=cc: e.transpose(out=pb[5][0:34, 0:128], in_=cvT[:, cc, :], identity=identf[:, :]),
                                     reads=["cvT.%d" % cc, "identf"], writes=["pb5"])
                                S.op("act", lambda e, cc=cc: e.activation(out=cvtok[:, cc * 128:(cc + 1) * 128], in_=pb[5][0:34, 0:128], func=AF.Copy),
                                     reads=["pb5"], writes=["cvtok.%d" % cc])
                            cv_all = ["cvtok.%d" % cc for cc in range(8)]
                            S.dma(lambda e: e.dma_start(out=conv_p, in_=cvtok[0:2, :]), "cvst", reads=cv_all, final=True)
                            S.dma(lambda e: e.dma_start(out=conv_s, in_=cvtok[2:34, :]), "cvst", reads=cv_all, final=True)

                    if STAGE >= 5:
                        with scope() as s6:
                            sga = sb([128, 512], F32, s6)
                            sgb = sb([128, 512], F32, s6)
                            by_all = ["byT.%d.%d" % (cc, gi) for cc in range(8) for gi in range(5)]
                            for cc in range(8):
                                wt, wtok = load_w([(w_out_a[:, cc * 128:(cc + 1) * 128], 8),
                                                   (w_in[:, 7680 + cc * 128:7680 + (cc + 1) * 128], 8),
                                                   (w_in[:, 8704 + cc * 128:8704 + (cc + 1) * 128], 8),
                                                   (w_out_b[:, cc * 128:(cc + 1) * 128], 8)])
                                for gi, (t0, n) in enumerate(tgroups):
                                    for kc in range(8):
                                        S.op("pe", lambda e, kc=kc, t0=t0, n=n, wt=wt: e.matmul(pb[2][:, 0:n], lhsT=wt[:, kc, 0:128], rhs=byT[:, kc, t0:t0 + n], start=(kc == 0), stop=(kc == 7)),
                                             reads=[wtok] + by_all, writes=["pb2"])
                                    for kc in range(8):
                                        S.op("pe", lambda e, kc=kc, t0=t0, n=n, wt=wt: e.matmul(pb[3][:, 0:n], lhsT=wt[0:64, kc, 384:512], rhs=OT[:, kc, t0:t0 + n], start=(kc == 0), stop=(kc == 7)),
                                             reads=[wtok] + OT_all, writes=["pb3"])
                                    for j in range(2):
                                        for kc in range(8):
                                            S.op("pe", lambda e, j=j, kc=kc, t0=t0, n=n, wt=wt: e.matmul(pb[4 + j][:, 0:n], lhsT=wt[:, kc, 128 + j * 128:256 + j * 128], rhs=hT[:, kc, t0:t0 + n], start=(kc == 0), stop=(kc == 7)),
                                                 reads=[wtok] + hT_all, writes=["pb%d" % (4 + j)])
                                    S.op("act", lambda e, n=n: e.activation(out=sga[:, 0:n], in_=pb[4][:, 0:n], func=AF.Sigmoid), reads=["pb4"], writes=["sga"])
                                    S.op("act", lambda e, n=n: e.activation(out=sgb[:, 0:n], in_=pb[5][:, 0:n], func=AF.Sigmoid), reads=["pb5"], writes=["sgb"])
                                    S.op("dve", lambda e, n=n: e.tensor_tensor(out=sga[:, 0:n], in0=sga[:, 0:n], in1=pb[2][:, 0:n], op=ALU.mult), reads=["sga", "pb2"], writes=["sga"])
                                    S.op("dve", lambda e, n=n: e.tensor_tensor(out=sgb[:, 0:n], in0=sgb[:, 0:n], in1=pb[3][:, 0:n], op=ALU.mult), reads=["sgb", "pb3"], writes=["sgb"])
                                    S.op("dve", lambda e, n=n, cc=cc, t0=t0: e.tensor_tensor(out=zT[:, cc, t0:t0 + n], in0=sga[:, 0:n], in1=sgb[:, 0:n], op=ALU.add),
                                         reads=["sga", "sgb"], writes=["zT.%d.%d" % (cc, gi)])

                    if STAGE >= 5:
                        with scope() as s7:
                            wo = sb([128, 8, D], BF16, s7)
                            x1t = [sb([128, D], F32, s7) for _ in range(2)]
                            z_all = ["zT.%d.%d" % (cc, gi) for cc in range(8) for gi in range(5)]
                            for half in range(2):
                                wt, wtok = load_w([(w_o[:, half * 512:(half + 1) * 512], 8)])
                                S.op("pool", lambda e, wt=wt, half=half: e.tensor_copy(out=wo[:, :, half * 512:(half + 1) * 512], in_=wt[:, :, :]), reads=[wtok], writes=["wo.%d" % half])
                            for i, (t0, n) in enumerate(tiles):
                                k = i % 2
                                S.dma(lambda e, k=k, t0=t0, n=n: e.dma_start(out=x1t[k][0:n, :], in_=x_src(t0, n)), "x1ld%d" % k, writes=["x1t%d" % k])
                                for half in range(2):
                                    for kc in range(8):
                                        S.op("pe", lambda e, half=half, kc=kc, t0=t0, n=n, k=k: e.matmul(pb[2 + 2 * k + half][0:n, :], lhsT=zT[:, kc, t0:t0 + n], rhs=wo[:, kc, half * 512:(half + 1) * 512], start=(kc == 0), stop=(kc == 7)),
                                             reads=z_all + ["wo.%d" % half], writes=["pb%d" % (2 + 2 * k + half)])
                                    S.op("dve", lambda e, half=half, n=n, k=k: e.tensor_tensor(out=x1t[k][0:n, half * 512:(half + 1) * 512], in0=x1t[k][0:n, half * 512:(half + 1) * 512], in1=pb[2 + 2 * k + half][0:n, :], op=ALU.add),
                                         reads=["x1t%d" % k, "pb%d" % (2 + 2 * k + half)], writes=["x1t%d" % k])
                                S.dma(lambda e, k=k, t0=t0, n=n: e.dma_start(out=scr_x1[t0:t0 + n, :], in_=x1t[k][0:n, :]), "x1st%d" % k, reads=["x1t%d" % k], writes=["x1dram.%d" % i])

        if STAGE >= 6:
            h2T = hT
            h2_all = ["h2T.%d" % i for i in range(17)]
            i1T = sb([128, NT], F32)
            i2T = sb([128, NT], F32)
            gTt = sb([128, NT], F32)

            def x1_src(t0, n):
                return scr_x1[t0:t0 + n, :]

            with scope() as b1:
              if not os.environ.get("MK_NOB1"):
                xts = [sb([128, D], F32, b1) for _ in range(2)]
                hb = sb([128, D], BF16, b1)
                junk = sb([128, D], BF16, b1)
                ss = sb([128, 4], F32, b1)
                rmsnorm_T(x1_src, g2t, "g2t", h2T, "h2T", xts, hb, junk, ss, "n2")
                wqb = sb([128, 8, 2048], BF16, b1)
                for pc in range(4 if B1CUT >= 1 else 0):
                    wt, wtok = load_w([(wq[:, pc * 512:(pc + 1) * 512], 8)])
                    S.op("pool", lambda e, wt=wt, pc=pc: e.tensor_copy(out=wqb[:, :, pc * 512:(pc + 1) * 512], in_=wt[:, :, :]), reads=[wtok], writes=["wqb.%d" % pc])
                wq_all = ["wqb.%d" % pc for pc in range(4)]
                kyf = sb([128, 16, 128], F32, b1)
                kyb = sb([128, 16, 128], BF16, b1)
                S.dma(lambda e: e.dma_start(out=kyf[:], in_=keysT), "const", writes=["kyf"])
                S.op("pool", lambda e: e.tensor_copy(out=kyb[:], in_=kyf[:]), reads=["kyf"], writes=["kyb"])
                for g in range(3):
                    W = GROUPS[g][0]
                    nb = 16 if W == 2048 else (4 if W == 512 else 1)
                    per = 16 // nb
                    for i in range(nb):
                        src = caches[g][i * per:(i + 1) * per, 4:W].rearrange("b r k s d -> b (r k s d)")
                        dst = kvs[g][i * per:(i + 1) * per, 0:W - 4].rearrange("b r k s d -> b (r k s d)")
                        S.dma(lambda e, src=src, dst=dst: e.dma_start(out=dst, in_=src), "cpy%d" % g, eng="sp", final=True)

                qTg = sb([128, 16, 512], BF16, b1)
                scs = sb([128, 16, 128], F32, b1)
                S.op("pool", lambda e: e.memset(qTg[:], 0.0), writes=["qTg.%d" % ch for ch in range(16)])
                v12 = sb([128, 16, 16], F32, b1)
                i12 = sb([128, 16, 16], U32, b1)
                i12f = sb([128, 16, 16], F32, b1)
                wk = sb([128, 256], F32, b1)
                cand = sb([128, 8, 256], F32, b1)
                svt = sb([128, 8, 16], F32, b1)
                pos = sb([128, 8, 16], U32, b1)
                pa_ = sb([128, 8, 16], U32, b1)
                pb_ = sb([128, 8, 16], U32, b1)
                paf = sb([128, 8, 16], F32, b1)
                pbf = sb([128, 8, 16], F32, b1)
                oh = sb([128, 8, 16, 16], F32, b1)
                i1f = sb([128, 8, 16], F32, b1)
                i2f = sb([128, 8, 16], F32, b1)
                gte = sb([128, 8, 16], F32, b1)
                zs = sb([128, 8], F32, b1)
                c4 = sb([128, 2], U32, b1)
                S.op("dve", lambda e: e.memset(c4[:, 0:1], 4), writes=["c4a"])
                S.op("dve", lambda e: e.memset(c4[:, 1:2], 15), writes=["c4b"])
                tgroups_b1 = [(i * 512, 512) for i in range(4)] + [(NT - 128, 128)]
                for gi, (t0g, ng) in enumerate(tgroups_b1 if (B1CUT >= 1 and B1SUB >= 2) else []):
                    for ch in range(16):
                        bk = 2 + ch % 2
                        for kc in range(8):
                            S.op("pe", lambda e, bk=bk, ch=ch, kc=kc, t0g=t0g, ng=ng: e.matmul(pb[bk][:, 0:ng], lhsT=wqb[:, kc, ch * 128:(ch + 1) * 128], rhs=h2T[:, kc, t0g:t0g + ng], start=(kc == 0), stop=(kc == 7)),
                                 reads=wq_all + h2_all, writes=["pb%d" % bk])
                        S.op("act", lambda e, bk=bk, ch=ch, ng=ng: e.activation(out=qTg[:, ch, 0:ng], in_=pb[bk][:, 0:ng], func=AF.Copy), reads=["pb%d" % bk], writes=["qTg.%d" % ch])
                    qT_all = ["qTg.%d" % ch for ch in range(16)]
                    for tl in range((ng + 127) // 128):
                        n = min(128, ng - tl * 128)
                        t0 = t0g + tl * 128
                        if B1SUB < 3:
                            continue
                        if os.environ.get("MK_GI") and str(gi) not in os.environ["MK_GI"]:
                            continue
                        for ch in range(16):
                            S.op("pe", lambda e, ch=ch, tl=tl, n=n: e.matmul(pb[4 + ch // 4][:, (ch % 4) * 128:(ch % 4 + 1) * 128], lhsT=qTg[:, ch, tl * 128:tl * 128 + 128], rhs=kyb[:, ch, :], start=True, stop=True),
                                 reads=qT_all + ["kyb"], writes=["sc%d" % ch])
                        if B1CUT < 2:
                            continue
                        for bq in range(4):
                            S.op("act", lambda e, bq=bq: e.activation(out=scs[:, bq * 4:(bq + 1) * 4, :], in_=pb[4 + bq][:, :].rearrange("p (c k) -> p c k", c=4), func=AF.Copy),
                                 reads=["sc%d" % (bq * 4 + j) for j in range(4)], writes=["sc%d" % (bq * 4 + j) for j in range(4)] + ["scs%d" % bq])
                        for ch in range(16):
                            sv_ = scs[0:n, ch, :]
                            S.op("dve", lambda e, sv_=sv_, ch=ch, n=n: e.max(out=v12[0:n, ch, 0:8], in_=sv_), reads=["scs%d" % (ch // 4)], writes=["v12a"])
                            S.op("dve", lambda e, sv_=sv_, ch=ch, n=n: e.max_index(out=i12[0:n, ch, 0:8], in_max=v12[0:n, ch, 0:8], in_values=sv_), reads=["scs%d" % (ch // 4), "v12a"], writes=["i12a"])
                            S.op("dve", lambda e, sv_=sv_, ch=ch, n=n: e.match_replace(out=wk[0:n, 0:128], in_to_replace=v12[0:n, ch, 0:8], in_values=sv_, imm_value=-1e30), reads=["scs%d" % (ch // 4), "v12a"], writes=["wk"])
                            S.op("dve", lambda e, ch=ch, n=n: e.max(out=v12[0:n, ch, 8:16], in_=wk[0:n, 0:128]), reads=["wk"], writes=["v12b"])
                            S.op("dve", lambda e, ch=ch, n=n: e.max_index(out=i12[0:n, ch, 8:16], in_max=v12[0:n, ch, 8:16], in_values=wk[0:n, 0:128]), reads=["wk", "v12b"], writes=["i12.%d" % ch, "v12.%d" % ch])
                        if B1CUT < 3:
                            continue
                        v_all = ["v12.%d" % ch for ch in range(16)]
                        i_all = ["i12.%d" % ch for ch in range(16)]
                        v12v = v12[:, :, :].rearrange("p (h f) k -> p h f k", f=2)
                        S.op("dve", lambda e, n=n, v12v=v12v: e.tensor_tensor(
                            out=cand[0:n].rearrange("p h (a b) -> p h a b", a=16), in0=v12v[0:n, :, 0, :].unsqueeze(3).broadcast_to([n, 8, 16, 16]),
                            in1=v12v[0:n, :, 1, :].unsqueeze(2).broadcast_to([n, 8, 16, 16]), op=ALU.add), reads=v_all, writes=["cand"])
                        S.op("dve", lambda e, n=n: e.tensor_copy(out=i12f[0:n], in_=i12[0:n]), reads=i_all, writes=["i12f"])
                        for h in range(8):
                            S.op("dve", lambda e, h=h, n=n: e.max(out=svt[0:n, h, 0:8], in_=cand[0:n, h, :]), reads=["cand"], writes=["sva"])
                            S.op("dve", lambda e, h=h, n=n: e.max_index(out=pos[0:n, h, 0:8], in_max=svt[0:n, h, 0:8], in_values=cand[0:n, h, :]), reads=["cand", "sva"], writes=["posa"])
                            S.op("dve", lambda e, h=h, n=n: e.match_replace(out=wk[0:n, :], in_to_replace=svt[0:n, h, 0:8], in_values=cand[0:n, h, :], imm_value=-1e30), reads=["cand", "sva"], writes=["wk"])
                            S.op("dve", lambda e, h=h, n=n: e.max(out=svt[0:n, h, 8:16], in_=wk[0:n, :]), reads=["wk"], writes=["svb"])
                            S.op("dve", lambda e, h=h, n=n: e.max_index(out=pos[0:n, h, 8:16], in_max=svt[0:n, h, 8:16], in_values=wk[0:n, :]), reads=["wk", "svb"], writes=["pos.%d" % h, "sv.%d" % h])
                        if B1CUT < 4:
                            continue
                        p_all = ["pos.%d" % h for h in range(8)]
                        s_all = ["sv.%d" % h for h in range(8)]
                        S.op("dve", lambda e, n=n: e.tensor_scalar(out=pa_[0:n], in0=pos[0:n], scalar1=c4[0:n, 0:1], scalar2=None, op0=ALU.logical_shift_right), reads=p_all + ["c4a"], writes=["pa_"])
                        S.op("dve", lambda e, n=n: e.tensor_scalar(out=pb_[0:n], in0=pos[0:n], scalar1=c4[0:n, 1:2], scalar2=None, op0=ALU.bitwise_and), reads=p_all + ["c4b"], writes=["pb_"])
                        S.op("dve", lambda e, n=n: e.tensor_copy(out=paf[0:n], in_=pa_[0:n]), reads=["pa_"], writes=["paf"])
                        S.op("dve", lambda e, n=n: e.tensor_copy(out=pbf[0:n], in_=pb_[0:n]), reads=["pb_"], writes=["pbf"])
                        i12v = i12f[:, :, :].rearrange("p (h f) k -> p h f k", f=2)
                        for (pf, pfn, f, dst, dn) in ((paf, "paf", 0, i1f, "i1f"), (pbf, "pbf", 1, i2f, "i2f")):
                            S.op("dve", lambda e, n=n, pf=pf: e.tensor_tensor(
                                out=oh[0:n], in0=pf[0:n].unsqueeze(3).broadcast_to([n, 8, 16, 16]),
                                in1=iota[0:n, 0:16].unsqueeze(1).unsqueeze(1).broadcast_to([n, 8, 16, 16]), op=ALU.is_equal), reads=[pfn, "iota"], writes=["oh"])
                            S.op("dve", lambda e, n=n, f=f, i12v=i12v: e.tensor_tensor(
                                out=oh[0:n], in0=oh[0:n], in1=i12v[0:n, :, f, :].unsqueeze(2).broadcast_to([n, 8, 16, 16]), op=ALU.mult), reads=["oh", "i12f"], writes=["oh"])
                            S.op("dve", lambda e, n=n, dst=dst: e.tensor_reduce(out=dst[0:n], in_=oh[0:n], axis=AX.X, op=ALU.add), reads=["oh"], writes=[dn])
                        if B1CUT < 5:
                            continue
                        S.op("dve", lambda e, n=n: e.tensor_tensor(out=gte[0:n], in0=svt[0:n], in1=svt[0:n, :, 0:1].broadcast_to([n, 8, 16]), op=ALU.subtract), reads=s_all, writes=["gte"])
                        S.op("act", lambda e, n=n: e.activation(out=gte[0:n], in_=gte[0:n], func=AF.Exp), reads=["gte"], writes=["gte"])
                        S.op("dve", lambda e, n=n: e.tensor_reduce(out=zs[0:n], in_=gte[0:n], axis=AX.X, op=ALU.add), reads=["gte"], writes=["zs"])
                        S.op("dve", lambda e, n=n: e.reciprocal(out=zs[0:n], in_=zs[0:n]), reads=["zs"], writes=["zs"])
                        S.op("dve", lambda e, n=n: e.tensor_tensor(out=gte[0:n], in0=gte[0:n], in1=zs[0:n, :].unsqueeze(2).broadcast_to([n, 8, 16]), op=ALU.mult), reads=["gte", "zs"], writes=["gte"])
                        for (srcx, sn, dstx, dn) in ((i1f, "i1f", i1T, "i1T"), (i2f, "i2f", i2T, "i2T"), (gte, "gte", gTt, "gTt")):
                            S.op("pe", lambda e, srcx=srcx, n=n: e.transpose(out=pb[2][:, 0:n], in_=srcx[0:n].rearrange("p h k -> p (h k)"), identity=identf[0:n, 0:n]),
                                 reads=[sn, "identf"], writes=["pb2"])
                            S.op("act", lambda e, dstx=dstx, t0=t0, n=n: e.activation(out=dstx[:, t0:t0 + n], in_=pb[2][:, 0:n], func=AF.Copy), reads=["pb2"], writes=[dn])

            with scope() as b2:
              if STAGE >= 7:
                  TS = 384
                  Wsb = sb([128, TS, 128], BF16, b2)
                  wb0 = wbf[0][:].rearrange("p a b -> p (a b)")
                  wb1 = wbf[1][:].rearrange("p a b -> p (a b)")
                  E1 = wb0[:, 0:2048].rearrange("p (t i) -> p t i", t=16)
                  E2 = wb0[:, 2048:4096].rearrange("p (t i) -> p t i", t=16)
                  G2 = wb1[:, 0:2048].rearrange("p (t i) -> p t i", t=16)
                  NSLOT = 4
                  wsb16 = wst[:].bitcast(BF16).rearrange("p a b -> p (a b)")
                  utb = [wsb16[:, sl * 2048:sl * 2048 + 1024].rearrange("p (k e) -> p k e", k=8) for sl in range(NSLOT)]
                  vtb = [wsb16[:, sl * 2048 + 1024:(sl + 1) * 2048] for sl in range(NSLOT)]
                  ge = [sb([128, TS], F32, b2) for _ in range(2)]
                  WA = [sb([128, TS], BF16, b2) for _ in range(2)]
                  ysb = [sb([128, D], F32, b2) for _ in range(2)]
                  x1r = [sb([128, D], F32, b2)] * 2
                  fg = sb([128, D], F32, b2)
                  junk2 = wb1[:, 2048:3072]
                  ss2 = sb([128, 4], F32, b2)
                  S.dma(lambda e: e.dma_start(out=fg[:], in_=fgb.broadcast_to([128, D])), "const", writes=["fg"])
                  stiles = [(i * TS, TS) for i in range(5)] + [(5 * TS, NT - 5 * TS)]
                  lctr = 0
                  fctr = 0
                  for (s0, T) in stiles:
                      for sbk in range(T // 16):
                          tt0 = sbk * 16
                          S.op("dve", lambda e, s0=s0, tt0=tt0: e.tensor_tensor(
                              out=E1, in0=iota[:, :].unsqueeze(1).broadcast_to([128, 16, 128]),
                              in1=i1T[:, s0 + tt0:s0 + tt0 + 16].unsqueeze(2).broadcast_to([128, 16, 128]), op=ALU.is_equal), reads=["iota", "i1T"], writes=["E1"])
                          S.op("dve", lambda e, s0=s0, tt0=tt0: e.tensor_tensor(
                              out=E2, in0=iota[:, :].unsqueeze(1).broadcast_to([128, 16, 128]),
                              in1=i2T[:, s0 + tt0:s0 + tt0 + 16].unsqueeze(2).broadcast_to([128, 16, 128]), op=ALU.is_equal), reads=["iota", "i2T"], writes=["E2"])
                          S.op("dve", lambda e, s0=s0, tt0=tt0: e.tensor_tensor(
                              out=G2, in0=E2, in1=gTt[:, s0 + tt0:s0 + tt0 + 16].unsqueeze(2).broadcast_to([128, 16, 128]), op=ALU.mult), reads=["E2", "gTt"], writes=["G2"])
                          for q4 in range(4):
                              bk = 6 + q4 % 2
                              for j in range(4):
                                  tl = q4 * 4 + j
                                  S.op("pe", lambda e, bk=bk, j=j, tl=tl: e.matmul(pb[bk][:, j * 128:(j + 1) * 128], lhsT=G2[:, tl, :], rhs=E1[:, tl, :], start=True, stop=True),
                                       reads=["G2", "E1"], writes=["pb%d" % bk])
                              S.op("act", lambda e, bk=bk, tt0=tt0, q4=q4: e.activation(out=Wsb[:, tt0 + q4 * 4:tt0 + q4 * 4 + 4, :], in_=pb[bk][:, :].rearrange("p (t i) -> p t i", t=4), func=AF.Copy),
                                   reads=["pb%d" % bk], writes=["Wsb"])
                      ntile = (T + 127) // 128
                      def ld(c):
                          sl = c % NSLOT
                          S.dma(lambda e, c=c, sl=sl: e.dma_start(out=utb[sl], in_=scr_ut[c]), "utb%d" % sl, reads=["scr_ut%d" % c], writes=["utb%d" % sl])
                          S.dma(lambda e, c=c, sl=sl: e.dma_start(out=vtb[sl], in_=scr_v[c]), "vtb%d" % sl, reads=["scr_v%d" % c], writes=["vtb%d" % sl])

                      def actp(c):
                          sl = c % NSLOT
                          k = c % 2
                          for kc in range(8):
                              S.op("pe", lambda e, k=k, kc=kc, sl=sl, s0=s0, T=T: e.matmul(pb[6 + k][:, 0:T], lhsT=utb[sl][:, kc, :], rhs=h2T[:, kc, s0:s0 + T], start=(kc == 0), stop=(kc == 7)),
                                   reads=["utb%d" % sl] + h2_all, writes=["pb%d" % (6 + k)])
                          S.op("act", lambda e, k=k, T=T: e.activation(out=ge[k][:, 0:T], in_=pb[6 + k][:, 0:T], func=AF.Gelu), reads=["pb%d" % (6 + k)], writes=["ge%d" % k])
                          S.op("dve", lambda e, k=k, T=T, c=c: e.tensor_tensor(out=WA[k][:, 0:T], in0=ge[k][:, 0:T], in1=Wsb[:, 0:T, c], op=ALU.mult), reads=["ge%d" % k, "Wsb"], writes=["WA%d" % k])

                      def vp(c):
                          sl = c % NSLOT
                          k = c % 2
                          for ti in range(ntile):
                              n = min(128, T - ti * 128)
                              for half in range(2):
                                  S.op("pe", lambda e, ti=ti, half=half, n=n, k=k, sl=sl, c=c: e.matmul(pb[2 * ti + half][0:n, :], lhsT=WA[k][:, ti * 128:ti * 128 + n], rhs=vtb[sl][:, half * 512:(half + 1) * 512], start=(c == 0), stop=(c == 127)),
                                       reads=["WA%d" % k, "vtb%d" % sl], writes=["pb%d" % (2 * ti + half)])

                      for c in range(NSLOT - 1):
                          ld(c)
                      actp(0)
                      for c in range(128):
                          if c + NSLOT - 1 < 128:
                              ld(c + NSLOT - 1)
                          if c + 1 < 128:
                              actp(c + 1)
                          vp(c)
                      for ti in range(ntile):
                          n = min(128, T - ti * 128)
                          t0 = s0 + ti * 128
                          k = fctr % 2
                          fctr += 1
                          tix = t0 // 128
                          S.dma(lambda e, k=k, t0=t0, n=n: e.dma_start(out=x1r[k][0:n, :], in_=scr_x1[t0:t0 + n, :]), "x1r", reads=["x1dram.%d" % tix], writes=["x1r"])
                          for half in range(2):
                              S.op("dve", lambda e, k=k, n=n, ti=ti, half=half: e.tensor_tensor(out=ysb[k][0:n, half * 512:(half + 1) * 512], in0=x1r[k][0:n, half * 512:(half + 1) * 512], in1=pb[2 * ti + half][0:n, :], op=ALU.add),
                                   reads=["x1r", "pb%d" % (2 * ti + half)], writes=["ysb%d" % k])
                          S.op("dve", lambda e: e.memset(ss2[:, 0:1], 0.0), writes=["ss2"])
                          S.op("act", lambda e, k=k, n=n: e.activation(out=junk2[0:n, :], in_=ysb[k][0:n, :], func=AF.Square, accum_out=ss2[0:n, 0:1]), reads=["ysb%d" % k, "ss2"], writes=["junk2", "ss2"])
                          S.op("act", lambda e, n=n: e.activation(out=ss2[0:n, 1:2], in_=ss2[0:n, 0:1], func=AF.Sqrt, bias=epst[0:n, :], scale=1.0 / D), reads=["ss2", "eps"], writes=["ss21"])
                          S.op("dve", lambda e, n=n: e.reciprocal(out=ss2[0:n, 2:3], in_=ss2[0:n, 1:2]), reads=["ss21"], writes=["ss22"])
                          S.op("dve", lambda e, k=k, n=n: e.scalar_tensor_tensor(out=ysb[k][0:n, :], in0=ysb[k][0:n, :], scalar=ss2[0:n, 2:3], in1=fg[0:n, :], op0=ALU.mult, op1=ALU.mult),
                               reads=["ysb%d" % k, "ss22", "fg"], writes=["ysb%d" % k])
                          dst = y_p[t0:t0 + n, :] if t0 < SEQ else y_s[:, :]
                          S.dma(lambda e, k=k, n=n, dst=dst: e.dma_start(out=dst, in_=ysb[k][0:n, :]), "yst%d" % k, reads=["ysb%d" % k], final=True)

        with nc.Block() as block:
            S.emit(block)
    return nc


_PROG = None


def kernel(x_prompt, x_sample, cache_kv_w128, cache_kv_w512, cache_kv_w2048, state_conv,
           norm1_g, w_in, conv_w, w_out_a, w_out_b, w_o, norm2_g,
           peer_wq, peer_keys, peer_u, peer_v, final_g):
    global _PROG
    if _PROG is None:
        _PROG = build_program()
    nc = _PROG
    f = lambda a: np.ascontiguousarray(np.asarray(a, dtype=np.float32))
    consts = _consts()
    shared = dict(
        g1T=f(np.asarray(norm1_g)[0].reshape(8, 128).T),
        g2T=f(np.asarray(norm2_g)[0].reshape(8, 128).T),
        w_in=f(np.asarray(w_in)[0]),
        convw=f(np.asarray(conv_w)[0].reshape(3, 8, 128).transpose(2, 1, 0)),
        w_out_a=f(np.asarray(w_out_a)[0]),
        w_out_b=f(np.asarray(w_out_b)[0]),
        w_o=f(np.asarray(w_o)[0]),
        wq=f(np.asarray(peer_wq)[0]),
        keysT=f(np.asarray(peer_keys)[0].transpose(1, 0, 2, 3).reshape(16, 128, 128).transpose(2, 0, 1)),
        uT=f(np.asarray(peer_u)[0].T),
        vtab=f(np.asarray(peer_v)[0]),
        fgb=f(np.asarray(final_g).reshape(1, D)),
    )
    shared.update(consts)
    caches = [np.asarray(cache_kv_w128)[0], np.asarray(cache_kv_w512)[0], np.asarray(cache_kv_w2048)[0]]
    xpr = np.asarray(x_prompt)
    xsa = np.asarray(x_sample)
    stc = np.asarray(state_conv)[0]
    in_maps = []
    for c in range(NCORES):
        m = dict(shared)
        m["xp"] = f(xpr[c])
        m["xs"] = f(xsa[c * 16:(c + 1) * 16].reshape(NS, D))
        for g in range(3):
            m["cache%d" % g] = f(caches[g][c * 16:(c + 1) * 16])
        m["stconv"] = f(stc[c * 16:(c + 1) * 16].reshape(32, D))
        in_maps.append(m)
    ncr = int(os.environ.get("MK_CORES", str(NCORES)))
    res = run_bass_kernel_spmd(nc, in_maps[0:ncr], core_ids=list(range(ncr)))
    R = list(res.results)
    while len(R) < NCORES:
        R.append(R[0])
    y_prompt = np.stack([R[c]["y_p"] for c in range(NCORES)], 0)
    y_sample = np.concatenate([R[c]["y_s"].reshape(16, 4, D) for c in range(NCORES)], 0)
    kvp = [np.stack([R[c]["kvp%d" % g] for c in range(NCORES)], 0)[None] for g in range(3)]
    convp = np.stack([R[c]["conv_p"] for c in range(NCORES)], 0)[None]
    kvs = [np.concatenate([R[c]["kvs%d" % g] for c in range(NCORES)], 0)[None] for g in range(3)]
    convs = np.concatenate([R[c]["conv_s"].reshape(16, 2, D) for c in range(NCORES)], 0)[None]
    return (y_prompt.astype(np.float32), y_sample.astype(np.float32), kvp[0], kvp[1], kvp[2], convp,
            kvs[0], kvs[1], kvs[2], convs)
```

```python
import os
import numpy as np
from contextlib import ExitStack, contextmanager
import concourse.bass as bass
import concourse.mybir as mybir
from concourse.bass_utils import run_bass_kernel_spmd
import ml_dtypes

F32 = mybir.dt.float32
BF16 = mybir.dt.bfloat16
U32 = mybir.dt.uint32
ALU = mybir.AluOpType
AF = mybir.ActivationFunctionType
AX = mybir.AxisListType

NCORES = 8
D = 1024
SEQ = 2048
NS = 64
NT = SEQ + NS
PROJ = 9728
GROUPS = ((128, 1), (512, 4), (2048, 16))
EPS = 1e-6
NEXP = 16384
ENGS = ("pe", "act", "dve", "pool", "sp")
STAGE = int(os.environ.get("MK_STAGE", "99"))
B1CUT = int(os.environ.get("MK_B1CUT", "99"))
CPY_ENG = os.environ.get("MK_CPYENG", "pe")
B1SUB = int(os.environ.get("MK_B1SUB", "99"))


class Sched:
    def __init__(self, nc, n_dma_sems=120):
        self.nc = nc
        self.streams = {e: [] for e in ENGS}
        self.count = {e: 0 for e in ENGS}
        self.last_w = {}
        self.readers = {}
        self.waited = {e: {} for e in ENGS}
        self.psem = {}
        self.dsems = {}
        self.dcount = {}
        self.n_dma_sems = n_dma_sems
        self.final_events = []
        self.pending = {e: [] for e in ENGS}

    def barrier(self):
        evs = [(e, self.count[e]) for e in ("pe", "act", "dve", "pool") if self.count[e] > 0]
        evs += [(("d", k), v) for k, v in self.dcount.items() if v > 0 and not str(k).startswith("cpy")]
        for eng in ENGS:
            for s_, v in evs:
                if s_ == eng:
                    continue
                if self.waited[eng].get(s_, 0) >= v:
                    continue
                self.waited[eng][s_] = v
                self.pending[eng].append((s_, v))

    def alloc(self, stack):
        for e in ("pe", "act", "dve", "pool"):
            self.psem[e] = stack.enter_context(self.nc.semaphore("p_" + e))
        self.stack = stack

    def dsem(self, key):
        if key not in self.dsems:
            assert len(self.dsems) < self.n_dma_sems, "too many dma sems"
            self.dsems[key] = self.stack.enter_context(self.nc.semaphore("d%d" % len(self.dsems)))
            self.dcount[key] = 0
        return self.dsems[key]

    def _deps(self, eng, reads, writes):
        ev = {}

        def add(e):
            if e is None:
                return
            s, v = e
            if ev.get(s, 0) < v:
                ev[s] = v

        for t in reads:
            add(self.last_w.get(t))
        for t in writes:
            add(self.last_w.get(t))
            for r in self.readers.get(t, ()):
                add(r)
        out = []
        for s, v in ev.items():
            if eng == "pe" and s == "pe":
                continue
            if self.waited[eng].get(s, 0) >= v:
                continue
            self.waited[eng][s] = v
            out.append((s, v))
        return out

    def _commit(self, event, reads, writes):
        for t in reads:
            self.readers.setdefault(t, []).append(event)
        for t in writes:
            self.last_w[t] = event
            self.readers[t] = []

    def op(self, eng, fn, reads=(), writes=()):
        waits = self.pending[eng] + self._deps(eng, reads, writes)
        self.pending[eng] = []
        self.count[eng] += 1
        event = (eng, self.count[eng])
        self.streams[eng].append((waits, fn, ("p", eng)))
        self._commit(event, reads, writes)
        return event

    def dma(self, fn, key, reads=(), writes=(), eng="sp", final=False):
        self.dsem(key)
        waits = self.pending[eng] + self._deps(eng, reads, writes)
        self.pending[eng] = []
        self.dcount[key] += 16
        event = (("d", key), self.dcount[key])
        self.streams[eng].append((waits, fn, ("d", key)))
        self._commit(event, reads, writes)
        if final:
            self.final_events.append(event)
        return event

    def _sem(self, s):
        if isinstance(s, tuple):
            return self.dsems[s[1]]
        return self.psem[s]

    def emit(self, block):
        S = self
        fin = {}
        for s, v in self.final_events:
            if fin.get(s, 0) < v:
                fin[s] = v

        def run(engname, engobj, extra_final=False):
            for waits, fn, inc in S.streams[engname]:
                for s, v in waits:
                    engobj.wait_ge(S._sem(s), v)
                ins = fn(engobj)
                if inc[0] == "p":
                    ins.then_inc(S.psem[inc[1]], 1)
                else:
                    ins.then_inc(S.dsems[inc[1]], 16)
            if extra_final:
                for s, v in fin.items():
                    engobj.wait_ge(S._sem(s), v)

        @block.sync
        def _(e):
            run("sp", e, True)

        @block.tensor
        def _(e):
            run("pe", e)

        @block.scalar
        def _(e):
            run("act", e)

        @block.vector
        def _(e):
            run("dve", e)

        @block.gpsimd
        def _(e):
            run("pool", e)


def _consts():
    slopes = np.exp2(-8.0 * np.arange(1, 9, dtype=np.float64) / 8.0)
    kj = np.arange(128)[:, None].astype(np.float64)
    qi = np.arange(128)[None, :].astype(np.float64)
    masks = np.zeros((128, 24, 2, 128), np.float32)
    for h in range(8):
        for g, (win, dil) in enumerate(GROUPS):
            dist_d = qi - kj
            md = np.where(dist_d >= 0, np.exp(-slopes[h] * dil * dist_d), 0.0)
            dist_p = 128 + qi - kj
            mp = np.where(dist_p <= 128, np.exp(-slopes[h] * dil * dist_p), 0.0)
            masks[:, h * 3 + g, 0, :] = mp
            masks[:, h * 3 + g, 1, :] = md
    sbias = np.zeros((128, 3, 129), np.float32)
    j = np.arange(129, dtype=np.float64)
    for p in range(128):
        s = p % 8
        for g, (win, dil) in enumerate(GROUPS):
            sbias[p, g, :] = -slopes[s] * dil * (128.0 - j)
    iota = np.tile(np.arange(128, dtype=np.float32)[None, :], (128, 1))
    return dict(
        c_identf=np.eye(128, dtype=np.float32),
        c_identb=np.eye(128, dtype=np.float32).astype(ml_dtypes.bfloat16),
        c_masks=masks.astype(ml_dtypes.bfloat16),
        c_sbias=sbias,
        c_iota=iota,
    )


def build_program():
    nc = bass.Bass("TRN2", target_bir_lowering=False)

    def din(name, shape, dt=F32):
        return nc.dram_tensor(name, list(shape), dt, kind="ExternalInput").ap()

    def dout(name, shape, dt=F32):
        return nc.dram_tensor(name, list(shape), dt, kind="ExternalOutput").ap()

    def dscr(name, shape, dt=F32):
        return nc.dram_tensor(name, list(shape), dt, kind="Internal").ap()

    xp = din("xp", [SEQ, D])
    xs = din("xs", [NS, D])
    caches = [din("cache%d" % g, [16, GROUPS[g][0], 2, 8, 64]) for g in range(3)]
    stconv = din("stconv", [32, D])
    g1T = din("g1T", [128, 8])
    g2T = din("g2T", [128, 8])
    w_in = din("w_in", [D, PROJ])
    convw = din("convw", [128, 8, 3])
    w_out_a = din("w_out_a", [D, D])
    w_out_b = din("w_out_b", [512, D])
    w_o = din("w_o", [D, D])
    wq = din("wq", [D, 2048])
    keysT = din("keysT", [128, 16, 128])
    uT = din("uT", [D, NEXP])
    vtab = din("vtab", [NEXP, D])
    fgb = din("fgb", [1, D])
    c_identf = din("c_identf", [128, 128])
    c_identb = din("c_identb", [128, 128], BF16)
    c_masks = din("c_masks", [128, 24, 2, 128], BF16)
    c_sbias = din("c_sbias", [128, 3, 129])
    c_iota = din("c_iota", [128, 128])

    y_p = dout("y_p", [SEQ, D])
    y_s = dout("y_s", [NS, D])
    kvp = [dout("kvp%d" % g, [min(GROUPS[g][0], SEQ), 2, 8, 64]) for g in range(3)]
    conv_p = dout("conv_p", [2, D])
    kvs = [dout("kvs%d" % g, [16, GROUPS[g][0], 2, 8, 64]) for g in range(3)]
    conv_s = dout("conv_s", [32, D])

    dbg_ot = None
    scr_q = dscr("scr_q", [NS, 1536])
    scr_o = dscr("scr_o", [NS, 512])
    scr_x1 = dscr("scr_x1", [NT, D])
    scr_ut = dscr("scr_ut", [128, 128, 8, 128], BF16)
    scr_v = dscr("scr_v", [128, 128, D], BF16)

    with ExitStack() as st:
        S = Sched(nc)
        S.alloc(st)

        @contextmanager
        def scope():
            with ExitStack() as es:
                yield es
                S.barrier()
        cnt = [0]

        def sb(shape, dt, stack=st):
            cnt[0] += 1
            return stack.enter_context(nc.sbuf_tensor("t%d" % cnt[0], list(shape), dt))

        pb = [st.enter_context(nc.psum_tensor("pb%d" % i, [128, 512], F32)) for i in range(8)]

        identf = sb([128, 128], F32)
        identb = sb([128, 128], BF16)
        iota = sb([128, 128], F32)
        g1t = sb([128, 8], F32)
        g2t = sb([128, 8], F32)
        cwt = sb([128, 8, 3], F32)
        epst = sb([128, 1], F32)
        for (t, src, nm) in ((identf, c_identf, "identf"), (identb, c_identb, "identb"), (iota, c_iota, "iota"),
                             (g1t, g1T, "g1t"), (g2t, g2T, "g2t"), (cwt, convw, "cwt")):
            S.dma(lambda e, t=t, src=src: e.dma_start(out=t[:], in_=src), "const", writes=[nm])
        S.op("dve", lambda e: e.memset(epst[:], EPS), writes=["eps"])

        wst = sb([128, 8, 512], F32)
        wbf = [sb([128, 8, 512], BF16) for _ in range(2)]
        wctr = [0]

        def load_w(pieces, rows=128):
            slot = wctr[0] % 2
            wctr[0] += 1
            off = 0
            toks = []
            for i, (src, kcn) in enumerate(pieces):
                n = src.shape[-1]
                r = src.shape[0] // kcn
                srcv = src.rearrange("(kc p) c -> p kc c", p=r)
                tok = "wst.%d" % i
                S.dma(lambda e, srcv=srcv, off=off, n=n, r=r, kcn=kcn: e.dma_start(out=wst[0:r, 0:kcn, off:off + n], in_=srcv),
                      "wst", writes=[tok])
                toks.append(tok)
                off += n
            S.op("pool", lambda e, slot=slot, off=off: e.tensor_copy(out=wbf[slot][:, :, 0:off], in_=wst[:, :, 0:off]),
                 reads=toks, writes=["wbf%d" % slot] + ["wstall"])
            for tok in ["wst.%d" % i for i in range(8)]:
                S.readers.setdefault(tok, []).append(S.last_w["wbf%d" % slot])
            return wbf[slot], "wbf%d" % slot

        tiles = [(i * 128, 128) for i in range(16)] + [(SEQ, NS)]
        tgroups = [(i * 512, 512) for i in range(4)] + [(SEQ, NS)]

        def x_src(t0, n):
            return xp[t0:t0 + n, :] if t0 < SEQ else xs[:, :]

        def rmsnorm_T(src_fn, gt, gname, dstT, dst_tok, xts, hb, junk, ss, stack_tag):
            for i, (t0, n) in enumerate(tiles):
                k = i % 2
                xt = xts[k]
                S.dma(lambda e, xt=xt, t0=t0, n=n: e.dma_start(out=xt[0:n, :], in_=src_fn(t0, n)), stack_tag + "x%d" % k,
                      reads=["x1dram.%d" % i] if stack_tag == "n2" else [], writes=["xt%d" % k])
                S.op("dve", lambda e: e.memset(ss[:, 0:1], 0.0), writes=["ss"])
                S.op("act", lambda e, xt=xt, n=n: e.activation(out=junk[0:n, :], in_=xt[0:n, :], func=AF.Square, accum_out=ss[0:n, 0:1]),
                     reads=["xt%d" % k, "ss"], writes=["junk", "ss"])
                S.op("act", lambda e, n=n: e.activation(out=ss[0:n, 1:2], in_=ss[0:n, 0:1], func=AF.Sqrt, bias=epst[0:n, :], scale=1.0 / D),
                     reads=["ss", "eps"], writes=["ss1"])
                S.op("dve", lambda e, n=n: e.reciprocal(out=ss[0:n, 2:3], in_=ss[0:n, 1:2]), reads=["ss1"], writes=["ss2"])
                S.op("dve", lambda e, xt=xt, n=n: e.tensor_scalar(out=hb[0:n, :], in0=xt[0:n, :], scalar1=ss[0:n, 2:3], scalar2=None, op0=ALU.mult),
                     reads=["xt%d" % k, "ss2"], writes=["hb"])
                pt = pb[k][:].bitcast(BF16)
                for kc in range(8):
                    S.op("pe", lambda e, pt=pt, kc=kc, n=n: e.transpose(out=pt[:, kc * 128:kc * 128 + n], in_=hb[0:n, kc * 128:(kc + 1) * 128], identity=identb[0:n, 0:n]),
                         reads=["hb", "identb"], writes=["pb%d" % k])
                S.op("dve", lambda e, pt=pt, t0=t0, n=n: e.tensor_tensor(
                    out=dstT[:, :, t0:t0 + n], in0=pt.rearrange("p (k t) -> p k t", k=8)[:, :, 0:n],
                    in1=gt[:, :].unsqueeze(2).broadcast_to([128, 8, n]), op=ALU.mult),
                    reads=["pb%d" % k, gname], writes=[dst_tok + ".%d" % i] + (["hT.%d" % i] if dst_tok != "hT" else []))

        hT = sb([128, 8, NT], BF16)
        hT_all = ["hT.%d" % i for i in range(17)]

        with scope() as pa:
            uf, vf, ub, vb = [], [], [], []
            prep_c = [0]

            def prep_chunk():
                c = prep_c[0]
                if c >= 128 or STAGE < 6:
                    return
                prep_c[0] += 1
                k = 0
                S.dma(lambda e, c=c, k=k: e.dma_start(out=uf[k][:], in_=uT[:, c * 128:(c + 1) * 128].rearrange("(kc p) e -> p kc e", p=128)),
                      "uf%d" % k, writes=["uf%d" % k])
                S.dma(lambda e, c=c, k=k: e.dma_start(out=vf[k][:], in_=vtab[c * 128:(c + 1) * 128, :]),
                      "vf%d" % k, writes=["vf%d" % k])
                S.op("act", lambda e, k=k: e.activation(out=ub[k][:], in_=uf[k][:], func=AF.Copy), reads=["uf%d" % k], writes=["ub%d" % k])
                S.op("pool", lambda e, k=k: e.tensor_copy(out=vb[k][:], in_=vf[k][:]), reads=["vf%d" % k], writes=["vb%d" % k])
                S.dma(lambda e, c=c, k=k: e.dma_start(out=scr_ut[c], in_=ub[k][:]), "ubs%d" % k, reads=["ub%d" % k], writes=["scr_ut%d" % c])
                S.dma(lambda e, c=c, k=k: e.dma_start(out=scr_v[c], in_=vb[k][:]), "vbs%d" % k, reads=["vb%d" % k], writes=["scr_v%d" % c])

            with scope() as pa1:
                OT = sb([64, 8, NT], BF16, pa1)
                with scope() as s1:
                    xts = [sb([128, D], F32, s1) for _ in range(2)]
                    hb = sb([128, D], BF16, s1)
                    junk = sb([128, D], BF16, s1)
                    ss = sb([128, 4], F32, s1)
                    rmsnorm_T(x_src, g1t, "g1t", hT, "hT", xts, hb, junk, ss, "n1")

                if STAGE >= 2:
                    with scope() as s2:
                        skv = sb([NS, 512], F32, s2)
                        pst = [sb([128, 512], F32, s2) for _ in range(2)]
                        pctr = 0
                        for typ in range(3):
                            for g in range(3):
                                W = GROUPS[g][0]
                                c0 = 3072 + typ * 1536 + g * 512
                                wt, wtok = load_w([(w_in[:, c0:c0 + 512], 8)])
                                for kc in range(8):
                                    S.op("pe", lambda e, wt=wt, kc=kc: e.matmul(pb[2][0:NS, :], lhsT=hT[:, kc, SEQ:NT], rhs=wt[:, kc, :], start=(kc == 0), stop=(kc == 7)),
                                         reads=[wtok, "hT.16"], writes=["pb2"])
                                S.op("act", lambda e: e.activation(out=skv[:], in_=pb[2][0:NS, :], func=AF.Copy), reads=["pb2"], writes=["skv"])
                                for b in range(16):
                                    if typ == 0:
                                        S.dma(lambda e, b=b, g=g: e.dma_start(out=scr_q[b * 4:(b + 1) * 4, g * 512:(g + 1) * 512], in_=skv[b * 4:(b + 1) * 4, :]),
                                              "skvst", reads=["skv"], writes=["scr_q.%d.%d" % (g, b)])
                                    else:
                                        S.dma(lambda e, b=b, g=g, W=W, typ=typ: e.dma_start(
                                            out=kvs[g][b, W - 4:W, typ - 1].rearrange("t s d -> t (s d)"), in_=skv[b * 4:(b + 1) * 4, :]),
                                            "skvst", reads=["skv"], writes=["kvsnew.%d.%d.%d" % (g, typ, b)], final=True)
                                if typ == 0:
                                    continue
                                keep = min(W, SEQ) // 128
                                for ti in range(16 - keep, 16):
                                    k = pctr % 2
                                    pctr += 1
                                    for kc in range(8):
                                        S.op("pe", lambda e, wt=wt, kc=kc, ti=ti, k=k: e.matmul(pb[3 + k][:, :], lhsT=hT[:, kc, ti * 128:(ti + 1) * 128], rhs=wt[:, kc, :], start=(kc == 0), stop=(kc == 7)),
                                             reads=[wtok, "hT.%d" % ti], writes=["pb%d" % (3 + k)])
                                    S.op("act", lambda e, k=k: e.activation(out=pst[k][:], in_=pb[3 + k][:], func=AF.Copy), reads=["pb%d" % (3 + k)], writes=["pst%d" % k])
                                    r0 = (ti - (16 - keep)) * 128
                                    S.dma(lambda e, g=g, r0=r0, typ=typ, k=k: e.dma_start(out=kvp[g][r0:r0 + 128, typ - 1].rearrange("t s d -> t (s d)"), in_=pst[k][:]),
                                          "pst%d" % k, reads=["pst%d" % k], final=True)

                if STAGE >= 3:
                    with scope() as s3:
                        Kt = sb([128, 132, 64], F32, s3)
                        Vt = sb([128, 132, 64], F32, s3)
                        prod = sb([128, 43, 64], F32, s3)
                        qs = sb([128, 4, 3, 64], F32, s3)
                        knew = sb([128, 3, 4, 64], F32, s3)
                        vnew = sb([128, 3, 4, 64], F32, s3)
                        sbias = sb([128, 3, 129], F32, s3)
                        sc = sb([128, 129], F32, s3)
                        ex = sb([128, 129], F32, s3)
                        lacc = sb([128, 12], F32, s3)
                        og = sb([128, 12, 64], F32, s3)
                        osum = sb([128, 4, 64], F32, s3)
                        lsum = sb([128, 4], F32, s3)
                        otok = sb([NS, 512], F32, s3)
                        S.dma(lambda e: e.dma_start(out=sbias[:], in_=c_sbias), "const", writes=["sbias"])
                        S.op("dve", lambda e: e.memset(lacc[:], 0.0), writes=["lacc.%d" % c for c in range(12)])
                        for b in range(16):
                            S.dma(lambda e, b=b: e.dma_start(out=qs[b * 8:(b + 1) * 8], in_=scr_q[b * 4:(b + 1) * 4, :].rearrange("t (g s d) -> s t g d", g=3, s=8)),
                                  "qs", reads=["scr_q.%d.%d" % (g, b) for g in range(3)], writes=["qs.%d" % b])
                            for g in range(3):
                                W = GROUPS[g][0]
                                S.dma(lambda e, b=b, g=g, W=W: e.dma_start(out=knew[b * 8:(b + 1) * 8, g], in_=kvs[g][b, W - 4:W, 0].rearrange("t s d -> s t d")),
                                      "qs", reads=["kvsnew.%d.1.%d" % (g, b)], writes=["knew.%d.%d" % (g, b)])
                                S.dma(lambda e, b=b, g=g, W=W: e.dma_start(out=vnew[b * 8:(b + 1) * 8, g], in_=kvs[g][b, W - 4:W, 1].rearrange("t s d -> s t d")),
                                      "qs", reads=["kvsnew.%d.2.%d" % (g, b)], writes=["vnew.%d.%d" % (g, b)])
                        qs_all = ["qs.%d" % b for b in range(16)]
                        kn_all = ["knew.%d.%d" % (g, b) for g in range(3) for b in range(16)]
                        vn_all = ["vnew.%d.%d" % (g, b) for g in range(3) for b in range(16)]
                        for g in range(3):
                            W, dil = GROUPS[g]
                            for t in range(4):
                                col = g * 4 + t
                                if g == 0 and t > 0:
                                    pass
                                else:
                                    for b in range(16):
                                        if g == 0:
                                            ksrc = caches[0][b, :, 0].rearrange("r s d -> s r d")
                                            vsrc = caches[0][b, :, 1].rearrange("r s d -> s r d")
                                        else:
                                            ksrc = caches[g][b, :, 0].rearrange("(j r) s d -> s r j d", r=dil)[:, t, 0:128, :]
                                            vsrc = caches[g][b, :, 1].rearrange("(j r) s d -> s r j d", r=dil)[:, t, 0:128, :]
                                        S.dma(lambda e, b=b, ksrc=ksrc: e.dma_start(out=Kt[b * 8:(b + 1) * 8, 0:128, :], in_=ksrc), "Kt", writes=["Kt.%d" % b])
                                        S.dma(lambda e, b=b, vsrc=vsrc: e.dma_start(out=Vt[b * 8:(b + 1) * 8, 0:128, :], in_=vsrc), "Vt", writes=["Vt.%d" % b], eng=os.environ.get("MK_VENG", "act"))
                                    if g == 0:
                                        S.op("pool", lambda e: e.tensor_copy(out=Kt[:, 128:132, :], in_=knew[:, 0]), reads=kn_all, writes=["Ktn"])
                                        S.op("pool", lambda e: e.tensor_copy(out=Vt[:, 128:132, :], in_=vnew[:, 0]), reads=vn_all, writes=["Vtn"])
                                    else:
                                        S.op("pool", lambda e, g=g, t=t: e.tensor_copy(out=Kt[:, 128:129, :], in_=knew[:, g, t:t + 1, :]), reads=kn_all, writes=["Ktn"])
                                        S.op("pool", lambda e, g=g, t=t: e.tensor_copy(out=Vt[:, 128:129, :], in_=vnew[:, g, t:t + 1, :]), reads=vn_all, writes=["Vtn"])
                                w0 = t if g == 0 else 0
                                kt_all = ["Kt.%d" % b for b in range(16)] + ["Ktn"]
                                vt_all = ["Vt.%d" % b for b in range(16)] + ["Vtn"]
                                for ch in range(3):
                                    k0 = ch * 43
                                    S.op("dve", lambda e, w0=w0, k0=k0, t=t, g=g: e.tensor_tensor(
                                        out=prod[:], in0=Kt[:, w0 + k0:w0 + k0 + 43, :],
                                        in1=qs[:, t, g, :].unsqueeze(1).broadcast_to([128, 43, 64]), op=ALU.mult),
                                        reads=kt_all + qs_all, writes=["prod"])
                                    S.op("dve", lambda e, k0=k0: e.tensor_reduce(out=sc[:, k0:k0 + 43], in_=prod[:], axis=AX.X, op=ALU.add),
                                         reads=["prod"], writes=["sc.%d" % ch])
                                S.op("dve", lambda e, g=g: e.scalar_tensor_tensor(out=ex[:], in0=sc[:], scalar=0.125, in1=sbias[:, g, :], op0=ALU.mult, op1=ALU.add),
                                     reads=["sc.0", "sc.1", "sc.2", "sbias"], writes=["ex"])
                                S.op("act", lambda e, col=col: e.activation(out=sc[:], in_=ex[:], func=AF.Exp, accum_out=lacc[:, col:col + 1]),
                                     reads=["ex", "lacc.%d" % col], writes=["sc.0", "sc.1", "sc.2", "lacc.%d" % col])
                                for ch in range(3):
                                    k0 = ch * 43
                                    S.op("dve", lambda e, w0=w0, k0=k0: e.tensor_tensor(
                                        out=prod[:], in0=Vt[:, w0 + k0:w0 + k0 + 43, :],
                                        in1=sc[:, k0:k0 + 43].unsqueeze(2).broadcast_to([128, 43, 64]), op=ALU.mult),
                                        reads=vt_all + ["sc.0", "sc.1", "sc.2"], writes=["prod"])
                                    dst = og[:, col, :] if ch == 0 else ex[:, ch * 64 - 64:ch * 64]
                                    S.op("dve", lambda e, dst=dst: e.tensor_reduce(out=dst, in_=prod[:].rearrange("p k d -> p d k"), axis=AX.X, op=ALU.add),
                                         reads=["prod"], writes=["ogp.0"] if ch == 0 else ["ex"])
                                S.op("dve", lambda e, col=col: e.tensor_tensor(out=og[:, col, :], in0=og[:, col, :], in1=ex[:, 0:64], op=ALU.add),
                                     reads=["ogp.0", "ex"], writes=["ogp.0"])
                                S.op("dve", lambda e, col=col: e.tensor_tensor(out=og[:, col, :], in0=og[:, col, :], in1=ex[:, 64:128], op=ALU.add),
                                     reads=["ogp.0", "ex"], writes=["ogp.0", "og.%d" % col])
                        og_all = ["og.%d" % c for c in range(12)]
                        la_all = ["lacc.%d" % c for c in range(12)]
                        S.op("dve", lambda e: e.tensor_tensor(out=osum[:], in0=og[:, 0:4, :], in1=og[:, 4:8, :], op=ALU.add), reads=og_all, writes=["osum"])
                        S.op("dve", lambda e: e.tensor_tensor(out=osum[:], in0=osum[:], in1=og[:, 8:12, :], op=ALU.add), reads=og_all + ["osum"], writes=["osum"])
                        S.op("dve", lambda e: e.tensor_tensor(out=lsum[:], in0=lacc[:, 0:4], in1=lacc[:, 4:8], op=ALU.add), reads=la_all, writes=["lsum"])
                        S.op("dve", lambda e: e.tensor_tensor(out=lsum[:], in0=lsum[:], in1=lacc[:, 8:12], op=ALU.add), reads=la_all + ["lsum"], writes=["lsum"])
                        S.op("dve", lambda e: e.reciprocal(out=lsum[:], in_=lsum[:]), reads=["lsum"], writes=["lsum"])
                        S.op("dve", lambda e: e.tensor_tensor(out=osum[:], in0=osum[:], in1=lsum[:, :].unsqueeze(2).broadcast_to([128, 4, 64]), op=ALU.mult),
                             reads=["osum", "lsum"], writes=["osum"])
                        for b in range(16):
                            S.dma(lambda e, b=b: e.dma_start(out=scr_o[b * 4:(b + 1) * 4, :].rearrange("t (s d) -> s t d", s=8), in_=osum[b * 8:(b + 1) * 8]),
                                  "scro", reads=["osum"], writes=["scr_o.%d" % b])
                        S.dma(lambda e: e.dma_start(out=otok[:], in_=scr_o), "otok", reads=["scr_o.%d" % b for b in range(16)], writes=["otok"])
                        for h in range(8):
                            S.op("pe", lambda e, h=h: e.transpose(out=pb[2][0:64, h * 64:(h + 1) * 64], in_=otok[:, h * 64:(h + 1) * 64], identity=identf[0:NS, 0:NS]),
                                 reads=["otok", "identf"], writes=["pb2"])
                        S.op("act", lambda e: e.activation(out=OT[:, :, SEQ:NT], in_=pb[2][0:64, :].rearrange("p (h t) -> p h t", h=8), func=AF.Copy),
                             reads=["pb2"], writes=["OTs"])

                uf.extend([sb([128, 8, 128], F32, pa1)] * 2)
                vf.extend([sb([128, D], F32, pa1)] * 2)
                ub.extend([sb([128, 8, 128], BF16, pa1)] * 2)
                vb.extend([sb([128, D], BF16, pa1)] * 2)
                if STAGE >= 4:
                    with scope() as s4:
                        masks = sb([128, 24, 2, 128], BF16, s4)
                        QT = sb([64, 3, SEQ], BF16, s4)
                        KT = sb([64, 3, SEQ], BF16, s4)
                        Vh = sb([128, 3, 16, 128], BF16, s4)
                        acc = sb([128, SEQ], F32, s4)
                        rc = sb([64, 512], F32, s4)
                        ebuf = [sb([128, 256], F32, s4) for _ in range(3)]
                        pT = [sb([128, 256], BF16, s4) for _ in range(3)]
                        S.dma(lambda e: e.dma_start(out=masks[:], in_=c_masks), "const", writes=["masks"])
                        S.op("pool", lambda e: e.memset(Vh[:], 1.0), writes=["Vh"])

                        def permv(ap2048, dil):
                            return ap2048.rearrange("p (j r) -> p r j", r=dil)

                        uctr = 0
                        for h in range(8):
                            pieces = []
                            for typ in range(3):
                                for g in range(3):
                                    c0 = 3072 + typ * 1536 + g * 512 + h * 64
                                    pieces.append((w_in[:, c0:c0 + 64], 8))
                            wt, wtok = load_w([pieces[0], pieces[3], pieces[1], pieces[4], pieces[2], pieces[5]])
                            wv, wvtok = load_w(pieces[6:9])
                            for g in range(3):
                                dil = GROUPS[g][1]
                                for tg in range(4):
                                    bk = 2 + (tg % 2)
                                    for kc in range(8):
                                        if dil == 1:
                                            rhs = hT[:, kc, tg * 512:(tg + 1) * 512]
                                        elif dil == 4:
                                            rhs = permv(hT[:, kc, 0:SEQ], 4)[:, tg, :]
                                        else:
                                            rhs = permv(hT[:, kc, 0:SEQ], 16)[:, tg * 4:(tg + 1) * 4, :]
                                        S.op("pe", lambda e, bk=bk, wt=wt, kc=kc, g=g, rhs=rhs: e.matmul(
                                            pb[bk][:, :], lhsT=wt[:, kc, g * 128:(g + 1) * 128], rhs=rhs, start=(kc == 0), stop=(kc == 7)),
                                            reads=[wtok] + hT_all[0:16], writes=["pb%d" % bk])
                                    S.op("act", lambda e, bk=bk, g=g, tg=tg: e.activation(out=QT[:, g, tg * 512:(tg + 1) * 512], in_=pb[bk][0:64, :], func=AF.Copy),
                                         reads=["pb%d" % bk], writes=["QT.%d.%d" % (g, tg)])
                                    S.op("act", lambda e, bk=bk, g=g, tg=tg: e.activation(out=KT[:, g, tg * 512:(tg + 1) * 512], in_=pb[bk][64:128, :], func=AF.Copy),
                                         reads=["pb%d" % bk], writes=["KT.%d.%d" % (g, tg)])
                            for g in range(3):
                                dil = GROUPS[g][1]
                                L = SEQ // dil
                                for half in range(2):
                                    bk = 4 + half
                                    for nb in range(8):
                                        n = half * 8 + nb
                                        r, lb = (n * 128) // L, ((n * 128) % L) // 128
                                        for kc in range(8):
                                            lhsT = permv(hT[:, kc, 0:SEQ], dil)[:, r, lb * 128:(lb + 1) * 128]
                                            S.op("pe", lambda e, bk=bk, nb=nb, lhsT=lhsT, kc=kc, g=g, wv=wv: e.matmul(
                                                pb[bk][:, nb * 64:(nb + 1) * 64], lhsT=lhsT, rhs=wv[:, kc, g * 64:(g + 1) * 64], start=(kc == 0), stop=(kc == 7)),
                                                reads=[wvtok] + hT_all[0:16], writes=["pb%d" % bk])
                                    S.op("act", lambda e, bk=bk, g=g, half=half: e.activation(
                                        out=Vh[:, g, half * 8:(half + 1) * 8, 0:64], in_=pb[bk][:, :].rearrange("p (n d) -> p n d", n=8), func=AF.Copy),
                                        reads=["pb%d" % bk, "Vh"], writes=["Vh.%d.%d" % (g, half)])
                            def front(g, n, u):
                                dil = GROUPS[g][1]
                                bpl = (SEQ // dil) // 128
                                lb = n % bpl
                                slots = ([0] if lb > 0 else []) + [1]
                                c_lo = slots[0] * 128
                                for sl in slots:
                                    kb = n - 1 if sl == 0 else n
                                    S.op("pe", lambda e, sl=sl, kb=kb, n=n, g=g, u=u: e.matmul(
                                        pb[2 + u][:, sl * 128:(sl + 1) * 128], lhsT=KT[:, g, kb * 128:(kb + 1) * 128], rhs=QT[:, g, n * 128:(n + 1) * 128],
                                        start=True, stop=True),
                                        reads=["KT.%d.%d" % (g, kb // 4), "QT.%d.%d" % (g, n // 4)], writes=["pb%d" % (2 + u)])
                                S.op("act", lambda e, u=u, c_lo=c_lo: e.activation(out=ebuf[u][:, c_lo:256], in_=pb[2 + u][:, c_lo:256], func=AF.Exp, scale=0.125),
                                     reads=["pb%d" % (2 + u)], writes=["ebuf%d" % u])
                                S.op("dve", lambda e, u=u, c_lo=c_lo, h=h, g=g: e.tensor_tensor(
                                    out=pT[u][:, c_lo:256], in0=ebuf[u][:, c_lo:256],
                                    in1=masks[:, h * 3 + g].rearrange("p s q -> p (s q)")[:, c_lo:256], op=ALU.mult),
                                    reads=["ebuf%d" % u, "masks"], writes=["pT%d" % u])

                            def back(g, n, u):
                                dil = GROUPS[g][1]
                                bpl = (SEQ // dil) // 128
                                r, lb = n // bpl, n % bpl
                                slots = ([0] if lb > 0 else []) + [1]
                                for i, sl in enumerate(slots):
                                    kb = n - 1 if sl == 0 else n
                                    S.op("pe", lambda e, u=u, sl=sl, kb=kb, g=g, i=i, last=(i == len(slots) - 1): e.matmul(
                                        pb[5 + u][:, 0:128], lhsT=Vh[:, g, kb, :], rhs=pT[u][:, sl * 128:(sl + 1) * 128], start=(i == 0), stop=last),
                                        reads=["pT%d" % u, "Vh.%d.%d" % (g, kb // 8), "Vh"], writes=["pb%d" % (5 + u)])
                                av = permv(acc[:, :], dil)[:, r, lb * 128:(lb + 1) * 128]
                                if g == 0:
                                    S.op("dve", lambda e, av=av, u=u: e.tensor_copy(out=av, in_=pb[5 + u][:, 0:128]), reads=["pb%d" % (5 + u)], writes=["acc"])
                                else:
                                    S.op("dve", lambda e, av=av, u=u: e.tensor_tensor(out=av, in0=av, in1=pb[5 + u][:, 0:128], op=ALU.add),
                                         reads=["pb%d" % (5 + u), "acc"], writes=["acc"])

                            units = [(g, n) for g in range(3) for n in range(16)]
                            front(units[0][0], units[0][1], 0)
                            front(units[1][0], units[1][1], 1)
                            for ui, (g, n) in enumerate(units):
                                if ui + 2 < len(units):
                                    front(units[ui + 2][0], units[ui + 2][1], (ui + 2) % 3)
                                back(g, n, ui % 3)
                                uctr += 1
                                if uctr % 3 == 0:
                                    prep_chunk()
                            for tg in range(4):
                                S.op("dve", lambda e, tg=tg: e.reciprocal(out=rc[:, :], in_=acc[64:128, tg * 512:(tg + 1) * 512]), reads=["acc"], writes=["rc"])
                                S.op("dve", lambda e, tg=tg, h=h: e.tensor_tensor(out=OT[:, h, tg * 512:(tg + 1) * 512], in0=acc[0:64, tg * 512:(tg + 1) * 512], in1=rc[:, :], op=ALU.mult),
                                     reads=["acc", "rc"], writes=["OT.%d" % h])

                while prep_c[0] < 128 and STAGE >= 6:
                    prep_chunk()
                OT_all = ["OT.%d" % h for h in range(8)] + ["OTs"]
                if dbg_ot is not None:
                    S.dma(lambda e: e.dma_start(out=dbg_ot, in_=OT[:]), "dbgot", reads=OT_all, final=True)
                with scope() as pa2:
                    byT = sb([128, 8, NT], BF16, pa2)
                    zT = sb([128, 8, NT], BF16, pa2)
                    if STAGE >= 5:
                        with scope() as s5:
                            extp = sb([128, SEQ + 2], F32, s5)
                            exts = sb([128, 16, 6], F32, s5)
                            stt = sb([32, D], F32, s5)
                            hv = sb([128, 512], F32, s5)
                            yb = sb([128, 512], F32, s5)
                            cvT = sb([128, 8, 34], F32, s5)
                            cvtok = sb([34, D], F32, s5)
                            S.dma(lambda e: e.dma_start(out=stt[:], in_=stconv), "const", writes=["stt"])
                            S.op("pool", lambda e: e.memset(extp[:, 0:2], 0.0), writes=["extp"])
                            for cc in range(8):
                                wt, wtok = load_w([(w_in[:, j * 1024 + cc * 128:j * 1024 + (cc + 1) * 128], 8) for j in range(3)])
                                S.op("pe", lambda e, cc=cc: e.transpose(out=pb[5][:, 0:32], in_=stt[:, cc * 128:(cc + 1) * 128], identity=identf[0:32, 0:32]),
                                     reads=["stt", "identf"], writes=["pb5"])
                                S.op("act", lambda e: e.activation(out=exts[:, :, 0:2], in_=pb[5][:, 0:32].rearrange("p (b r) -> p b r", r=2), func=AF.Copy),
                                     reads=["pb5"], writes=["exts"])
                                for gi, (t0, n) in enumerate(tgroups):
                                    for j in range(3):
                                        for kc in range(8):
                                            S.op("pe", lambda e, j=j, kc=kc, t0=t0, n=n, wt=wt: e.matmul(
                                                pb[2 + j][:, 0:n], lhsT=wt[:, kc, j * 128:(j + 1) * 128], rhs=hT[:, kc, t0:t0 + n], start=(kc == 0), stop=(kc == 7)),
                                                reads=[wtok] + hT_all, writes=["pb%d" % (2 + j)])
                                    S.op("act", lambda e, n=n: e.activation(out=hv[:, 0:n], in_=pb[4][:, 0:n], func=AF.Copy), reads=["pb4"], writes=["hv"])
                                    if t0 < SEQ:
                                        uo = extp[:, 2 + t0:2 + t0 + n]
                                        e0, e1, e2 = extp[:, t0:t0 + n], extp[:, t0 + 1:t0 + 1 + n], extp[:, t0 + 2:t0 + 2 + n]
                                        hvv, cps, bps, yv, byv = hv[:, 0:n], pb[3][:, 0:n], pb[2][:, 0:n], yb[:, 0:n], byT[:, cc, t0:t0 + n]
                                        etok = "extp"
                                    else:
                                        uo = exts[:, :, 2:6]
                                        e0, e1, e2 = exts[:, :, 0:4], exts[:, :, 1:5], exts[:, :, 2:6]
                                        v4 = lambda ap: ap.rearrange("p (b t) -> p b t", t=4)
                                        hvv, cps, bps, yv, byv = v4(hv[:, 0:n]), v4(pb[3][:, 0:n]), v4(pb[2][:, 0:n]), v4(yb[:, 0:n]), v4(byT[:, cc, t0:t0 + n])
                                        etok = "exts"
                                    S.op("dve", lambda e, uo=uo, cps=cps, hvv=hvv: e.tensor_tensor(out=uo, in0=cps, in1=hvv, op=ALU.mult),
                                         reads=["pb3", "hv", etok], writes=[etok])
                                    S.op("dve", lambda e, yv=yv, e0=e0, cc=cc: e.tensor_scalar(out=yv, in0=e0, scalar1=cwt[:, cc, 0:1], scalar2=None, op0=ALU.mult),
                                         reads=[etok, "cwt"], writes=["yb"])
                                    S.op("dve", lambda e, yv=yv, e1=e1, cc=cc: e.scalar_tensor_tensor(out=yv, in0=e1, scalar=cwt[:, cc, 1:2], in1=yv, op0=ALU.mult, op1=ALU.add),
                                         reads=[etok, "cwt", "yb"], writes=["yb"])
                                    S.op("dve", lambda e, yv=yv, e2=e2, cc=cc: e.scalar_tensor_tensor(out=yv, in0=e2, scalar=cwt[:, cc, 2:3], in1=yv, op0=ALU.mult, op1=ALU.add),
                                         reads=[etok, "cwt", "yb"], writes=["yb"])
                                    S.op("dve", lambda e, byv=byv, bps=bps, yv=yv: e.tensor_tensor(out=byv, in0=bps, in1=yv, op=ALU.mult),
                                         reads=["pb2", "yb"], writes=["byT.%d.%d" % (cc, gi)])
                                S.op("act", lambda e, cc=cc: e.activation(out=cvT[:, cc, 0:2], in_=extp[:, SEQ:SEQ + 2], func=AF.Copy), reads=["extp"], writes=["cvT.%d" % cc])
                                S.op("act", lambda e, cc=cc: e.activation(out=cvT[:, cc, 2:34].rearrange("p (b r) -> p b r", r=2), in_=exts[:, :, 4:6], func=AF.Copy),
                                     reads=["exts", "cvT.%d" % cc], writes=["cvT.%d" % cc])
                            for cc in range(8):
                                S.op("pe", lambda e, cc=cc: e.transpose(out=pb[5][0:34, 0:128], in_=cvT[:, cc, :], identity=identf[:, :]),
                                     reads=["cvT.%d" % cc, "identf"], writes=["pb5"])
                                S.op("act", lambda e, cc=cc: e.activation(out=cvtok[:, cc * 128:(cc + 1) * 128], in_=pb[5][0:34, 0:128], func=AF.Copy),
                                     reads=["pb5"], writes=["cvtok.%d" % cc])
                            cv_all = ["cvtok.%d" % cc for cc in range(8)]
                            S.dma(lambda e: e.dma_start(out=conv_p, in_=cvtok[0:2, :]), "cvst", reads=cv_all, final=True)
                            S.dma(lambda e: e.dma_start(out=conv_s, in_=cvtok[2:34, :]), "cvst", reads=cv_all, final=True)

                    if STAGE >= 5:
                        with scope() as s6:
                            sga = sb([128, 512], F32, s6)
                            sgb = sb([128, 512], F32, s6)
                            by_all = ["byT.%d.%d" % (cc, gi) for cc in range(8) for gi in range(5)]
                            for cc in range(8):
                                wt, wtok = load_w([(w_out_a[:, cc * 128:(cc + 1) * 128], 8),
                                                   (w_in[:, 7680 + cc * 128:7680 + (cc + 1) * 128], 8),
                                                   (w_in[:, 8704 + cc * 128:8704 + (cc + 1) * 128], 8),
                                                   (w_out_b[:, cc * 128:(cc + 1) * 128], 8)])
                                for gi, (t0, n) in enumerate(tgroups):
                                    for kc in range(8):
                                        S.op("pe", lambda e, kc=kc, t0=t0, n=n, wt=wt: e.matmul(pb[2][:, 0:n], lhsT=wt[:, kc, 0:128], rhs=byT[:, kc, t0:t0 + n], start=(kc == 0), stop=(kc == 7)),
                                             reads=[wtok] + by_all, writes=["pb2"])
                                    for kc in range(8):
                                        S.op("pe", lambda e, kc=kc, t0=t0, n=n, wt=wt: e.matmul(pb[3][:, 0:n], lhsT=wt[0:64, kc, 384:512], rhs=OT[:, kc, t0:t0 + n], start=(kc == 0), stop=(kc == 7)),
                                             reads=[wtok] + OT_all, writes=["pb3"])
                                    for j in range(2):
                                        for kc in range(8):
                                            S.op("pe", lambda e, j=j, kc=kc, t0=t0, n=n, wt=wt: e.matmul(pb[4 + j][:, 0:n], lhsT=wt[:, kc, 128 + j * 128:256 + j * 128], rhs=hT[:, kc, t0:t0 + n], start=(kc == 0), stop=(kc == 7)),
                                                 reads=[wtok] + hT_all, writes=["pb%d" % (4 + j)])
                                    S.op("act", lambda e, n=n: e.activation(out=sga[:, 0:n], in_=pb[4][:, 0:n], func=AF.Sigmoid), reads=["pb4"], writes=["sga"])
                                    S.op("act", lambda e, n=n: e.activation(out=sgb[:, 0:n], in_=pb[5][:, 0:n], func=AF.Sigmoid), reads=["pb5"], writes=["sgb"])
                                    S.op("dve", lambda e, n=n: e.tensor_tensor(out=sga[:, 0:n], in0=sga[:, 0:n], in1=pb[2][:, 0:n], op=ALU.mult), reads=["sga", "pb2"], writes=["sga"])
                                    S.op("dve", lambda e, n=n: e.tensor_tensor(out=sgb[:, 0:n], in0=sgb[:, 0:n], in1=pb[3][:, 0:n], op=ALU.mult), reads=["sgb", "pb3"], writes=["sgb"])
                                    S.op("dve", lambda e, n=n, cc=cc, t0=t0: e.tensor_tensor(out=zT[:, cc, t0:t0 + n], in0=sga[:, 0:n], in1=sgb[:, 0:n], op=ALU.add),
                                         reads=["sga", "sgb"], writes=["zT.%d.%d" % (cc, gi)])

                    if STAGE >= 5:
                        with scope() as s7:
                            wo = sb([128, 8, D], BF16, s7)
                            x1t = [sb([128, D], F32, s7) for _ in range(2)]
                            z_all = ["zT.%d.%d" % (cc, gi) for cc in range(8) for gi in range(5)]
                            for half in range(2):
                                wt, wtok = load_w([(w_o[:, half * 512:(half + 1) * 512], 8)])
                                S.op("pool", lambda e, wt=wt, half=half: e.tensor_copy(out=wo[:, :, half * 512:(half + 1) * 512], in_=wt[:, :, :]), reads=[wtok], writes=["wo.%d" % half])
                            for i, (t0, n) in enumerate(tiles):
                                k = i % 2
                                S.dma(lambda e, k=k, t0=t0, n=n: e.dma_start(out=x1t[k][0:n, :], in_=x_src(t0, n)), "x1ld%d" % k, writes=["x1t%d" % k])
                                for half in range(2):
                                    for kc in range(8):
                                        S.op("pe", lambda e, half=half, kc=kc, t0=t0, n=n, k=k: e.matmul(pb[2 + 2 * k + half][0:n, :], lhsT=zT[:, kc, t0:t0 + n], rhs=wo[:, kc, half * 512:(half + 1) * 512], start=(kc == 0), stop=(kc == 7)),
                                             reads=z_all + ["wo.%d" % half], writes=["pb%d" % (2 + 2 * k + half)])
                                    S.op("dve", lambda e, half=half, n=n, k=k: e.tensor_tensor(out=x1t[k][0:n, half * 512:(half + 1) * 512], in0=x1t[k][0:n, half * 512:(half + 1) * 512], in1=pb[2 + 2 * k + half][0:n, :], op=ALU.add),
                                         reads=["x1t%d" % k, "pb%d" % (2 + 2 * k + half)], writes=["x1t%d" % k])
                                S.dma(lambda e, k=k, t0=t0, n=n: e.dma_start(out=scr_x1[t0:t0 + n, :], in_=x1t[k][0:n, :]), "x1st%d" % k, reads=["x1t%d" % k], writes=["x1dram.%d" % i])

        if STAGE >= 6:
            h2T = hT
            h2_all = ["h2T.%d" % i for i in range(17)]
            i1T = sb([128, NT], F32)
            i2T = sb([128, NT], F32)
            gTt = sb([128, NT], F32)

            def x1_src(t0, n):
                return scr_x1[t0:t0 + n, :]

            with scope() as b1:
              if not os.environ.get("MK_NOB1"):
                xts = [sb([128, D], F32, b1) for _ in range(2)]
                hb = sb([128, D], BF16, b1)
                junk = sb([128, D], BF16, b1)
                ss = sb([128, 4], F32, b1)
                rmsnorm_T(x1_src, g2t, "g2t", h2T, "h2T", xts, hb, junk, ss, "n2")
                wqb = sb([128, 8, 2048], BF16, b1)
                for pc in range(4 if B1CUT >= 1 else 0):
                    wt, wtok = load_w([(wq[:, pc * 512:(pc + 1) * 512], 8)])
                    S.op("pool", lambda e, wt=wt, pc=pc: e.tensor_copy(out=wqb[:, :, pc * 512:(pc + 1) * 512], in_=wt[:, :, :]), reads=[wtok], writes=["wqb.%d" % pc])
                wq_all = ["wqb.%d" % pc for pc in range(4)]
                kyf = sb([128, 16, 128], F32, b1)
                kyb = sb([128, 16, 128], BF16, b1)
                S.dma(lambda e: e.dma_start(out=kyf[:], in_=keysT), "const", writes=["kyf"])
                S.op("pool", lambda e: e.tensor_copy(out=kyb[:], in_=kyf[:]), reads=["kyf"], writes=["kyb"])
                for g in range(3):
                    W = GROUPS[g][0]
                    nb = 16 if W == 2048 else (4 if W == 512 else 1)
                    per = 16 // nb
                    for i in range(nb):
                        src = caches[g][i * per:(i + 1) * per, 4:W].rearrange("b r k s d -> b (r k s d)")
                        dst = kvs[g][i * per:(i + 1) * per, 0:W - 4].rearrange("b r k s d -> b (r k s d)")
                        S.dma(lambda e, src=src, dst=dst: e.dma_start(out=dst, in_=src), "cpy%d" % g, eng="sp", final=True)

                qTg = sb([128, 16, 512], BF16, b1)
                scs = sb([128, 16, 128], F32, b1)
                S.op("pool", lambda e: e.memset(qTg[:], 0.0), writes=["qTg.%d" % ch for ch in range(16)])
                v12 = sb([128, 16, 16], F32, b1)
                i12 = sb([128, 16, 16], U32, b1)
                i12f = sb([128, 16, 16], F32, b1)
                wk = sb([128, 256], F32, b1)
                cand = sb([128, 8, 256], F32, b1)
                svt = sb([128, 8, 16], F32, b1)
                pos = sb([128, 8, 16], U32, b1)
                pa_ = sb([128, 8, 16], U32, b1)
                pb_ = sb([128, 8, 16], U32, b1)
                paf = sb([128, 8, 16], F32, b1)
                pbf = sb([128, 8, 16], F32, b1)
                oh = sb([128, 8, 16, 16], F32, b1)
                i1f = sb([128, 8, 16], F32, b1)
                i2f = sb([128, 8, 16], F32, b1)
                gte = sb([128, 8, 16], F32, b1)
                zs = sb([128, 8], F32, b1)
                c4 = sb([128, 2], U32, b1)
                S.op("dve", lambda e: e.memset(c4[:, 0:1], 4), writes=["c4a"])
                S.op("dve", lambda e: e.memset(c4[:, 1:2], 15), writes=["c4b"])
                tgroups_b1 = [(i * 512, 512) for i in range(4)] + [(NT - 128, 128)]
                for gi, (t0g, ng) in enumerate(tgroups_b1 if (B1CUT >= 1 and B1SUB >= 2) else []):
                    for ch in range(16):
                        bk = 2 + ch % 2
                        for kc in range(8):
                            S.op("pe", lambda e, bk=bk, ch=ch, kc=kc, t0g=t0g, ng=ng: e.matmul(pb[bk][:, 0:ng], lhsT=wqb[:, kc, ch * 128:(ch + 1) * 128], rhs=h2T[:, kc, t0g:t0g + ng], start=(kc == 0), stop=(kc == 7)),
                                 reads=wq_all + h2_all, writes=["pb%d" % bk])
                        S.op("act", lambda e, bk=bk, ch=ch, ng=ng: e.activation(out=qTg[:, ch, 0:ng], in_=pb[bk][:, 0:ng], func=AF.Copy), reads=["pb%d" % bk], writes=["qTg.%d" % ch])
                    qT_all = ["qTg.%d" % ch for ch in range(16)]
                    for tl in range((ng + 127) // 128):
                        n = min(128, ng - tl * 128)
                        t0 = t0g + tl * 128
                        if B1SUB < 3:
                            continue
                        if os.environ.get("MK_GI") and str(gi) not in os.environ["MK_GI"]:
                            continue
                        for ch in range(16):
                            S.op("pe", lambda e, ch=ch, tl=tl, n=n: e.matmul(pb[4 + ch // 4][:, (ch % 4) * 128:(ch % 4 + 1) * 128], lhsT=qTg[:, ch, tl * 128:tl * 128 + 128], rhs=kyb[:, ch, :], start=True, stop=True),
                                 reads=qT_all + ["kyb"], writes=["sc%d" % ch])
                        if B1CUT < 2:
                            continue
                        for bq in range(4):
                            S.op("act", lambda e, bq=bq: e.activation(out=scs[:, bq * 4:(bq + 1) * 4, :], in_=pb[4 + bq][:, :].rearrange("p (c k) -> p c k", c=4), func=AF.Copy),
                                 reads=["sc%d" % (bq * 4 + j) for j in range(4)], writes=["sc%d" % (bq * 4 + j) for j in range(4)] + ["scs%d" % bq])
                        for ch in range(16):
                            sv_ = scs[0:n, ch, :]
                            S.op("dve", lambda e, sv_=sv_, ch=ch, n=n: e.max(out=v12[0:n, ch, 0:8], in_=sv_), reads=["scs%d" % (ch // 4)], writes=["v12a"])
                            S.op("dve", lambda e, sv_=sv_, ch=ch, n=n: e.max_index(out=i12[0:n, ch, 0:8], in_max=v12[0:n, ch, 0:8], in_values=sv_), reads=["scs%d" % (ch // 4), "v12a"], writes=["i12a"])
                            S.op("dve", lambda e, sv_=sv_, ch=ch, n=n: e.match_replace(out=wk[0:n, 0:128], in_to_replace=v12[0:n, ch, 0:8], in_values=sv_, imm_value=-1e30), reads=["scs%d" % (ch // 4), "v12a"], writes=["wk"])
                            S.op("dve", lambda e, ch=ch, n=n: e.max(out=v12[0:n, ch, 8:16], in_=wk[0:n, 0:128]), reads=["wk"], writes=["v12b"])
                            S.op("dve", lambda e, ch=ch, n=n: e.max_index(out=i12[0:n, ch, 8:16], in_max=v12[0:n, ch, 8:16], in_values=wk[0:n, 0:128]), reads=["wk", "v12b"], writes=["i12.%d" % ch, "v12.%d" % ch])
                        if B1CUT < 3:
                            continue
                        v_all = ["v12.%d" % ch for ch in range(16)]
                        i_all = ["i12.%d" % ch for ch in range(16)]
                        v12v = v12[:, :, :].rearrange("p (h f) k -> p h f k", f=2)
                        S.op("dve", lambda e, n=n, v12v=v12v: e.tensor_tensor(
                            out=cand[0:n].rearrange("p h (a b) -> p h a b", a=16), in0=v12v[0:n, :, 0, :].unsqueeze(3).broadcast_to([n, 8, 16, 16]),
                            in1=v12v[0:n, :, 1, :].unsqueeze(2).broadcast_to([n, 8, 16, 16]), op=ALU.add), reads=v_all, writes=["cand"])
                        S.op("dve", lambda e, n=n: e.tensor_copy(out=i12f[0:n], in_=i12[0:n]), reads=i_all, writes=["i12f"])
                        for h in range(8):
                            S.op("dve", lambda e, h=h, n=n: e.max(out=svt[0:n, h, 0:8], in_=cand[0:n, h, :]), reads=["cand"], writes=["sva"])
                            S.op("dve", lambda e, h=h, n=n: e.max_index(out=pos[0:n, h, 0:8], in_max=svt[0:n, h, 0:8], in_values=cand[0:n, h, :]), reads=["cand", "sva"], writes=["posa"])
                            S.op("dve", lambda e, h=h, n=n: e.match_replace(out=wk[0:n, :], in_to_replace=svt[0:n, h, 0:8], in_values=cand[0:n, h, :], imm_value=-1e30), reads=["cand", "sva"], writes=["wk"])
                            S.op("dve", lambda e, h=h, n=n: e.max(out=svt[0:n, h, 8:16], in_=wk[0:n, :]), reads=["wk"], writes=["svb"])
                            S.op("dve", lambda e, h=h, n=n: e.max_index(out=pos[0:n, h, 8:16], in_max=svt[0:n, h, 8:16], in_values=wk[0:n, :]), reads=["wk", "svb"], writes=["pos.%d" % h, "sv.%d" % h])
                        if B1CUT < 4:
                            continue
                        p_all = ["pos.%d" % h for h in range(8)]
                        s_all = ["sv.%d" % h for h in range(8)]
                        S.op("dve", lambda e, n=n: e.tensor_scalar(out=pa_[0:n], in0=pos[0:n], scalar1=c4[0:n, 0:1], scalar2=None, op0=ALU.logical_shift_right), reads=p_all + ["c4a"], writes=["pa_"])
                        S.op("dve", lambda e, n=n: e.tensor_scalar(out=pb_[0:n], in0=pos[0:n], scalar1=c4[0:n, 1:2], scalar2=None, op0=ALU.bitwise_and), reads=p_all + ["c4b"], writes=["pb_"])
                        S.op("dve", lambda e, n=n: e.tensor_copy(out=paf[0:n], in_=pa_[0:n]), reads=["pa_"], writes=["paf"])
                        S.op("dve", lambda e, n=n: e.tensor_copy(out=pbf[0:n], in_=pb_[0:n]), reads=["pb_"], writes=["pbf"])
                        i12v = i12f[:, :, :].rearrange("p (h f) k -> p h f k", f=2)
                        for (pf, pfn, f, dst, dn) in ((paf, "paf", 0, i1f, "i1f"), (pbf, "pbf", 1, i2f, "i2f")):
                            S.op("dve", lambda e, n=n, pf=pf: e.tensor_tensor(
                                out=oh[0:n], in0=pf[0:n].unsqueeze(3).broadcast_to([n, 8, 16, 16]),
                                in1=iota[0:n, 0:16].unsqueeze(1).unsqueeze(1).broadcast_to([n, 8, 16, 16]), op=ALU.is_equal), reads=[pfn, "iota"], writes=["oh"])
                            S.op("dve", lambda e, n=n, f=f, i12v=i12v: e.tensor_tensor(
                                out=oh[0:n], in0=oh[0:n], in1=i12v[0:n, :, f, :].unsqueeze(2).broadcast_to([n, 8, 16, 16]), op=ALU.mult), reads=["oh", "i12f"], writes=["oh"])
                            S.op("dve", lambda e, n=n, dst=dst: e.tensor_reduce(out=dst[0:n], in_=oh[0:n], axis=AX.X, op=ALU.add), reads=["oh"], writes=[dn])
                        if B1CUT < 5:
                            continue
                        S.op("dve", lambda e, n=n: e.tensor_tensor(out=gte[0:n], in0=svt[0:n], in1=svt[0:n, :, 0:1].broadcast_to([n, 8, 16]), op=ALU.subtract), reads=s_all, writes=["gte"])
                        S.op("act", lambda e, n=n: e.activation(out=gte[0:n], in_=gte[0:n], func=AF.Exp), reads=["gte"], writes=["gte"])
                        S.op("dve", lambda e, n=n: e.tensor_reduce(out=zs[0:n], in_=gte[0:n], axis=AX.X, op=ALU.add), reads=["gte"], writes=["zs"])
                        S.op("dve", lambda e, n=n: e.reciprocal(out=zs[0:n], in_=zs[0:n]), reads=["zs"], writes=["zs"])
                        S.op("dve", lambda e, n=n: e.tensor_tensor(out=gte[0:n], in0=gte[0:n], in1=zs[0:n, :].unsqueeze(2).broadcast_to([n, 8, 16]), op=ALU.mult), reads=["gte", "zs"], writes=["gte"])
                        for (srcx, sn, dstx, dn) in ((i1f, "i1f", i1T, "i1T"), (i2f, "i2f", i2T, "i2T"), (gte, "gte", gTt, "gTt")):
                            S.op("pe", lambda e, srcx=srcx, n=n: e.transpose(out=pb[2][:, 0:n], in_=srcx[0:n].rearrange("p h k -> p (h k)"), identity=identf[0:n, 0:n]),
                                 reads=[sn, "identf"], writes=["pb2"])
                            S.op("act", lambda e, dstx=dstx, t0=t0, n=n: e.activation(out=dstx[:, t0:t0 + n], in_=pb[2][:, 0:n], func=AF.Copy), reads=["pb2"], writes=[dn])

            with scope() as b2:
              if STAGE >= 7:
                  TS = 384
                  Wsb = sb([128, TS, 128], BF16, b2)
                  wb0 = wbf[0][:].rearrange("p a b -> p (a b)")
                  wb1 = wbf[1][:].rearrange("p a b -> p (a b)")
                  E1 = wb0[:, 0:2048].rearrange("p (t i) -> p t i", t=16)
                  E2 = wb0[:, 2048:4096].rearrange("p (t i) -> p t i", t=16)
                  G2 = wb1[:, 0:2048].rearrange("p (t i) -> p t i", t=16)
                  NSLOT = 4
                  wsb16 = wst[:].bitcast(BF16).rearrange("p a b -> p (a b)")
                  utb = [wsb16[:, sl * 2048:sl * 2048 + 1024].rearrange("p (k e) -> p k e", k=8) for sl in range(NSLOT)]
                  vtb = [wsb16[:, sl * 2048 + 1024:(sl + 1) * 2048] for sl in range(NSLOT)]
                  ge = [sb([128, TS], F32, b2) for _ in range(2)]
                  WA = [sb([128, TS], BF16, b2) for _ in range(2)]
                  ysb = [sb([128, D], F32, b2) for _ in range(2)]
                  x1r = [sb([128, D], F32, b2)] * 2
                  fg = sb([128, D], F32, b2)
                  junk2 = wb1[:, 2048:3072]
                  ss2 = sb([128, 4], F32, b2)
                  S.dma(lambda e: e.dma_start(out=fg[:], in_=fgb.broadcast_to([128, D])), "const", writes=["fg"])
                  stiles = [(i * TS, TS) for i in range(5)] + [(5 * TS, NT - 5 * TS)]
                  lctr = 0
                  fctr = 0
                  for (s0, T) in stiles:
                      for sbk in range(T // 16):
                          tt0 = sbk * 16
                          S.op("dve", lambda e, s0=s0, tt0=tt0: e.tensor_tensor(
                              out=E1, in0=iota[:, :].unsqueeze(1).broadcast_to([128, 16, 128]),
                              in1=i1T[:, s0 + tt0:s0 + tt0 + 16].unsqueeze(2).broadcast_to([128, 16, 128]), op=ALU.is_equal), reads=["iota", "i1T"], writes=["E1"])
                          S.op("dve", lambda e, s0=s0, tt0=tt0: e.tensor_tensor(
                              out=E2, in0=iota[:, :].unsqueeze(1).broadcast_to([128, 16, 128]),
                              in1=i2T[:, s0 + tt0:s0 + tt0 + 16].unsqueeze(2).broadcast_to([128, 16, 128]), op=ALU.is_equal), reads=["iota", "i2T"], writes=["E2"])
                          S.op("dve", lambda e, s0=s0, tt0=tt0: e.tensor_tensor(
                              out=G2, in0=E2, in1=gTt[:, s0 + tt0:s0 + tt0 + 16].unsqueeze(2).broadcast_to([128, 16, 128]), op=ALU.mult), reads=["E2", "gTt"], writes=["G2"])
                          for q4 in range(4):
                              bk = 6 + q4 % 2
                              for j in range(4):
                                  tl = q4 * 4 + j
                                  S.op("pe", lambda e, bk=bk, j=j, tl=tl: e.matmul(pb[bk][:, j * 128:(j + 1) * 128], lhsT=G2[:, tl, :], rhs=E1[:, tl, :], start=True, stop=True),
                                       reads=["G2", "E1"], writes=["pb%d" % bk])
                              S.op("act", lambda e, bk=bk, tt0=tt0, q4=q4: e.activation(out=Wsb[:, tt0 + q4 * 4:tt0 + q4 * 4 + 4, :], in_=pb[bk][:, :].rearrange("p (t i) -> p t i", t=4), func=AF.Copy),
                                   reads=["pb%d" % bk], writes=["Wsb"])
                      ntile = (T + 127) // 128
                      def ld(c):
                          sl = c % NSLOT
                          S.dma(lambda e, c=c, sl=sl: e.dma_start(out=utb[sl], in_=scr_ut[c]), "utb%d" % sl, reads=["scr_ut%d" % c], writes=["utb%d" % sl])
                          S.dma(lambda e, c=c, sl=sl: e.dma_start(out=vtb[sl], in_=scr_v[c]), "vtb%d" % sl, reads=["scr_v%d" % c], writes=["vtb%d" % sl])

                      def actp(c):
                          sl = c % NSLOT
                          k = c % 2
                          for kc in range(8):
                              S.op("pe", lambda e, k=k, kc=kc, sl=sl, s0=s0, T=T: e.matmul(pb[6 + k][:, 0:T], lhsT=utb[sl][:, kc, :], rhs=h2T[:, kc, s0:s0 + T], start=(kc == 0), stop=(kc == 7)),
                                   reads=["utb%d" % sl] + h2_all, writes=["pb%d" % (6 + k)])
                          S.op("act", lambda e, k=k, T=T: e.activation(out=ge[k][:, 0:T], in_=pb[6 + k][:, 0:T], func=AF.Gelu), reads=["pb%d" % (6 + k)], writes=["ge%d" % k])
                          S.op("dve", lambda e, k=k, T=T, c=c: e.tensor_tensor(out=WA[k][:, 0:T], in0=ge[k][:, 0:T], in1=Wsb[:, 0:T, c], op=ALU.mult), reads=["ge%d" % k, "Wsb"], writes=["WA%d" % k])

                      def vp(c):
                          sl = c % NSLOT
                          k = c % 2
                          for ti in range(ntile):
                              n = min(128, T - ti * 128)
                              for half in range(2):
                                  S.op("pe", lambda e, ti=ti, half=half, n=n, k=k, sl=sl, c=c: e.matmul(pb[2 * ti + half][0:n, :], lhsT=WA[k][:, ti * 128:ti * 128 + n], rhs=vtb[sl][:, half * 512:(half + 1) * 512], start=(c == 0), stop=(c == 127)),
                                       reads=["WA%d" % k, "vtb%d" % sl], writes=["pb%d" % (2 * ti + half)])

                      for c in range(NSLOT - 1):
                          ld(c)
                      actp(0)
                      for c in range(128):
                          if c + NSLOT - 1 < 128:
                              ld(c + NSLOT - 1)
                          if c + 1 < 128:
                              actp(c + 1)
                          vp(c)
                      for ti in range(ntile):
                          n = min(128, T - ti * 128)
                          t0 = s0 + ti * 128
                          k = fctr % 2
                          fctr += 1
                          tix = t0 // 128
                          S.dma(lambda e, k=k, t0=t0, n=n: e.dma_start(out=x1r[k][0:n, :], in_=scr_x1[t0:t0 + n, :]), "x1r", reads=["x1dram.%d" % tix], writes=["x1r"])
                          for half in range(2):
                              S.op("dve", lambda e, k=k, n=n, ti=ti, half=half: e.tensor_tensor(out=ysb[k][0:n, half * 512:(half + 1) * 512], in0=x1r[k][0:n, half * 512:(half + 1) * 512], in1=pb[2 * ti + half][0:n, :], op=ALU.add),
                                   reads=["x1r", "pb%d" % (2 * ti + half)], writes=["ysb%d" % k])
                          S.op("dve", lambda e: e.memset(ss2[:, 0:1], 0.0), writes=["ss2"])
                          S.op("act", lambda e, k=k, n=n: e.activation(out=junk2[0:n, :], in_=ysb[k][0:n, :], func=AF.Square, accum_out=ss2[0:n, 0:1]), reads=["ysb%d" % k, "ss2"], writes=["junk2", "ss2"])
                          S.op("act", lambda e, n=n: e.activation(out=ss2[0:n, 1:2], in_=ss2[0:n, 0:1], func=AF.Sqrt, bias=epst[0:n, :], scale=1.0 / D), reads=["ss2", "eps"], writes=["ss21"])
                          S.op("dve", lambda e, n=n: e.reciprocal(out=ss2[0:n, 2:3], in_=ss2[0:n, 1:2]), reads=["ss21"], writes=["ss22"])
                          S.op("dve", lambda e, k=k, n=n: e.scalar_tensor_tensor(out=ysb[k][0:n, :], in0=ysb[k][0:n, :], scalar=ss2[0:n, 2:3], in1=fg[0:n, :], op0=ALU.mult, op1=ALU.mult),
                               reads=["ysb%d" % k, "ss22", "fg"], writes=["ysb%d" % k])
                          dst = y_p[t0:t0 + n, :] if t0 < SEQ else y_s[:, :]
                          S.dma(lambda e, k=k, n=n, dst=dst: e.dma_start(out=dst, in_=ysb[k][0:n, :]), "yst%d" % k, reads=["ysb%d" % k], final=True)

        with nc.Block() as block:
            S.emit(block)
    return nc


_PROG = None


def kernel(x_prompt, x_sample, cache_kv_w128, cache_kv_w512, cache_kv_w2048, state_conv,
           norm1_g, w_in, conv_w, w_out_a, w_out_b, w_o, norm2_g,
           peer_wq, peer_keys, peer_u, peer_v, final_g):
    global _PROG
    if _PROG is None:
        _PROG = build_program()
    nc = _PROG
    f = lambda a: np.ascontiguousarray(np.asarray(a, dtype=np.float32))
    consts = _consts()
    shared = dict(
        g1T=f(np.asarray(norm1_g)[0].reshape(8, 128).T),
        g2T=f(np.asarray(norm2_g)[0].reshape(8, 128).T),
        w_in=f(np.asarray(w_in)[0]),
        convw=f(np.asarray(conv_w)[0].reshape(3, 8, 128).transpose(2, 1, 0)),
        w_out_a=f(np.asarray(w_out_a)[0]),
        w_out_b=f(np.asarray(w_out_b)[0]),
        w_o=f(np.asarray(w_o)[0]),
        wq=f(np.asarray(peer_wq)[0]),
        keysT=f(np.asarray(peer_keys)[0].transpose(1, 0, 2, 3).reshape(16, 128, 128).transpose(2, 0, 1)),
        uT=f(np.asarray(peer_u)[0].T),
        vtab=f(np.asarray(peer_v)[0]),
        fgb=f(np.asarray(final_g).reshape(1, D)),
    )
    shared.update(consts)
    caches = [np.asarray(cache_kv_w128)[0], np.asarray(cache_kv_w512)[0], np.asarray(cache_kv_w2048)[0]]
    xpr = np.asarray(x_prompt)
    xsa = np.asarray(x_sample)
    stc = np.asarray(state_conv)[0]
    in_maps = []
    for c in range(NCORES):
        m = dict(shared)
        m["xp"] = f(xpr[c])
        m["xs"] = f(xsa[c * 16:(c + 1) * 16].reshape(NS, D))
        for g in range(3):
            m["cache%d" % g] = f(caches[g][c * 16:(c + 1) * 16])
        m["stconv"] = f(stc[c * 16:(c + 1) * 16].reshape(32, D))
        in_maps.append(m)
    ncr = int(os.environ.get("MK_CORES", str(NCORES)))
    res = run_bass_kernel_spmd(nc, in_maps[0:ncr], core_ids=list(range(ncr)))
    R = list(res.results)
    while len(R) < NCORES:
        R.append(R[0])
    y_prompt = np.stack([R[c]["y_p"] for c in range(NCORES)], 0)
    y_sample = np.concatenate([R[c]["y_s"].reshape(16, 4, D) for c in range(NCORES)], 0)
    kvp = [np.stack([R[c]["kvp%d" % g] for c in range(NCORES)], 0)[None] for g in range(3)]
    convp = np.stack([R[c]["conv_p"] for c in range(NCORES)], 0)[None]
    kvs = [np.concatenate([R[c]["kvs%d" % g] for c in range(NCORES)], 0)[None] for g in range(3)]
    convs = np.concatenate([R[c]["conv_s"].reshape(16, 2, D) for c in range(NCORES)], 0)[None]
    return (y_prompt.astype(np.float32), y_sample.astype(np.float32), kvp[0], kvp[1], kvp[2], convp,
            kvs[0], kvs[1], kvs[2], convs)
```
